# Optimizing a Trainium2 kernel written in Bass

```python
import math
import jax, jax.numpy as jnp
from jax import lax
import numpy as np

D_MODEL = 1024
BATCH = 16
SEQ = 2048
DEPTH = 1

HEAD_DIM = 64
SB_HEADS = 8
SB_WIDTH = SB_HEADS * HEAD_DIM
RWKV_HEADS = 8
RWKV_WIDTH = RWKV_HEADS * HEAD_DIM
DECAY_RANK = 64
ICLR_RANK = 64
GATE_RANK = 128
RWKV_COLS = 3 * RWKV_WIDTH + DECAY_RANK + ICLR_RANK + GATE_RANK
IN_COLS = 3 * SB_WIDTH + RWKV_COLS + 2 * D_MODEL
PLE_DIM = 256
N_EXPERTS = 32
TOP_K = 4
EXPERT_HIDDEN = D_MODEL
SWIGLU_LIMIT = 7.0
SWIGLU_ALPHA = 1.702
Q_BLOCK = 128
MOE_BLOCK = 256
RMS_EPS = 1e-5
GN_EPS = 64e-5

kernel_name = "hybrid_stickbreak_rwkv7_moe_block"


def rmsnorm(x, g):
    xf = x.astype(jnp.float32)
    y = xf * lax.rsqrt(jnp.mean(xf * xf, axis=-1, keepdims=True) + RMS_EPS)
    return (y * g.astype(jnp.float32)).astype(x.dtype)


def stick_breaking_attention(q, k, v):
    S = q.shape[2]
    scale = HEAD_DIM ** -0.5
    outs = []
    for blk in range(S // Q_BLOCK):
        t0 = blk * Q_BLOCK
        kl = t0 + Q_BLOCK
        qb = q[:, :, t0:kl]
        kb = k[:, :, :kl]
        vb = v[:, :, :kl]
        z = jnp.einsum('bhqd,bhkd->bhqk', qb, kb).astype(jnp.float32) * scale
        t_idx = t0 + jnp.arange(Q_BLOCK)[:, None]
        s_idx = jnp.arange(kl)[None, :]
        mask = s_idx < t_idx
        log1m = jnp.where(mask, -jax.nn.softplus(z), 0.0)
        after = lax.cumsum(log1m, axis=3, reverse=True) - log1m
        log_a = jax.nn.log_sigmoid(z) + after
        att = jnp.where(mask, jnp.exp(log_a), 0.0)
        outs.append(jnp.einsum('bhqk,bhkd->bhqd', att.astype(vb.dtype), vb))
    return jnp.concatenate(outs, axis=2)


def rwkv7_scan(r, w, k, v, kk, a):
    B, S, H, N = r.shape

    def step(state, inp):
        r_t, w_t, k_t, v_t, kk_t, a_t = inp
        sa = jnp.einsum('bhij,bhj->bhi', state, -kk_t)
        state = (state * w_t[:, :, None, :]
                 + sa[..., None] * (kk_t * a_t)[:, :, None, :]
                 + v_t[..., None] * k_t[:, :, None, :])
        y = jnp.einsum('bhij,bhj->bhi', state, r_t)
        return state, y

    xs = tuple(jnp.moveaxis(t, 1, 0) for t in (r, w, k, v, kk, a))
    state0 = jnp.zeros((B, H, N, N), jnp.float32)
    _, ys = lax.scan(step, state0, xs)
    return jnp.moveaxis(ys, 0, 1)


def rwkv7_branch(zr, mu, w0, w_up, a0, a_up, g_up, k_k, k_a, r_k, lnx_w, lnx_b):
    B, S, _ = zr.shape
    prev = jnp.pad(zr, ((0, 0), (1, 0), (0, 0)))[:, :-1]
    zr = zr + mu * (prev - zr)
    c1, c2, c3 = RWKV_WIDTH, 2 * RWKV_WIDTH, 3 * RWKV_WIDTH
    c4, c5 = c3 + DECAY_RANK, c3 + DECAY_RANK + ICLR_RANK
    r, k, v = zr[..., :c1], zr[..., c1:c2], zr[..., c2:c3]
    xw, xa, xg = zr[..., c3:c4], zr[..., c4:c5], zr[..., c5:]
    w_log = -jax.nn.softplus(-(w0 + jnp.tanh(xw) @ w_up)) - 0.5
    decay = jnp.exp(-jnp.exp(w_log.astype(jnp.float32)))
    a = jax.nn.sigmoid(a0 + xa @ a_up)
    g = jax.nn.sigmoid(xg) @ g_up
    kk = k * k_k
    hs = lambda t: t.astype(jnp.float32).reshape(B, S, RWKV_HEADS, HEAD_DIM)
    r_h, k_h, v_h, kk_h, a_h, w_h = hs(r), hs(k), hs(v), hs(kk), hs(a), hs(decay)
    kk_h = kk_h / jnp.maximum(jnp.linalg.norm(kk_h, axis=-1, keepdims=True), 1e-12)
    k_h = k_h * (1.0 + (a_h - 1.0) * hs(k_a * jnp.ones_like(k)))
    y = rwkv7_scan(r_h, w_h, k_h, v_h, kk_h, a_h)
    mean = jnp.mean(y, axis=-1, keepdims=True)
    var = jnp.mean(jnp.square(y - mean), axis=-1, keepdims=True)
    y = (y - mean) * lax.rsqrt(var + GN_EPS)
    y = y.reshape(B, S, RWKV_WIDTH) * lnx_w + lnx_b
    bonus = jnp.sum(r_h * k_h * r_k.astype(jnp.float32), axis=-1, keepdims=True) * v_h
    y = y + bonus.reshape(B, S, RWKV_WIDTH)
    return (y * g).astype(zr.dtype)


def moe_ffn(h, router_w, router_b, w1, b1, w2, b2):
    Bsz, S, D = h.shape
    N = Bsz * S
    NK = N * TOP_K
    hf = h.reshape(N, D)
    logits = (hf @ router_w + router_b).astype(jnp.float32)
    top_val, top_idx = lax.top_k(logits, TOP_K)
    gate = jax.nn.softmax(top_val, axis=-1)
    flat_e = top_idx.reshape(-1).astype(jnp.int32)
    flat_tok = jnp.arange(NK, dtype=jnp.int32) // TOP_K
    flat_w = gate.reshape(-1)
    order = jnp.argsort(flat_e)
    sorted_e = flat_e[order]
    counts = jnp.zeros((N_EXPERTS,), jnp.int32).at[flat_e].add(1)
    offsets = jnp.cumsum(counts) - counts
    padded = (counts + MOE_BLOCK - 1) // MOE_BLOCK * MOE_BLOCK
    pad_end = jnp.cumsum(padded)
    pad_off = pad_end - padded
    rank = jnp.arange(NK, dtype=jnp.int32) - offsets[sorted_e]
    dest = pad_off[sorted_e] + rank
    n_blocks = -(-NK // MOE_BLOCK) + N_EXPERTS
    P = n_blocks * MOE_BLOCK
    buf_tok = jnp.zeros((P,), jnp.int32).at[dest].set(flat_tok[order])
    buf_w = jnp.zeros((P,), jnp.float32).at[dest].set(flat_w[order])
    block_start = jnp.arange(n_blocks, dtype=jnp.int32) * MOE_BLOCK
    block_e = jnp.minimum(jnp.searchsorted(pad_end, block_start, side='right'), N_EXPERTS - 1)

    def expert_block(args):
        e, tok = args
        xb = hf[tok]
        gu = xb @ w1[e] + b1[e]
        g_lin, u_lin = gu[:, ::2], gu[:, 1::2]
        g_lin = jnp.minimum(g_lin, SWIGLU_LIMIT)
        u_lin = jnp.clip(u_lin, -SWIGLU_LIMIT, SWIGLU_LIMIT)
        glu = g_lin * jax.nn.sigmoid(SWIGLU_ALPHA * g_lin)
        return ((u_lin + 1.0) * glu) @ w2[e] + b2[e]

    y = lax.map(expert_block, (block_e, buf_tok.reshape(n_blocks, MOE_BLOCK)))
    y = y.reshape(P, D) * buf_w[:, None].astype(y.dtype)
    out = jnp.zeros((N, D), h.dtype).at[buf_tok].add(y.astype(h.dtype))
    return out.reshape(Bsz, S, D)


def setup_inputs(seed: int = 0) -> dict:
    key = jax.random.key(seed)
    ks = jax.random.split(key, 32)
    L, D, E, F = DEPTH, D_MODEL, N_EXPERTS, EXPERT_HIDDEN
    nrm = lambda k, shape, s: jax.random.normal(k, shape, jnp.float32) * s
    return {
        "x": nrm(ks[0], (BATCH, SEQ, D), 1.0),
        "p": nrm(ks[1], (L, BATCH, SEQ, PLE_DIM), 1.0),
        "mix_norm_g": 1.0 + nrm(ks[2], (L, D), 0.02),
        "w_in": nrm(ks[3], (L, D, IN_COLS), D ** -0.5),
        "rwkv_mu": jax.random.uniform(ks[4], (L, RWKV_COLS), jnp.float32),
        "rwkv_w0": jax.random.uniform(ks[5], (L, RWKV_WIDTH), jnp.float32, -4.0, 1.0),
        "rwkv_w_up": nrm(ks[6], (L, DECAY_RANK, RWKV_WIDTH), 0.5 * DECAY_RANK ** -0.5),
        "rwkv_a0": nrm(ks[7], (L, RWKV_WIDTH), 0.1),
        "rwkv_a_up": nrm(ks[8], (L, ICLR_RANK, RWKV_WIDTH), ICLR_RANK ** -0.5),
        "rwkv_g_up": nrm(ks[9], (L, GATE_RANK, RWKV_WIDTH), GATE_RANK ** -0.5),
        "rwkv_k_k": 0.85 + nrm(ks[10], (L, RWKV_WIDTH), 0.05),
        "rwkv_k_a": 1.0 + nrm(ks[11], (L, RWKV_WIDTH), 0.05),
        "rwkv_r_k": nrm(ks[12], (L, RWKV_HEADS, HEAD_DIM), 0.1),
        "rwkv_lnx_w": 1.0 + nrm(ks[13], (L, RWKV_WIDTH), 0.02),
        "rwkv_lnx_b": nrm(ks[14], (L, RWKV_WIDTH), 0.01),
        "w_out_a": nrm(ks[15], (L, SB_WIDTH, D), SB_WIDTH ** -0.5),
        "w_out_b": nrm(ks[16], (L, RWKV_WIDTH, D), RWKV_WIDTH ** -0.5),
        "w_out": nrm(ks[17], (L, D, D), D ** -0.5),
        "ffn_norm_g": 1.0 + nrm(ks[18], (L, D), 0.02),
        "router_w": nrm(ks[19], (L, D, E), D ** -0.5),
        "router_b": nrm(ks[20], (L, E), 0.01),
        "exp_w1": nrm(ks[21], (L, E, D, 2 * F), D ** -0.5),
        "exp_b1": nrm(ks[22], (L, E, 2 * F), 0.01),
        "exp_w2": nrm(ks[23], (L, E, F, D), F ** -0.5),
        "exp_b2": nrm(ks[24], (L, E, D), 0.01),
        "ple_norm_g": 1.0 + nrm(ks[25], (L, D), 0.02),
        "ple_gate_w": nrm(ks[26], (L, D, D), D ** -0.5),
        "ple_proj_w": nrm(ks[27], (L, PLE_DIM, D), PLE_DIM ** -0.5),
        "final_norm_g": 1.0 + nrm(ks[28], (D,), 0.02),
    }


def reference(x, p, mix_norm_g, w_in, rwkv_mu, rwkv_w0, rwkv_w_up, rwkv_a0, rwkv_a_up,
              rwkv_g_up, rwkv_k_k, rwkv_k_a, rwkv_r_k, rwkv_lnx_w, rwkv_lnx_b,
              w_out_a, w_out_b, w_out, ffn_norm_g, router_w, router_b,
              exp_w1, exp_b1, exp_w2, exp_b2, ple_norm_g, ple_gate_w, ple_proj_w,
              final_norm_g):
    B, S, D = x.shape
    for i in range(DEPTH):
        h = rmsnorm(x, mix_norm_g[i])
        z = h @ w_in[i]
        c_sb = 3 * SB_WIDTH
        c_rw = c_sb + RWKV_COLS
        zq, zk, zv = (z[..., j * SB_WIDTH:(j + 1) * SB_WIDTH] for j in range(3))
        to_heads = lambda t: t.reshape(B, S, SB_HEADS, HEAD_DIM).transpose(0, 2, 1, 3)
        ya = stick_breaking_attention(to_heads(zq), to_heads(zk), to_heads(zv))
        ya = ya.transpose(0, 2, 1, 3).reshape(B, S, SB_WIDTH)
        yb = rwkv7_branch(z[..., c_sb:c_rw], rwkv_mu[i], rwkv_w0[i], rwkv_w_up[i], rwkv_a0[i],
                          rwkv_a_up[i], rwkv_g_up[i], rwkv_k_k[i], rwkv_k_a[i], rwkv_r_k[i],
                          rwkv_lnx_w[i], rwkv_lnx_b[i])
        gate_a = jax.nn.sigmoid(z[..., c_rw:c_rw + D])
        gate_b = jax.nn.sigmoid(z[..., c_rw + D:])
        merged = gate_a * (ya @ w_out_a[i]) + gate_b * (yb @ w_out_b[i])
        x = x + (merged @ w_out[i]).astype(x.dtype)
        x = x + moe_ffn(rmsnorm(x, ffn_norm_g[i]), router_w[i], router_b[i],
                        exp_w1[i], exp_b1[i], exp_w2[i], exp_b2[i])
        hp = rmsnorm(x, ple_norm_g[i])
        x = x + (jax.nn.sigmoid(hp @ ple_gate_w[i]) * (p[i] @ ple_proj_w[i])).astype(x.dtype)
    return rmsnorm(x, final_norm_g)
```

```python
import numpy as np
import concourse.bass as bass
import concourse.mybir as mybir
from concourse.bass_utils import run_bass_kernel_spmd

F32 = mybir.dt.float32
BF16 = mybir.dt.bfloat16
I32 = mybir.dt.int32
U32 = mybir.dt.uint32
AF = mybir.ActivationFunctionType
ALU = mybir.AluOpType
AX = mybir.AxisListType

D = 1024
KC = 8
HD = 64
NH = 8
IN_COLS = 5376
RW_COLS = 1792
RMS_EPS = 1e-5


class Prog:
    ENGS = ["pe", "act", "dve", "pool", "sp"]
    EPOCH = 20000
    NDMA = 8

    def __init__(self, nc, same_engine_sync=True):
        self.nc = nc
        self.ops = {e: [] for e in self.ENGS}
        self.ncomp = {e: 0 for e in self.ENGS}
        self.last_write = {}
        self.readers = {}
        self.seen = {e: {} for e in self.ENGS}
        self.sems = {}
        self.dma_cnt = {}
        self.dma_n = {e: 0 for e in self.ENGS}
        self.same_engine_sync = same_engine_sync
        self.sem_ctx = []

    def _sem(self, key):
        if key not in self.sems:
            cm = self.nc.semaphore("s_" + "_".join(str(k) for k in key))
            h = cm.__enter__()
            self.sem_ctx.append(cm)
            self.sems[key] = h
        return self.sems[key]

    def _deps(self, reads, writes):
        deps = []
        for r in reads:
            t = self.last_write.get(r)
            if t is not None:
                deps.append(t)
        for w in writes:
            t = self.last_write.get(w)
            if t is not None:
                deps.append(t)
            deps.extend(self.readers.get(w, ()))
        return deps

    def _commit(self, tok, reads, writes):
        for w in writes:
            self.last_write[w] = tok
            self.readers[w] = []
        for r in reads:
            if r in writes:
                continue
            self.readers.setdefault(r, []).append(tok)

    def _waits(self, eng, deps):
        waits = {}
        for (semkey, val, src_eng) in deps:
            if src_eng == eng and (eng == "pe" or not self.same_engine_sync) and semkey[0] == "c":
                continue
            if self.seen[eng].get(semkey, 0) >= val:
                continue
            if waits.get(semkey, 0) < val:
                waits[semkey] = val
        for k, v in waits.items():
            self.seen[eng][k] = v
        return list(waits.items())

    _cap = None

    def merged(self, fa, fb):
        A = []; B = []
        self._cap = A; fa()
        self._cap = B
        if fb is not None:
            fb()
        self._cap = None
        ia = ib = 0
        while ia < len(A) or ib < len(B):
            if ib >= len(B) or (ia < len(A) and ia * len(B) <= ib * len(A)):
                it = A[ia]; ia += 1
            else:
                it = B[ib]; ib += 1
            (self.op if it[0] == "op" else self.dma)(*it[1], **it[2])

    def op(self, eng, fn, reads=(), writes=()):
        if self._cap is not None:
            self._cap.append(("op", (eng, fn), dict(reads=list(reads), writes=list(writes))))
            return None
        reads = list(reads); writes = list(writes)
        deps = self._deps(reads, writes)
        waits = self._waits(eng, deps)
        k = self.ncomp[eng]
        self.ncomp[eng] += 1
        semkey = ("c", eng, k // self.EPOCH)
        val = k % self.EPOCH + 1
        tok = (semkey, val, eng)
        self.ops[eng].append((waits, fn, semkey, 1))
        self._commit(tok, reads, writes)
        return tok

    def dma(self, eng, fn, reads=(), writes=(), grp=""):
        if self._cap is not None:
            self._cap.append(("dma", (eng, fn), dict(reads=list(reads), writes=list(writes), grp=grp)))
            return None
        reads = list(reads); writes = list(writes)
        deps = self._deps(reads, writes)
        n = self.dma_n.get(eng + grp, 0)
        self.dma_n[eng + grp] = n + 1
        semkey = ("d", eng + grp, n % self.NDMA)
        cnt = self.dma_cnt.get(semkey, 0)
        if cnt > 0:
            deps.append((semkey, cnt * 16, eng + "_dma"))
        waits = self._waits(eng, deps)
        self.dma_cnt[semkey] = cnt + 1
        tok = (semkey, (cnt + 1) * 16, eng + "_dma")
        self.ops[eng].append((waits, fn, semkey, 16))
        self._commit(tok, reads, writes)
        return tok

    def barrier(self):
        deps = [(k, c * 16, "x") for k, c in self.dma_cnt.items()]
        for e in self.ENGS:
            k = self.ncomp[e]
            if k > 0:
                deps.append((("c", e, (k - 1) // self.EPOCH), (k - 1) % self.EPOCH + 1, "x"))
        for e in self.ENGS:
            waits = self._waits(e, deps)
            if waits:
                self.ops[e].append((waits, None, None, 0))

    def finish(self, eng="sp"):
        deps = [(k, c * 16, "x") for k, c in self.dma_cnt.items()]
        waits = self._waits(eng, deps)
        self.ops[eng].append((waits, None, None, 0))

    def emit(self):
        nc = self.nc
        for e in self.ENGS:
            for (waits, fn, semkey, amt) in self.ops[e]:
                for k, v in waits:
                    self._sem(k)
                if semkey is not None:
                    self._sem(semkey)
        handles = {"pe": "tensor", "act": "scalar", "dve": "vector", "pool": "gpsimd", "sp": "sync"}
        with nc.Block() as block:
            for e in self.ENGS:
                def body(engine, e=e):
                    for (waits, fn, semkey, amt) in self.ops[e]:
                        for k, v in waits:
                            engine.wait_ge(self.sems[k], v)
                        if fn is not None:
                            ins = fn(engine)
                            ins.then_inc(self.sems[semkey], amt)
                getattr(block, handles[e])(body)
        for cm in reversed(self.sem_ctx):
            cm.__exit__(None, None, None)


class Ctx:
    pass


def rmsnorm_tile(p, c, xt_ap, xt_key, gB, out_ap, out_key, tag, junk, ss, rstd):
    p.op("act", lambda e: e.activation(out=junk[:], in_=xt_ap, func=AF.Square, accum_out=ss[:]),
         reads=[xt_key], writes=[tag + "junk", tag + "ss"])
    p.op("act", lambda e: e.activation(out=rstd[:], in_=ss[:], func=AF.Sqrt, scale=1.0 / D, bias=c.eps_t[:]),
         reads=[tag + "ss", "eps_t"], writes=[tag + "rstd"])
    p.op("dve", lambda e: e.reciprocal(out=rstd[:], in_=rstd[:]), reads=[tag + "rstd"], writes=[tag + "rstd"])
    p.op("dve", lambda e: e.scalar_tensor_tensor(out=out_ap, in0=xt_ap, scalar=rstd[:], in1=gB[:],
                                                  op0=ALU.mult, op1=ALU.mult),
         reads=[xt_key, tag + "rstd", "gB_" + tag], writes=[out_key])


def stage1(p, nc, c):
    T = c.T
    NT = T // 128
    NG = T // c.TG
    TG = c.TG
    from contextlib import ExitStack
    with ExitStack() as es:
        def sb(name, shape, dt):
            return es.enter_context(nc.sbuf_tensor(name, shape, dt))
        def ps(name, shape, dt):
            return es.enter_context(nc.psum_tensor(name, shape, dt))
        gB = sb("s1_gB", [128, D], F32)
        xt = [sb(f"s1_xt{i}", [128, D], F32) for i in range(2)]
        junk = sb("s1_junk", [128, D], BF16)
        ss = sb("s1_ss", [128, 1], F32)
        rstd = sb("s1_rstd", [128, 1], F32)
        xn = [sb(f"s1_xn{i}", [128, D], BF16) for i in range(2)]
        hT = sb("s1_hT", [128, KC, T], BF16)
        wb = [sb(f"s1_wb{i}", [128, KC, 512], BF16) for i in range(2)]
        evf = [sb(f"s1_evf{i}", [128, TG], F32) for i in range(3)]
        evb = [sb(f"s1_evb{i}", [128, 512], BF16) for i in range(3)]
        psT = [ps(f"s1_psT{i}", [128, D], BF16) for i in range(2)]
        pz = [ps(f"s1_pz{i}", [128, 512], F32) for i in range(4)]

        p.dma("sp", lambda e: e.dma_start(out=gB[:], in_=c.mix_norm_g.partition_broadcast(128)), writes=["gB_s1"])
        for i in range(NT):
            b = i % 2
            p.dma("sp", lambda e, i=i, b=b: e.dma_start(out=xt[b][:], in_=c.x[i * 128:(i + 1) * 128, :]),
                  writes=[f"s1_xt{b}"])
            rmsnorm_tile(p, c, xt[b][:], f"s1_xt{b}", gB, xn[b][:], f"s1_xn{b}", "s1", junk, ss, rstd)
            for kc in range(KC):
                p.op("pe", lambda e, b=b, kc=kc: e.transpose(out=psT[b][:, kc * 128:(kc + 1) * 128],
                                                              in_=xn[b][:, kc * 128:(kc + 1) * 128],
                                                              identity=c.ident_b[:]),
                     reads=[f"s1_xn{b}", "ident_b"], writes=[f"s1_psT{b}"])
            eng = "act" if i % 2 == 0 else "dve"
            if eng == "act":
                p.op("act", lambda e, b=b, i=i: e.activation(out=hT[:, :, i * 128:(i + 1) * 128],
                                                            in_=psT[b][:].rearrange("p (k t) -> p k t", k=KC),
                                                            func=AF.Copy),
                     reads=[f"s1_psT{b}"], writes=[f"s1_hT{i // (TG // 128)}"])
            else:
                p.op("dve", lambda e, b=b, i=i: e.tensor_copy(out=hT[:, :, i * 128:(i + 1) * 128],
                                                             in_=psT[b][:].rearrange("p (k t) -> p k t", k=KC)),
                     reads=[f"s1_psT{b}"], writes=[f"s1_hT{i // (TG // 128)}"])

        w_v = c.w_in.rearrange("(kc p) n -> p kc n", p=128)
        nblk = (IN_COLS + 511) // 512
        cnt = 0
        for blk in range(nblk):
            c0 = blk * 512
            cw = min(512, IN_COLS - c0)
            wbb = wb[blk % 2]
            wkey = f"s1_wb{blk % 2}"
            p.dma("pool", lambda e, wbb=wbb, c0=c0, cw=cw: e.dma_start(out=wbb[:, :, 0:cw], in_=w_v[:, :, c0:c0 + cw]),
                  writes=[wkey])
            if c0 == 1024:
                for i in range(NT):
                    pzz = pz[cnt % 4]; pk = f"s1_pz{cnt % 4}"
                    ev = evb[cnt % 3]; ek = f"s1_evb{cnt % 3}"
                    for kc in range(KC):
                        p.op("pe", lambda e, pzz=pzz, kc=kc, i=i, wbb=wbb: e.matmul(
                            pzz[:], hT[:, kc, i * 128:(i + 1) * 128], wbb[:, kc, :], start=(kc == 0), stop=(kc == KC - 1)),
                            reads=[f"s1_hT{i // (TG // 128)}", wkey], writes=[pk])
                    if cnt % 2 == 0:
                        p.op("act", lambda e, pzz=pzz, ev=ev: e.activation(out=ev[:], in_=pzz[:], func=AF.Copy),
                             reads=[pk], writes=[ek])
                    else:
                        p.op("dve", lambda e, pzz=pzz, ev=ev: e.tensor_copy(out=ev[:], in_=pzz[:]),
                             reads=[pk], writes=[ek])
                    p.dma("sp", lambda e, ev=ev, i=i: e.dma_start(out=c.V[i * 128:(i + 1) * 128, :], in_=ev[:]),
                          reads=[ek], writes=[])
                    cnt += 1
                continue
            for ch in range(cw // 128):
                col = c0 + ch * 128
                for g in range(NG):
                    pzz = pz[cnt % 4]; pk = f"s1_pz{cnt % 4}"
                    for kc in range(KC):
                        p.op("pe", lambda e, pzz=pzz, kc=kc, g=g, ch=ch, wbb=wbb: e.matmul(
                            pzz[:, 0:TG], wbb[:, kc, ch * 128:(ch + 1) * 128], hT[:, kc, g * TG:(g + 1) * TG],
                            start=(kc == 0), stop=(kc == KC - 1)),
                            reads=[f"s1_hT{g}", wkey], writes=[pk])
                    if col < 1024:
                        ev = evb[cnt % 3]; ek = f"s1_evb{cnt % 3}"
                        sc = 0.125 if col < 512 else 1.0
                        dst = (c.QT if col < 512 else c.KT)
                        r0 = col % 512
                        p.op("act", lambda e, pzz=pzz, ev=ev, sc=sc: e.activation(out=ev[:, 0:TG], in_=pzz[:, 0:TG], func=AF.Copy, scale=sc),
                             reads=[pk], writes=[ek])
                        p.dma("sp", lambda e, ev=ev, dst=dst, r0=r0, g=g: e.dma_start(
                            out=dst[r0:r0 + 128, g * TG:(g + 1) * TG], in_=ev[:, 0:TG]), reads=[ek], writes=[])
                    elif col < 1536 + RW_COLS:
                        ev = evf[cnt % 3]; ek = f"s1_evf{cnt % 3}"
                        r0 = col - 1536
                        p.op("dve", lambda e, pzz=pzz, ev=ev: e.tensor_copy(out=ev[:, 0:TG], in_=pzz[:, 0:TG]),
                             reads=[pk], writes=[ek])
                        p.dma("sp", lambda e, ev=ev, r0=r0, g=g: e.dma_start(
                            out=c.ZR[r0:r0 + 128, g * TG:(g + 1) * TG], in_=ev[:, 0:TG]), reads=[ek], writes=[])
                    else:
                        ev = evb[cnt % 3]; ek = f"s1_evb{cnt % 3}"
                        r0 = col - (1536 + RW_COLS)
                        p.op("act", lambda e, pzz=pzz, ev=ev: e.activation(out=ev[:, 0:TG], in_=pzz[:, 0:TG], func=AF.Sigmoid),
                             reads=[pk], writes=[ek])
                        p.dma("sp", lambda e, ev=ev, r0=r0, g=g: e.dma_start(
                            out=c.GT[r0:r0 + 128, g * TG:(g + 1) * TG], in_=ev[:, 0:TG]), reads=[ek], writes=[])
                    cnt += 1


def stage2(p, nc, c):
    S = c.S
    QG = min(512, S)
    NQG = S // QG
    nbg = QG // 128
    NKB = S // 128
    from contextlib import ExitStack
    with ExitStack() as es:
        def sb(name, shape, dt):
            return es.enter_context(nc.sbuf_tensor(name, shape, dt))
        def ps(name, shape, dt):
            return es.enter_context(nc.psum_tensor(name, shape, dt))
        qT = sb("s2_qT", [64, NH, S], BF16)
        kT = sb("s2_kT", [64, NH, S], BF16)
        vv = sb("s2_v", [128, NKB, 512], BF16)
        Lall = [[sb(f"s2_L{pp}_{i}", [128, QG], BF16) for i in range(NKB)] for pp in range(2)]
        Eb = [sb(f"s2_E{i}", [128, QG], F32) for i in range(2)]
        att = [sb(f"s2_att{i}", [128, QG], BF16) for i in range(3)]
        yas = [sb(f"s2_ya{i}", [64, QG], BF16) for i in range(2)]
        pz = [ps(f"s2_pz{i}", [128, QG], F32) for i in range(2)]
        pa = [ps(f"s2_pa{i}", [128, QG], F32) for i in range(3)]
        py = [ps(f"s2_py{i}", [64, QG], F32) for i in range(2)]
        cntA = [0]; cntB = [0]; cntY = [0]
        for b in range(c.NB):
            t0 = b * S
            p.dma("sp", lambda e, t0=t0: e.dma_start(out=qT[:], in_=c.QT.rearrange("(h d) t -> d h t", d=64)[:, :, t0:t0 + S]),
                  reads=[], writes=["s2_q"])
            p.dma("sp", lambda e, t0=t0: e.dma_start(out=kT[:], in_=c.KT.rearrange("(h d) t -> d h t", d=64)[:, :, t0:t0 + S]),
                  reads=[], writes=["s2_k"])
            p.dma("sp", lambda e, t0=t0: e.dma_start(out=vv[:], in_=c.V[t0:t0 + S, :].rearrange("(i p) n -> p i n", p=128)),
                  reads=[], writes=["s2_v"])
            work = [(h, G) for h in range(NH) for G in range(NQG)]
            def phaseA(h, G, par, t0=t0):
                L = Lall[par]
                nkb = (G + 1) * nbg
                q0 = G * QG
                for kb in range(nkb):
                    j = kb - G * nbg
                    c0 = max(0, j) * 128
                    ia = cntA[0]; cntA[0] += 1
                    pzz = pz[ia % 2]; pk = f"s2_pz{ia % 2}"
                    E = Eb[ia % 2]; ek = f"s2_E{ia % 2}"
                    p.op("pe", lambda e, pzz=pzz, kb=kb, c0=c0: e.matmul(
                        pzz[:, c0:QG], kT[:, h, kb * 128:(kb + 1) * 128], qT[:, h, q0 + c0:q0 + QG], start=True, stop=True),
                        reads=["s2_q", "s2_k"], writes=[pk])
                    p.op("act", lambda e, pzz=pzz, E=E, c0=c0: e.activation(out=E[:, c0:QG], in_=pzz[:, c0:QG], func=AF.Exp),
                         reads=[pk], writes=[ek])
                    p.op("act", lambda e, E=E, kb=kb, c0=c0: e.activation(out=L[kb][:, c0:QG], in_=E[:, c0:QG], func=AF.Ln, bias=1.0),
                         reads=[ek], writes=[f"s2_L{par}_{kb}"])
                    if j >= 0:
                        p.op("dve", lambda e, kb=kb, c0=c0: e.tensor_tensor(out=L[kb][:, c0:c0 + 128], in0=L[kb][:, c0:c0 + 128],
                                                                              in1=c.maskS[:], op=ALU.mult),
                             reads=[f"s2_L{par}_{kb}", "maskS"], writes=[f"s2_L{par}_{kb}"])
            def phaseB(h, G, par, t0=t0):
                L = Lall[par]
                nkb = (G + 1) * nbg
                q0 = G * QG
                iy = cntY[0]; cntY[0] += 1
                pyy = py[iy % 2]; pyk = f"s2_py{iy % 2}"
                ya = yas[iy % 2]; yak = f"s2_ya{iy % 2}"
                for kb in range(nkb):
                    j = kb - G * nbg
                    c0 = max(0, j) * 128
                    ib = cntB[0]; cntB[0] += 1
                    paa = pa[ib % 3]; pk = f"s2_pa{ib % 3}"
                    at = att[ib % 3]; ak = f"s2_att{ib % 3}"
                    p.op("pe", lambda e, paa=paa, kb=kb, c0=c0: e.matmul(
                        paa[:, c0:QG], kT[:, h, kb * 128:(kb + 1) * 128], qT[:, h, q0 + c0:q0 + QG], start=True, stop=False),
                        reads=["s2_q", "s2_k"], writes=[pk])
                    for kb2 in range(nkb - 1, kb - 1, -1):
                        j2 = kb2 - G * nbg
                        c2 = max(0, j2) * 128
                        lhs = c.negU if kb2 == kb else c.negOnes
                        p.op("pe", lambda e, paa=paa, lhs=lhs, kb2=kb2, c2=c2, stp=(kb2 == kb): e.matmul(
                            paa[:, c2:QG], lhs[:], L[kb2][:, c2:QG], start=False, stop=stp),
                            reads=[f"s2_L{par}_{kb2}", "negU"], writes=[pk])
                    p.op("act", lambda e, paa=paa, at=at, c0=c0: e.activation(out=at[:, c0:QG], in_=paa[:, c0:QG], func=AF.Exp),
                         reads=[pk], writes=[ak])
                    if j >= 0:
                        p.op("dve", lambda e, at=at, c0=c0: e.tensor_tensor(out=at[:, c0:c0 + 128], in0=at[:, c0:c0 + 128],
                                                                             in1=c.maskS[:], op=ALU.mult),
                             reads=[ak, "maskS"], writes=[ak])
                    p.op("pe", lambda e, pyy=pyy, at=at, kb=kb, c0=c0, nkb=nkb: e.matmul(
                        pyy[:, c0:QG], vv[:, kb, h * 64:(h + 1) * 64], at[:, c0:QG], start=(kb == 0), stop=(kb == nkb - 1)),
                        reads=[ak, "s2_v"], writes=[pyk])
                p.op("dve", lambda e, pyy=pyy, ya=ya: e.tensor_copy(out=ya[:], in_=pyy[:]), reads=[pyk], writes=[yak])
                p.dma("sp", lambda e, ya=ya: e.dma_start(
                    out=c.YA[h * 64:(h + 1) * 64, t0 + q0:t0 + q0 + QG], in_=ya[:]), reads=[yak], writes=[])
            phaseA(work[0][0], work[0][1], 0)
            for wi, (h, G) in enumerate(work):
                par = wi % 2
                if wi + 1 < len(work):
                    hn_, Gn_ = work[wi + 1]
                    p.merged(lambda: phaseB(h, G, par), lambda: phaseA(hn_, Gn_, 1 - par))
                else:
                    phaseB(h, G, par)


def stage3(p, nc, c):
    S = c.S
    TG = min(512, S)
    NG = S // TG
    NTG = TG // 128
    SC = 32
    HI = c.HI
    R32 = c.R32
    from contextlib import ExitStack
    with ExitStack() as es:
        def sb(name, shape, dt=F32):
            return es.enter_context(nc.sbuf_tensor("s3_" + name, shape, dt))
        def ps(name, shape, dt=F32):
            return es.enter_context(nc.psum_tensor("s3_" + name, shape, dt))
        cap = {"buf": None}
        def flat(ks):
            out = []
            for k in ks:
                if isinstance(k, (tuple, list)):
                    out.extend(k)
                else:
                    out.append(k)
            return out
        def o(eng, fn, reads, writes):
            it = ("op", eng, fn, ["s3_" + r for r in flat(reads)], ["s3_" + w for w in flat(writes)])
            if cap["buf"] is not None:
                cap["buf"].append(it)
            else:
                p.op(eng, fn, reads=it[3], writes=it[4])
        def d(eng, fn, reads=(), writes=()):
            it = ("dma", eng, fn, list(reads), list(writes))
            if cap["buf"] is not None:
                cap["buf"].append(it)
            else:
                p.dma(eng, fn, reads=it[3], writes=it[4])
        def run_item(it):
            if it[0] == "op":
                p.op(it[1], it[2], reads=it[3], writes=it[4])
            else:
                p.dma(it[1], it[2], reads=it[3], writes=it[4])
        def merged(fa, fb):
            A = []; B = []
            cap["buf"] = A; fa(); cap["buf"] = B
            if fb is not None:
                fb()
            cap["buf"] = None
            ia = ib = 0
            while ia < len(A) or ib < len(B):
                if ib >= len(B) or (ia < len(A) and ia * len(B) <= ib * len(A)):
                    run_item(A[ia]); ia += 1
                else:
                    run_item(B[ib]); ib += 1
        def r32(ap):
            return ap.bitcast(mybir.dt.float32r) if R32 else ap

        pc = sb("pc", [64, 80]); omka = sb("omka", [64, 8]); mul = sb("mul", [128, 2])
        WL = sb("WL", [128, 512]); GU = sb("GU", [128, 512]); resetm = sb("resetm", [64, TG])
        M1 = sb("M1", [128, 128]); MB = sb("MB", [128, 256]); MC = sb("MC", [128, 256])
        identf = sb("identf", [128, 128]); ones64 = sb("ones64", [64, 64]); TI0 = sb("TI0", [128, 64])
        for nm, t, src in (("pc", pc, c.pc_d), ("mul", mul, c.mul_d), ("GU", GU, c.g_up), ("M1", M1, c.M1_d), ("MB", MB, c.MB_d),
                           ("MC", MC, c.MC_d), ("identf", identf, c.identf_d), ("ones64", ones64, c.ones64_d), ("TI0", TI0, c.TI0_d)):
            p.dma("sp", lambda e, t=t, src=src: e.dma_start(out=t[:], in_=src), writes=["s3_" + nm])
        p.dma("sp", lambda e: e.dma_start(out=WL[0:64, :], in_=c.w_up), writes=["s3_WL"])
        p.dma("sp", lambda e: e.dma_start(out=WL[64:128, :], in_=c.a_up), writes=["s3_WL"])
        p.dma("sp", lambda e: e.dma_start(out=resetm[:], in_=c.resetm_d[:, 0:TG]), writes=["s3_resetm"])
        PMU, PW0, PA0, PKK, PKA, PRK, PLW, PLB = 0, 24, 32, 40, 48, 56, 64, 72
        o("act", lambda e: e.activation(out=WLb[:], in_=WL[:], func=AF.Copy), ["WL"], ["WLb"])
        o("act", lambda e: e.activation(out=GUb[:], in_=GU[:], func=AF.Copy), ["GU"], ["GUb"])
        o("act", lambda e: e.activation(out=ones64b[:], in_=ones64[:], func=AF.Copy), ["ones64"], ["ones64b"])
        o("dve", lambda e: e.tensor_scalar(out=omka[:], in0=pc[:, PKA:PKA + 8], scalar1=-1.0, scalar2=1.0, op0=ALU.mult, op1=ALU.add), ["pc"], ["omka"])

        zl = [sb(f"zl{i}", [128, TG]) for i in range(2)]
        zlb = [sb(f"zlb{i}", [128, TG], BF16) for i in range(2)]
        WLb = sb("WLb", [128, 512], BF16); GUb = sb("GUb", [128, 512], BF16); ones64b = sb("ones64b", [64, 64], BF16)
        tmpAb = sb("tmpAb", [64, TG], BF16); tmpBb = sb("tmpBb", [64, TG], BF16)
        zlp = [sb(f"zlp{i}", [128, TG]) for i in range(2)]
        z3 = sb("z3", [64, 3, TG]); pv3 = sb("pv3", [64, 3, TG])
        logw = sb("logw", [64, TG]); av = sb("av", [64, TG]); kk = sb("kk", [64, TG]); tmpA = sb("tmpA", [64, TG]); rn = sb("rn", [64, TG])
        kkn = sb("kkn", [64, TG]); kp = sb("kp", [64, TG]); bv = sb("bv", [64, TG]); LG = sb("LG", [64, TG]); eNeg = sb("eNeg", [64, TG])
        tmpB = sb("tmpB", [64, TG]); eD = sb("eD", [64, TG]); eLG = sb("eLG", [64, TG])
        KR = [sb(f"KR{i}", [64, 2, TG], BF16) for i in range(2)]; btil = [sb(f"btil{i}", [64, TG], BF16) for i in range(2)]
        ktil = [sb(f"ktil{i}", [64, TG], BF16) for i in range(2)]; BK = [sb(f"BK{i}", [64, 2, TG], BF16) for i in range(2)]
        vv = [sb(f"vv{i}", [64, TG], BF16) for i in range(2)]; GC = [sb(f"GC{i}", [64, TG // SC]) for i in range(2)]
        gv = [sb(f"gv{j}", [64, TG]) for j in range(2 * HI)]; bon = [sb(f"bon{j}", [64, TG]) for j in range(2 * HI)]
        PHI = [[sb(f"PHI{j}_{t}", [128, 4, 64]) for t in range(NTG)] for j in range(HI)]
        RYT = [[sb(f"RYT{j}_{t}", [128, 128]) for t in range(NTG)] for j in range(HI)]
        NBUF = NTG
        def tl(name, shape):
            return [sb(f"{name}{i}", shape, BF16) for i in range(NBUF)]
        idb = c.ident_b[0:64, 0:64]
        def qb_(q):
            return q[:].bitcast(BF16)
        Nm = tl("Nm", [128, 128]); NTm = tl("NTm", [128, 128]); AT = tl("AT", [128, 3, 128])
        KA = tl("KA", [128, 128]); ZV = tl("ZV", [128, 128]); BKtok = tl("BKtok", [128, 128]); X3 = tl("X3", [32, 192])
        Ma = tl("Ma", [128, 128]); Mat = tl("Mat", [128, 128]); Mb = tl("Mb", [128, 128]); Mbt = tl("Mbt", [128, 128])
        Ra = tl("Ra", [128, 128]); Rat = tl("Rat", [128, 128]); Rb = tl("Rb", [128, 128]); Rbt = tl("Rbt", [128, 128])
        nWX = tl("nWX", [128, 128]); nWX3 = tl("nWX3", [32, 128]); ZV3 = tl("ZV3", [32, 128])
        TIall = [[sb(f"TIst{hh}_{i}", [128, 64]) for i in range(2)] for hh in range(NH)]
        ti_cur = [0] * NH

        assert HI <= 4
        py2 = [ps(f"py{i}", [128, 512]) for i in range(2)]
        pL = [ps(f"pL{i}", [128, 512]) for i in range(2)]
        pq = [ps(f"pq{i}", [128, 512]) for i in range(2)]
        pS2 = [ps(f"pS{i}", [128, 512]) for i in range(2)]
        cnt = {"q": 0, "L": 0}
        def nq():
            i = cnt["q"] % 2; cnt["q"] += 1
            return pq[i], f"pq{i}"
        banks6 = [(pq[0], ("pq0",)), (pq[1], ("pq1",)), (py2[0], ("py0", "py1")), (py2[1], ("py2", "py3")),
                  (pS2[0], ("pS0", "pS1")), (pS2[1], ("pS2", "pS3"))]
        cnt["q6"] = 0
        def nq6():
            i = cnt["q6"] % 6; cnt["q6"] += 1
            return banks6[i]
        def nL():
            i = cnt["L"] % 2; cnt["L"] += 1
            return pL[i], f"pL{i}"
        for i in range(NBUF):
            o("pool", lambda e, i=i: e.memset(ZV[i][:, 0:64], 0.0), [], [f"ZV{i}"])
            o("pool", lambda e, i=i: e.memset(ZV3[i][:, 0:64], 0.0), [], [f"X3b{i}"])

        def lora_prep(b, G, t0):
            for li in range(2):
                r0 = 1536 + li * 128
                d("sp", lambda e, li=li, r0=r0: e.dma_start(out=zl[li][:], in_=c.ZR[r0:r0 + 128, t0:t0 + TG]), reads=[], writes=[f"s3_zl{li}"])
                if G == 0:
                    o("pool", lambda e, li=li: e.memset(zlp[li][:, 0:1], 0.0), [], [f"zlp{li}"])
                    d("sp", lambda e, li=li, r0=r0: e.dma_start(out=zlp[li][:, 1:TG], in_=c.ZR[r0:r0 + 128, t0:t0 + TG - 1]), reads=[], writes=[f"s3_zlp{li}"])
                else:
                    d("sp", lambda e, li=li, r0=r0: e.dma_start(out=zlp[li][:], in_=c.ZR[r0:r0 + 128, t0 - 1:t0 + TG - 1]), reads=[], writes=[f"s3_zlp{li}"])
                o("dve", lambda e, li=li: e.tensor_tensor(out=zlp[li][:], in0=zlp[li][:], in1=zl[li][:], op=ALU.subtract), [f"zlp{li}", f"zl{li}"], [f"zlp{li}"])
                o("dve", lambda e, li=li: e.scalar_tensor_tensor(out=zl[li][:], in0=zlp[li][:], scalar=mul[:, li:li + 1], in1=zl[li][:], op0=ALU.mult, op1=ALU.add),
                  [f"zlp{li}", f"zl{li}", "mul"], [f"zl{li}"])
            o("act", lambda e: e.activation(out=zlb[0][0:64, :], in_=zl[0][0:64, :], func=AF.Tanh), ["zl0"], ["zlb0"])
            o("act", lambda e: e.activation(out=zlb[0][64:128, :], in_=zl[0][64:128, :], func=AF.Copy), ["zl0"], ["zlb0"])
            o("act", lambda e: e.activation(out=zlb[1][:], in_=zl[1][:], func=AF.Sigmoid), ["zl1"], ["zlb1"])

        def head_prep(h, j, hp, G, t0, js):
            hc = slice(h * 64, (h + 1) * 64)
            zv = c.ZR[0:1536, :].rearrange("(three hh d) t -> d three hh t", three=3, hh=NH)
            d("sp", lambda e: e.dma_start(out=z3[:], in_=zv[:, :, h, t0:t0 + TG]), reads=[], writes=["s3_z3"])
            if G == 0:
                o("pool", lambda e: e.memset(pv3[:, :, 0:1], 0.0), [], ["pv3"])
                d("sp", lambda e: e.dma_start(out=pv3[:, :, 1:TG], in_=zv[:, :, h, t0:t0 + TG - 1]), reads=[], writes=["s3_pv3"])
            else:
                d("sp", lambda e: e.dma_start(out=pv3[:], in_=zv[:, :, h, t0 - 1:t0 + TG - 1]), reads=[], writes=["s3_pv3"])
            o("pool", lambda e: e.tensor_tensor(out=pv3[:], in0=pv3[:], in1=z3[:], op=ALU.subtract), ["pv3", "z3"], ["pv3"])
            for q in range(3):
                dst = pv3[:, q, :] if q < 2 else vv[hp][:]
                dk = "pv3" if q < 2 else f"vv{hp}"
                o("dve", lambda e, q=q, dst=dst: e.scalar_tensor_tensor(out=dst, in0=pv3[:, q, :], scalar=pc[:, PMU + q * 8 + h:PMU + q * 8 + h + 1],
                                                                        in1=z3[:, q, :], op0=ALU.mult, op1=ALU.add), ["pv3", "z3", "pc"], [dk])
            rr = pv3[:, 0, :]; kr = pv3[:, 1, :]; vr = vv[hp][:]
            pa, pak = nL()
            o("pe", lambda e: e.matmul(pa[0:64, 0:TG], WLb[0:64, hc], zlb[0][0:64, :], start=True, stop=True), ["WLb", "zlb0"], [pak])
            o("act", lambda e: e.activation(out=logw[:], in_=pa[0:64, 0:TG], func=AF.Sigmoid, bias=pc[:, PW0 + h:PW0 + h + 1]), [pak, "pc"], ["logw"])
            pb, pbk = nL()
            o("pe", lambda e: e.matmul(pb[0:64, 0:TG], WLb[64:128, hc], zlb[0][64:128, :], start=True, stop=True), ["WLb", "zlb0"], [pbk])
            o("act", lambda e: e.activation(out=av[:], in_=pb[0:64, 0:TG], func=AF.Sigmoid, bias=pc[:, PA0 + h:PA0 + h + 1]), [pbk, "pc"], ["av"])
            pg_, pgk = nL()
            o("pe", lambda e: e.matmul(pg_[0:64, 0:TG], GUb[:, hc], zlb[1][:], start=True, stop=True), ["GUb", "zlb1"], [pgk])
            o("act", lambda e: e.activation(out=gv[js][:], in_=pg_[0:64, 0:TG], func=AF.Copy), [pgk], [f"gv{js}"])
            o("pool", lambda e: e.tensor_scalar(out=logw[:], in0=logw[:], scalar1=-0.6065306597126334, scalar2=None, op0=ALU.mult), ["logw"], ["logw"])
            o("dve", lambda e: e.tensor_scalar(out=kk[:], in0=kr, scalar1=pc[:, PKK + h:PKK + h + 1], scalar2=None, op0=ALU.mult), ["pv3", "pc"], ["kk"])
            o("act", lambda e: e.activation(out=tmpAb[:], in_=kk[:], func=AF.Square), ["kk"], ["tmpAb"])
            pk_, pkk = nL()
            o("pe", lambda e: e.matmul(pk_[0:64, 0:TG], ones64b[:], tmpAb[:], start=True, stop=True), ["ones64b", "tmpAb"], [pkk])
            o("act", lambda e: e.activation(out=rn[:], in_=pk_[0:64, 0:TG], func=AF.Sqrt, bias=c.tiny_t[0:64, :]), [pkk, "tiny"], ["rn"])
            o("dve", lambda e: e.reciprocal(out=rn[:], in_=rn[:]), ["rn"], ["rn"])
            o("dve", lambda e: e.tensor_tensor(out=kkn[:], in0=kk[:], in1=rn[:], op=ALU.mult), ["kk", "rn"], ["kkn"])
            o("pool", lambda e: e.tensor_scalar(out=tmpB[:], in0=av[:], scalar1=pc[:, PKA + h:PKA + h + 1], scalar2=omka[:, h:h + 1], op0=ALU.mult, op1=ALU.add),
              ["av", "pc", "omka"], ["tmpB"])
            o("pool", lambda e: e.tensor_tensor(out=kp[:], in0=kr, in1=tmpB[:], op=ALU.mult), ["pv3", "tmpB"], ["kp"])
            o("pool", lambda e: e.tensor_tensor(out=bv[:], in0=av[:], in1=kkn[:], op=ALU.mult), ["av", "kkn"], ["bv"])
            o("dve", lambda e: e.tensor_tensor_scan(out=LG[:], data0=resetm[:], data1=logw[:], initial=0.0, op0=ALU.mult, op1=ALU.add), ["resetm", "logw"], ["LG"])
            o("act", lambda e: e.activation(out=eLG[:], in_=LG[:], func=AF.Exp), ["LG"], ["eLG"])
            o("act", lambda e: e.activation(out=eNeg[:], in_=LG[:], func=AF.Exp, scale=-1.0), ["LG"], ["eNeg"])
            o("pool", lambda e: e.tensor_tensor(out=tmpA[:], in0=LG[:], in1=logw[:], op=ALU.subtract), ["LG", "logw"], ["tmpA"])
            o("act", lambda e: e.activation(out=tmpA[:], in_=tmpA[:], func=AF.Exp), ["tmpA"], ["tmpA"])
            o("dve", lambda e: e.tensor_tensor(out=KR[hp][:, 0, :], in0=kkn[:], in1=tmpA[:], op=ALU.mult), ["kkn", "tmpA"], [f"KR{hp}"])
            o("dve", lambda e: e.tensor_tensor(out=KR[hp][:, 1, :], in0=rr, in1=eLG[:], op=ALU.mult), ["pv3", "eLG"], [f"KR{hp}"])
            o("pool", lambda e: e.tensor_copy(out=GC[hp][:], in_=eLG[:].rearrange("p (c s) -> p c s", s=SC)[:, :, SC - 1]), ["eLG"], [f"GC{hp}"])
            o("pool", lambda e: e.tensor_tensor(out=btil[hp][:], in0=bv[:], in1=eNeg[:], op=ALU.mult), ["bv", "eNeg"], [f"btil{hp}"])
            o("pool", lambda e: e.tensor_tensor(out=ktil[hp][:], in0=kp[:], in1=eNeg[:], op=ALU.mult), ["kp", "eNeg"], [f"ktil{hp}"])
            LG3 = LG[:].rearrange("p (c s) -> p c s", s=SC)
            o("dve", lambda e: e.tensor_tensor(out=eD[:].rearrange("p (c s) -> p c s", s=SC), in0=LG3[:, :, SC - 1:SC].to_broadcast([64, TG // SC, SC]),
                                               in1=LG3, op=ALU.subtract), ["LG"], ["eD"])
            o("act", lambda e: e.activation(out=eD[:], in_=eD[:], func=AF.Exp), ["eD"], ["eD"])
            o("dve", lambda e: e.tensor_tensor(out=BK[hp][:, 0, :], in0=bv[:], in1=eD[:], op=ALU.mult), ["bv", "eD"], [f"BK{hp}"])
            o("pool", lambda e: e.tensor_tensor(out=BK[hp][:, 1, :], in0=kp[:], in1=eD[:], op=ALU.mult), ["kp", "eD"], [f"BK{hp}"])
            o("dve", lambda e: e.scalar_tensor_tensor(out=tmpBb[:], in0=rr, scalar=pc[:, PRK + h:PRK + h + 1], in1=kp[:], op0=ALU.mult, op1=ALU.mult),
              ["pv3", "pc", "kp"], ["tmpBb"])
            pbn, pbnk = nL()
            o("pe", lambda e: e.matmul(pbn[0:64, 0:TG], ones64b[:], tmpBb[:], start=True, stop=True), ["ones64b", "tmpBb"], [pbnk])
            o("dve", lambda e: e.tensor_tensor(out=bon[js][:], in0=pbn[0:64, 0:TG], in1=vr, op=ALU.mult), [pbnk, f"vv{hp}"], [f"bon{js}"])

        def tile_precompute(h, j, hp):
            U = list(range(NTG))
            def K_(n, u):
                return f"{n}{u}"
            def cs(u):
                return slice(u * 128, (u + 1) * 128)
            def step_mm(outs, evac):
                for u in U:
                    pb_, pbk_ = nq6()
                    outs(u, pb_, pbk_)
                    evac(u, pb_, pbk_)
            step_mm(lambda u, q, qk: o("pe", lambda e: e.matmul(q[:, 0:128], r32(KR[hp][:, 0, cs(u)]), r32(btil[hp][:, cs(u)]), start=True, stop=True),
                                       [f"KR{hp}", f"btil{hp}"], [qk]),
                    lambda u, q, qk: o("dve", lambda e: e.tensor_tensor(out=Nm[u][:], in0=q[:, 0:128], in1=M1[:], op=ALU.mult), [qk, "M1"], [K_("Nm", u)]))
            def evB(u, q, qk):
                o("dve", lambda e: e.tensor_tensor(out=NTm[u][:], in0=q[:, 0:128], in1=MB[:, 0:128], op=ALU.mult), [qk, "MB"], [K_("NTm", u)])
                o("pool", lambda e: e.tensor_tensor(out=Rat[u][:], in0=Nm[u][:], in1=identf[:], op=ALU.add), [K_("Nm", u), "identf"], [K_("Rat", u)])
                o("dve", lambda e: e.tensor_tensor(out=AT[u][:, 0, :], in0=q[:, 128:256], in1=MB[:, 128:256], op=ALU.mult), [qk, "MB"], [K_("AT0", u)])
                o("pool", lambda e: e.tensor_tensor(out=Ra[u][:], in0=NTm[u][:], in1=identf[:], op=ALU.add), [K_("NTm", u), "identf"], [K_("Ra", u)])
            step_mm(lambda u, q, qk: o("pe", lambda e: e.matmul(q[:, 0:256], r32(btil[hp][:, cs(u)]), r32(KR[hp][:, :, cs(u)]), start=True, stop=True),
                                       [f"KR{hp}", f"btil{hp}"], [qk]), evB)
            step_mm(lambda u, q, qk: o("pe", lambda e: e.matmul(q[:, 0:256], r32(ktil[hp][:, cs(u)]), r32(KR[hp][:, :, cs(u)]), start=True, stop=True),
                                       [f"KR{hp}", f"ktil{hp}"], [qk]),
                    lambda u, q, qk: o("dve", lambda e: e.tensor_tensor(out=AT[u][:, 1:3, :], in0=q[:, 0:256].rearrange("p (a b) -> p a b", a=2),
                                                                        in1=MC[:].rearrange("p (a b) -> p a b", a=2), op=ALU.mult), [qk, "MC"], [K_("AT12", u)]))
            def trA(u, q, qk):
                o("pe", lambda e: e.transpose(out=qb_(q)[:, 0:64], in_=KR[hp][:, 0, cs(u)], identity=idb), [f"KR{hp}"], [qk])
                o("pe", lambda e: e.transpose(out=qb_(q)[:, 64:128], in_=vv[hp][:, cs(u)], identity=idb), [f"vv{hp}"], [qk])
            def evA(u, q, qk):
                o("act", lambda e: e.activation(out=KA[u][:, 0:64], in_=qb_(q)[:, 0:64], func=AF.Copy), [qk], [K_("KAa", u)])
                o("act", lambda e: e.activation(out=ZV[u][:, 64:128], in_=qb_(q)[:, 64:128], func=AF.Copy), [qk], [K_("ZV", u)])
            step_mm(trA, evA)
            def trB(u, q, qk):
                o("pe", lambda e: e.transpose(out=qb_(q)[:, 0:64], in_=BK[hp][:, 0, cs(u)], identity=idb), [f"BK{hp}"], [qk])
                o("pe", lambda e: e.transpose(out=qb_(q)[:, 64:128], in_=BK[hp][:, 1, cs(u)], identity=idb), [f"BK{hp}"], [qk])
                c3 = u * 128 + 96
                o("pe", lambda e: e.transpose(out=qb_(q)[0:32, 128:192], in_=BK[hp][:, 0, c3:c3 + 32], identity=idb), [f"BK{hp}"], [qk])
                o("pe", lambda e: e.transpose(out=qb_(q)[0:32, 192:256], in_=BK[hp][:, 1, c3:c3 + 32], identity=idb), [f"BK{hp}"], [qk])
            def evB2(u, q, qk):
                o("act", lambda e: e.activation(out=BKtok[u][:], in_=qb_(q)[:, 0:128], func=AF.Copy), [qk], [K_("BKtok", u)])
                o("act", lambda e: e.activation(out=X3[u][:, 0:128], in_=qb_(q)[0:32, 128:256], func=AF.Copy), [qk], [K_("X3a", u)])
            step_mm(trB, evB2)
            def trC(u, q, qk):
                c3 = u * 128 + 96
                o("pe", lambda e: e.transpose(out=qb_(q)[0:32, 0:64], in_=vv[hp][:, c3:c3 + 32], identity=idb), [f"vv{hp}"], [qk])
                o("pe", lambda e: e.matmul(q[:, 64:128], r32(AT[u][:, 1, :]), r32(ZV[u][:, 64:128]), start=True, stop=True), [K_("AT12", u), K_("ZV", u)], [qk])
            def evC(u, q, qk):
                o("act", lambda e: e.activation(out=ZV3[u][:, 64:128], in_=qb_(q)[0:32, 0:64], func=AF.Copy), [qk], [K_("X3b", u)])
                o("act", lambda e: e.activation(out=KA[u][:, 64:128], in_=q[:, 64:128], func=AF.Copy), [qk], [K_("KAb", u)])
            step_mm(trC, evC)
            def nm(dst, lhsT, rhs, add=None, eng="dve"):
                def mmf(u, q, qk):
                    o("pe", lambda e: e.matmul(q[:, 0:128], r32(lhsT[0][u][:]), r32(rhs[0][u][:]), start=True, stop=True), [K_(lhsT[1], u), K_(rhs[1], u)], [qk])
                def evf(u, q, qk):
                    if add is None:
                        if eng == "act":
                            o("act", lambda e: e.activation(out=dst[0][u][:], in_=q[:, 0:128], func=AF.Copy), [qk], [K_(dst[1], u)])
                        else:
                            o("dve", lambda e: e.tensor_copy(out=dst[0][u][:], in_=q[:, 0:128]), [qk], [K_(dst[1], u)])
                    else:
                        o("dve", lambda e: e.tensor_tensor(out=dst[0][u][:], in0=q[:, 0:128], in1=add[0][u][:], op=ALU.add), [qk, K_(add[1], u)], [K_(dst[1], u)])
                step_mm(mmf, evf)
            M_, Mt_ = (NTm, "NTm"), (Nm, "Nm")
            A_, At_ = (Ma, "Ma"), (Mat, "Mat")
            B_, Bt_ = (Mb, "Mb"), (Mbt, "Mbt")
            R_, Rt_ = (Ra, "Ra"), (Rat, "Rat")
            Q_, Qt_ = (Rb, "Rb"), (Rbt, "Rbt")
            nm(A_, Mt_, M_, eng="act"); nm(At_, M_, Mt_, eng="dve")
            nm(Q_, Rt_, A_, add=R_); nm(Qt_, A_, Rt_, add=Rt_)
            nm(B_, At_, A_, eng="act"); nm(Bt_, A_, At_, eng="dve")
            nm(R_, Qt_, B_, add=Q_); nm(Rt_, B_, Qt_, add=Qt_)
            nm(A_, Bt_, B_, eng="act"); nm(At_, B_, Bt_, eng="dve")
            nm(Q_, Rt_, A_, add=R_); nm(Qt_, A_, Rt_, add=Rt_)
            nm(B_, At_, A_, eng="act")
            nm(R_, Qt_, B_, add=Q_)
            invT = Ra
            def mmW(u, q, qk):
                o("pe", lambda e: e.matmul(q[:, 0:128], r32(invT[u][:]), r32(KA[u][:]), start=True, stop=True), [K_("Ra", u), K_("KAa", u), K_("KAb", u)], [qk])
                o("pe", lambda e: e.matmul(q[0:32, 128:256], invT[u][:, 96:128], KA[u][:], start=True, stop=True), [K_("Ra", u), K_("KAa", u), K_("KAb", u)], [qk])
            def evW(u, q, qk):
                o("act", lambda e: e.activation(out=nWX[u][:], in_=q[:, 0:128], func=AF.Copy, scale=-1.0), [qk], [K_("nWX", u)])
                o("act", lambda e: e.activation(out=nWX3[u][:], in_=q[0:32, 128:256], func=AF.Copy, scale=-1.0), [qk], [K_("nWX3", u)])
            step_mm(mmW, evW)
            def mmR(u, q, qk):
                o("pe", lambda e: e.matmul(q[:, 0:128], r32(ZV[u][:]), r32(AT[u][:, 2, :]), start=True, stop=False), [K_("ZV", u), K_("AT12", u)], [qk])
                o("pe", lambda e: e.matmul(q[:, 0:128], r32(nWX[u][:]), r32(AT[u][:, 0, :]), start=False, stop=True), [K_("nWX", u), K_("AT0", u)], [qk])
            def evR(u, q, qk):
                o("dve", lambda e: e.tensor_tensor(out=RYT[j][u][0:64, :], in0=q[0:64, 0:128], in1=KR[hp][:, 1, cs(u)], op=ALU.add), [qk, f"KR{hp}"], [f"RYT{j}_{u}"])
                o("act", lambda e: e.activation(out=RYT[j][u][64:128, :], in_=q[64:128, 0:128], func=AF.Copy), [qk], [f"RYT{j}_{u}"])
            step_mm(mmR, evR)
            for sc in range(4):
                def mmP(u, q, qk, sc=sc):
                    if sc < 3:
                        prt = slice(sc * SC, (sc + 1) * SC)
                        o("pe", lambda e: e.matmul(q[:, 0:64], nWX[u][prt, :], BKtok[u][prt, 0:64], start=True, stop=False), [K_("nWX", u), K_("BKtok", u)], [qk])
                        o("pe", lambda e: e.matmul(q[:, 0:64], ZV[u][prt, :], BKtok[u][prt, 64:128], start=False, stop=True), [K_("ZV", u), K_("BKtok", u)], [qk])
                    else:
                        o("pe", lambda e: e.matmul(q[:, 0:64], nWX3[u][:], X3[u][:, 0:64], start=True, stop=False), [K_("nWX3", u), K_("X3a", u)], [qk])
                        o("pe", lambda e: e.matmul(q[:, 0:64], ZV3[u][:], X3[u][:, 64:128], start=False, stop=True), [K_("X3a", u), K_("X3b", u)], [qk])
                def evP(u, q, qk, sc=sc):
                    gcol = u * 4 + sc
                    o("dve", lambda e: e.scalar_tensor_tensor(out=PHI[j][u][0:64, sc, :], in0=identf[0:64, 0:64], scalar=GC[hp][:, gcol:gcol + 1], in1=q[0:64, 0:64],
                                                              op0=ALU.mult, op1=ALU.add), ["identf", f"GC{hp}", qk], [f"PHI{j}_{u}"])
                    o("act", lambda e: e.activation(out=PHI[j][u][64:128, sc, :], in_=q[64:128, 0:64], func=AF.Copy), [qk], [f"PHI{j}_{u}"])
                step_mm(mmP, evP)

        def chains(heads):
            for tt in range(NTG):
                for sc in range(4):
                    for j, h in enumerate(heads):
                        TI = TIall[h]
                        cur = ti_cur[h]; nxt = 1 - cur
                        gcol = tt * 128 + sc * SC
                        prt = slice(sc * SC, (sc + 1) * SC)
                        r0 = (j % 2) * 64
                        o("pe", lambda e, TI=TI, cur=cur, j=j, tt=tt, sc=sc, r0=r0: e.matmul(pS2[j // 2][r0:r0 + 64, 0:64], PHI[j][tt][:, sc, :], TI[cur][:], start=True, stop=True),
                          [f"PHI{j}_{tt}", f"TI{h}_{cur}_"], [f"pS{j}"])
                        o("pe", lambda e, TI=TI, cur=cur, j=j, tt=tt, prt=prt, gcol=gcol, r0=r0: e.matmul(py2[j // 2][r0:r0 + 64, gcol:gcol + SC], TI[cur][:], RYT[j][tt][:, prt], start=True, stop=True),
                          [f"RYT{j}_{tt}", f"TI{h}_{cur}_"], [f"py{j}"])
                        o("act", lambda e, TI=TI, nxt=nxt, j=j, r0=r0: e.activation(out=TI[nxt][0:64, :], in_=pS2[j // 2][r0:r0 + 64, 0:64], func=AF.Copy), [f"pS{j}"], [f"TI{h}_{nxt}_"])
                        ti_cur[h] = nxt

        ysbs = [sb(f"ysb{j}", [64, TG]) for j in range(HI)]; ysbb = [sb(f"ysbb{j}", [64, TG], BF16) for j in range(HI)]
        ycs = [sb(f"yc{j}", [64, TG]) for j in range(HI)]; tcs = [sb(f"tmpC{j}", [64, TG]) for j in range(HI)]
        tcb = [sb(f"tmpCb{j}", [64, TG], BF16) for j in range(HI)]; yos = [sb(f"yo{j}", [64, TG], BF16) for j in range(HI)]
        def post_block(heads, t0, bp):
            J = list(enumerate(heads))
            pas = {}; pbs = {}
            for j, h in J:
                r0 = (j % 2) * 64
                o("act", lambda e, j=j, r0=r0: e.activation(out=ysbs[j][:], in_=py2[j // 2][r0:r0 + 64, 0:TG], func=AF.Copy), [f"py{j}"], [f"ysb{j}"])
                o("act", lambda e, j=j, r0=r0: e.activation(out=ysbb[j][:], in_=py2[j // 2][r0:r0 + 64, 0:TG], func=AF.Copy), [f"py{j}"], [f"ysbb{j}"])
            for j, h in J:
                pas[j] = nq()
                o("pe", lambda e, j=j: e.matmul(pas[j][0][0:64, 0:TG], ones64b[:], ysbb[j][:], start=True, stop=True), ["ones64b", f"ysbb{j}"], [pas[j][1]])
                o("dve", lambda e, j=j: e.scalar_tensor_tensor(out=ycs[j][:], in0=pas[j][0][0:64, 0:TG], scalar=-1.0 / 64, in1=ysbs[j][:], op0=ALU.mult, op1=ALU.add),
                  [pas[j][1], f"ysb{j}"], [f"yc{j}"])
                o("act", lambda e, j=j: e.activation(out=tcb[j][:], in_=ycs[j][:], func=AF.Square), [f"yc{j}"], [f"tmpCb{j}"])
            for j, h in J:
                pbs[j] = nq()
                o("pe", lambda e, j=j: e.matmul(pbs[j][0][0:64, 0:TG], ones64b[:], tcb[j][:], start=True, stop=True), ["ones64b", f"tmpCb{j}"], [pbs[j][1]])
                o("act", lambda e, j=j: e.activation(out=tcs[j][:], in_=pbs[j][0][0:64, 0:TG], func=AF.Sqrt, scale=1.0 / 64, bias=c.gneps_t[0:64, :]),
                  [pbs[j][1], "gneps"], [f"tmpC{j}"])
            for j, h in J:
                o("dve", lambda e, j=j: e.reciprocal(out=tcs[j][:], in_=tcs[j][:]), [f"tmpC{j}"], [f"tmpC{j}"])
            for j, h in J:
                o("dve", lambda e, j=j: e.tensor_tensor(out=ycs[j][:], in0=ycs[j][:], in1=tcs[j][:], op=ALU.mult), [f"yc{j}", f"tmpC{j}"], [f"yc{j}"])
            for j, h in J:
                o("dve", lambda e, j=j, h=h: e.tensor_scalar(out=ycs[j][:], in0=ycs[j][:], scalar1=pc[:, PLW + h:PLW + h + 1], scalar2=pc[:, PLB + h:PLB + h + 1],
                                                          op0=ALU.mult, op1=ALU.add), [f"yc{j}", "pc"], [f"yc{j}"])
            for j, h in J:
                js = bp * HI + j
                o("pool", lambda e, j=j, js=js: e.tensor_tensor(out=ycs[j][:], in0=ycs[j][:], in1=bon[js][:], op=ALU.add), [f"yc{j}", f"bon{js}"], [f"yc{j}"])
            for j, h in J:
                js = bp * HI + j
                o("pool", lambda e, j=j, js=js: e.tensor_tensor(out=yos[j][:], in0=ycs[j][:], in1=gv[js][:], op=ALU.mult), [f"yc{j}", f"gv{js}"], [f"yo{j}"])
                d("sp", lambda e, j=j, h=h: e.dma_start(out=c.YB[h * 64:(h + 1) * 64, t0:t0 + TG], in_=yos[j][:]), reads=[f"s3_yo{j}"], writes=[])

        nhead = [0]
        nblk = [0]
        def prep_stream(h, j, G, t0, js):
            hp = nhead[0] % 2; nhead[0] += 1
            def f():
                if G == 0:
                    TI = TIall[h]
                    o("dve", lambda e, TI=TI: e.tensor_copy(out=TI[0][:], in_=TI0[:]), ["TI0"], [f"TI{h}_0_"])
                    o("dve", lambda e, TI=TI: e.tensor_copy(out=TI[1][:], in_=TI0[:]), ["TI0"], [f"TI{h}_1_"])
                    ti_cur[h] = 0
                head_prep(h, j, hp, G, t0, js)
            return f, hp
        for b in range(c.NB):
            for G in range(NG):
                t0 = b * S + G * TG
                lora_prep(b, G, t0)
                NBLK = NH // HI
                pend = None
                for hb in range(NBLK):
                    heads = [hb * HI + j for j in range(HI)]
                    bp = nblk[0] % 2; nblk[0] += 1
                    if pend is None:
                        f0, hp0 = prep_stream(heads[0], 0, G, t0, bp * HI)
                        f0()
                    else:
                        hp0 = pend
                    hps = [hp0]
                    for j, h in enumerate(heads):
                        if j + 1 < HI:
                            fb, hpn = prep_stream(heads[j + 1], j + 1, G, t0, bp * HI + j + 1)
                            hps.append(hpn)
                        else:
                            fb = None
                        merged(lambda h=h, j=j: tile_precompute(h, j, hps[j]), fb)
                    def tail(heads=heads, bp=bp):
                        chains(heads)
                        post_block(heads, t0, bp)
                    if hb + 1 < NBLK:
                        fn, pend = prep_stream((hb + 1) * HI, 0, G, t0, (1 - bp) * HI)
                        merged(tail, fn)
                    else:
                        pend = None
                        tail()


def stage4(p, nc, c):
    T = c.T
    TG = min(512, T)
    NG = T // TG
    from contextlib import ExitStack
    with ExitStack() as es:
        def sb(name, shape, dt=F32):
            return es.enter_context(nc.sbuf_tensor("s4_" + name, shape, dt))
        def ps(name, shape, dt=F32):
            return es.enter_context(nc.psum_tensor("s4_" + name, shape, dt))
        def o(eng, fn, reads, writes):
            p.op(eng, fn, reads=["s4_" + r for r in reads], writes=["s4_" + w for w in writes])
        woa = sb("woa", [128, 4, D], BF16); wob = sb("wob", [128, 4, D], BF16); wo = sb("wo", [128, KC, D], BF16)
        ya = sb("ya", [128, 4, TG], BF16); yb = sb("yb", [128, 4, TG], BF16); gt = sb("gt", [128, 16, TG], BF16)
        mg = sb("mg", [128, KC, TG], BF16)
        t1 = [sb(f"t1{i}", [128, TG]) for i in range(2)]
        t2 = [sb(f"t2{i}", [128, TG]) for i in range(2)]
        xt = [sb(f"xt{i}", [128, D]) for i in range(2)]
        x2 = [sb(f"x2{i}", [128, D]) for i in range(2)]
        PA = [ps(f"PA{i}", [128, 512]) for i in range(2)]
        PB = [ps(f"PB{i}", [128, 512]) for i in range(2)]
        PX = [ps(f"PX{i}", [128, 512]) for i in range(2)]
        p.dma("pool", lambda e: e.dma_start(out=woa[:], in_=c.w_out_a.rearrange("(kc p) n -> p kc n", p=128)), writes=["s4_woa"])
        p.dma("pool", lambda e: e.dma_start(out=wob[:], in_=c.w_out_b.rearrange("(kc p) n -> p kc n", p=128)), writes=["s4_wob"])
        p.dma("pool", lambda e: e.dma_start(out=wo[:], in_=c.w_out.rearrange("(kc p) n -> p kc n", p=128)), writes=["s4_wo"])
        n1 = 0; n2 = 0
        for g in range(NG):
            ts = slice(g * TG, (g + 1) * TG)
            p.dma("sp", lambda e, ts=ts: e.dma_start(out=ya[:], in_=c.YA.rearrange("(kc p) t -> p kc t", p=128)[:, :, ts]), reads=[], writes=["s4_ya"])
            p.dma("sp", lambda e, ts=ts: e.dma_start(out=yb[:], in_=c.YB.rearrange("(kc p) t -> p kc t", p=128)[:, :, ts]), reads=[], writes=["s4_yb"])
            p.dma("sp", lambda e, ts=ts: e.dma_start(out=gt[:], in_=c.GT.rearrange("(kc p) t -> p kc t", p=128)[:, :, ts]), reads=[], writes=["s4_gt"])
            for m in range(KC):
                u = n1 % 2; n1 += 1
                ms = slice(m * 128, (m + 1) * 128)
                for kc in range(4):
                    o("pe", lambda e, u=u, kc=kc, ms=ms: e.matmul(PA[u][:, 0:TG], woa[:, kc, ms], ya[:, kc, :], start=(kc == 0), stop=(kc == 3)),
                      ["woa", "ya"], [f"PA{u}"])
                for kc in range(4):
                    o("pe", lambda e, u=u, kc=kc, ms=ms: e.matmul(PB[u][:, 0:TG], wob[:, kc, ms], yb[:, kc, :], start=(kc == 0), stop=(kc == 3)),
                      ["wob", "yb"], [f"PB{u}"])
                o("dve", lambda e, u=u, m=m: e.tensor_tensor(out=t1[u][:], in0=PA[u][:, 0:TG], in1=gt[:, m, :], op=ALU.mult), [f"PA{u}", "gt"], [f"t1{u}"])
                o("dve", lambda e, u=u, m=m: e.tensor_tensor(out=t2[u][:], in0=PB[u][:, 0:TG], in1=gt[:, 8 + m, :], op=ALU.mult), [f"PB{u}", "gt"], [f"t2{u}"])
                o("pool", lambda e, u=u, m=m: e.tensor_tensor(out=mg[:, m, :], in0=t1[u][:], in1=t2[u][:], op=ALU.add), [f"t1{u}", f"t2{u}"], ["mg"])
            for tt in range(TG // 128):
                i = g * (TG // 128) + tt
                b = i % 2
                p.dma("sp", lambda e, i=i, b=b: e.dma_start(out=xt[b][:], in_=c.x[i * 128:(i + 1) * 128, :]), writes=[f"s4_xt{b}"])
                for n in range(2):
                    u = n2 % 2; n2 += 1
                    for m in range(KC):
                        o("pe", lambda e, u=u, m=m, tt=tt, n=n: e.matmul(PX[u][:], mg[:, m, tt * 128:(tt + 1) * 128], wo[:, m, n * 512:(n + 1) * 512],
                                                                          start=(m == 0), stop=(m == KC - 1)), ["mg", "wo"], [f"PX{u}"])
                    o("dve", lambda e, u=u, b=b, n=n: e.tensor_tensor(out=x2[b][:, n * 512:(n + 1) * 512], in0=PX[u][:], in1=xt[b][:, n * 512:(n + 1) * 512], op=ALU.add),
                      [f"PX{u}", f"xt{b}"], [f"x2{b}"])
                p.dma("sp", lambda e, i=i, b=b: e.dma_start(out=c.X2[i * 128:(i + 1) * 128, :], in_=x2[b][:]), reads=[f"s4_x2{b}"], writes=[])


def _bc_reg(c, e, val):
    if getattr(c, "_bc", None) is None:
        c._bc = e.to_reg(val)
    return c._bc


def stage5a(p, nc, c, es):
    T = c.T; NT = T // 128; C = c.CAP; NE = 32
    BIG = 1.0e6
    def sb(name, shape, dt=F32):
        return es.enter_context(nc.sbuf_tensor("s5_" + name, shape, dt))
    def o(eng, fn, reads, writes):
        p.op(eng, fn, reads=["s5_" + r for r in reads], writes=["s5_" + w for w in writes])
    c.slot4i = sb("slot4i", [128, NT, 4], I32)
    c.gate4 = sb("gate4", [128, NT, 4])
    from contextlib import ExitStack
    with ExitStack() as es2:
        def sb2(name, shape, dt=F32):
            return es2.enter_context(nc.sbuf_tensor("s5_" + name, shape, dt))
        def ps(name, shape, dt=F32):
            return es2.enter_context(nc.psum_tensor("s5_" + name, shape, dt))
        gB = sb2("gB", [128, D]); rw = sb2("rw", [128, KC, NE]); rb = sb2("rb", [1, NE]); ones_row = sb2("ones_row", [1, 128])
        SUt = sb2("SUt", [128, 128]); onesf = sb2("onesf", [128, 128]); identf = sb2("identf", [128, 128]); eC = sb2("eC", [128, NE])
        msum = sb2("msum", [128, NE])
        xt = [sb2(f"xt{i}", [128, D]) for i in range(2)]
        hn = sb2("hn", [128, D]); hnb = [sb2(f"hnb{i}", [128, D], BF16) for i in range(2)]
        junk = sb2("junk", [128, D], BF16); ss = sb2("ss", [128, 1]); rstd = sb2("rstd", [128, 1])
        hnT = sb2("hnT", [128, KC, 128])
        lg = sb2("lg", [128, NE]); top8 = sb2("top8", [128, 8]); mask = sb2("mask", [128, NE]); nm1 = sb2("nm1", [128, 1])
        ex = sb2("ex", [128, NE]); den = sb2("den", [128, 1]); gw = sb2("gw", [128, NE]); slotf = sb2("slotf", [128, NE])
        valid = sb2("valid", [128, NE]); big = sb2("big", [128, NE]); oh = sb2("oh", [128, NE]); j32 = sb2("j32", [128, NE])
        slot4f = sb2("slot4f", [128, 4])
        pT = ps("pT", [128, D]); PL = ps("PL", [128, NE]); PP = ps("PP", [128, NE])
        p.dma("sp", lambda e: e.dma_start(out=gB[:], in_=c.ffn_norm_g.partition_broadcast(128)), writes=["gB_s5"])
        p.dma("sp", lambda e: e.dma_start(out=rw[:], in_=c.router_w.rearrange("(kc p) e -> p kc e", p=128)), writes=["s5_rw"])
        p.dma("sp", lambda e: e.dma_start(out=rb[:], in_=c.router_b), writes=["s5_rb"])
        p.dma("sp", lambda e: e.dma_start(out=SUt[:], in_=c.SUt_d), writes=["s5_SUt"])
        p.dma("sp", lambda e: e.dma_start(out=identf[:], in_=c.identf_d), writes=["s5_identf"])
        p.dma("sp", lambda e: e.dma_start(out=eC[:], in_=c.eC_d), writes=["s5_eC"])
        o("pool", lambda e: e.memset(ones_row[:], 1.0), [], ["ones_row"])
        o("pool", lambda e: e.memset(onesf[:], 1.0), [], ["onesf"])
        o("pool", lambda e: e.memset(msum[:], 0.0), [], ["msum"])
        for i in range(NT):
            b = i % 2
            p.dma("sp", lambda e, i=i, b=b: e.dma_start(out=xt[b][:], in_=c.X2[i * 128:(i + 1) * 128, :]), reads=[], writes=[f"s5_xt{b}"])
            rmsnorm_tile(p, c, xt[b][:], f"s5_xt{b}", gB, hn[:], "s5_hn", "s5", junk, ss, rstd)
            o("act", lambda e, b=b: e.activation(out=hnb[b][:], in_=hn[:], func=AF.Copy), ["hn"], [f"hnb{b}"])
            for kc in range(KC):
                o("pe", lambda e, kc=kc: e.transpose(out=pT[:, kc * 128:(kc + 1) * 128], in_=hn[:, kc * 128:(kc + 1) * 128], identity=identf[:]),
                  ["hn", "identf"], ["pT"])
            o("act", lambda e: e.activation(out=hnT[:, 0:4, :], in_=pT[:, 0:512].rearrange("p (k t) -> p k t", k=4), func=AF.Copy), ["pT"], ["hnTa"])
            o("dve", lambda e: e.tensor_copy(out=hnT[:, 4:8, :], in_=pT[:, 512:1024].rearrange("p (k t) -> p k t", k=4)), ["pT"], ["hnTb"])
            for kc in range(KC):
                o("pe", lambda e, kc=kc: e.matmul(PL[:], hnT[:, kc, :], rw[:, kc, :], start=(kc == 0), stop=False), ["hnTa", "hnTb", "rw"], ["PL"])
            o("pe", lambda e: e.matmul(PL[:], ones_row[:], rb[:], start=False, stop=True), ["ones_row", "rb"], ["PL"])
            o("dve", lambda e: e.tensor_copy(out=lg[:], in_=PL[:]), ["PL"], ["lg"])
            o("dve", lambda e: e.max(out=top8[:], in_=lg[:]), ["lg"], ["top8"])
            o("dve", lambda e: e.tensor_scalar(out=mask[:], in0=lg[:], scalar1=top8[:, 3:4], scalar2=None, op0=ALU.is_ge), ["lg", "top8"], ["mask"])
            o("dve", lambda e: e.tensor_scalar(out=nm1[:], in0=top8[:, 0:1], scalar1=-1.0, scalar2=None, op0=ALU.mult), ["top8"], ["nm1"])
            o("act", lambda e: e.activation(out=ex[:], in_=lg[:], func=AF.Exp, bias=nm1[:]), ["lg", "nm1"], ["ex"])
            o("dve", lambda e: e.scalar_tensor_tensor(out=ex[:], in0=ex[:], scalar=1.0, in1=mask[:], op0=ALU.mult, op1=ALU.mult, accum_out=den[:]),
              ["ex", "mask"], ["ex", "den"])
            o("dve", lambda e: e.reciprocal(out=den[:], in_=den[:]), ["den"], ["den"])
            o("dve", lambda e: e.tensor_scalar(out=gw[:], in0=ex[:], scalar1=den[:], scalar2=None, op0=ALU.mult), ["ex", "den"], ["gw"])
            o("pe", lambda e: e.matmul(PP[:], SUt[:], mask[:], start=True, stop=False), ["SUt", "mask"], ["PP"])
            o("pe", lambda e: e.matmul(PP[:], onesf[:], msum[:], start=False, stop=True), ["onesf", "msum"], ["PP"])
            o("dve", lambda e: e.tensor_tensor(out=slotf[:], in0=PP[:], in1=eC[:], op=ALU.add), ["PP", "eC"], ["slotf"])
            o("dve", lambda e: e.tensor_scalar(out=valid[:], in0=PP[:], scalar1=float(C), scalar2=None, op0=ALU.is_lt), ["PP"], ["valid"])
            o("pool", lambda e: e.tensor_tensor(out=msum[:], in0=msum[:], in1=mask[:], op=ALU.add), ["msum", "mask"], ["msum"])
            o("dve", lambda e: e.tensor_tensor(out=valid[:], in0=valid[:], in1=mask[:], op=ALU.mult), ["valid", "mask"], ["valid"])
            o("dve", lambda e: e.tensor_scalar(out=big[:], in0=valid[:], scalar1=-BIG, scalar2=BIG, op0=ALU.mult, op1=ALU.add), ["valid"], ["big"])
            o("dve", lambda e: e.tensor_tensor(out=slotf[:], in0=slotf[:], in1=valid[:], op=ALU.mult), ["slotf", "valid"], ["slotf"])
            o("dve", lambda e: e.tensor_tensor(out=slotf[:], in0=slotf[:], in1=big[:], op=ALU.add), ["slotf", "big"], ["slotf"])
            for k in range(4):
                o("dve", lambda e, k=k: e.tensor_scalar(out=oh[:], in0=lg[:], scalar1=top8[:, k:k + 1], scalar2=None, op0=ALU.is_equal), ["lg", "top8"], ["oh"])
                o("dve", lambda e, k=k: e.scalar_tensor_tensor(out=j32[:], in0=oh[:], scalar=1.0, in1=slotf[:], op0=ALU.mult, op1=ALU.mult,
                                                               accum_out=slot4f[:, k:k + 1]), ["oh", "slotf"], ["j32", "slot4f"])
                o("dve", lambda e, k=k, i=i: e.scalar_tensor_tensor(out=j32[:], in0=oh[:], scalar=1.0, in1=gw[:], op0=ALU.mult, op1=ALU.mult,
                                                                    accum_out=c.gate4[:, i, k:k + 1]), ["oh", "gw"], ["j32", "gate4"])
            o("dve", lambda e, i=i: e.tensor_copy(out=c.slot4i[:, i, :], in_=slot4f[:]), ["slot4f"], ["slot4i"])
            for k in range(4):
                p.dma("pool", lambda e, i=i, k=k, b=b: e.indirect_dma_start(
                    out=c.XS[:, :], out_offset=bass.IndirectOffsetOnAxis(ap=c.slot4i[:, i, k:k + 1], axis=0),
                    in_=hnb[b][:], in_offset=None, bounds_check=_bc_reg(c, e, NE * C - 1), oob_is_err=False),
                    reads=[f"s5_hnb{b}", "s5_slot4i"], writes=[], grp="i")


def stage5_dbg(p, nc, c):
    NT = c.T // 128
    p.dma("sp", lambda e: e.dma_start(out=c.dbg_slot, in_=c.slot4i[:].rearrange("p a b -> p (a b)")), reads=["s5_slot4i"], writes=["dbg_slot"])
    p.dma("sp", lambda e: e.dma_start(out=c.dbg_gate, in_=c.gate4[:].rearrange("p a b -> p (a b)")), reads=["s5_gate4"], writes=["dbg_gate"])


def stage5b(p, nc, c):
    T = c.T; NT = T // 128; C = c.CAP; NE = 32
    NST = C // 128
    SG = C // 2 if C > 512 else C
    NSG = C // SG
    from contextlib import ExitStack
    with ExitStack() as es:
        def sb(name, shape, dt=F32):
            return es.enter_context(nc.sbuf_tensor("s5b_" + name, shape, dt))
        def ps(name, shape, dt=F32):
            return es.enter_context(nc.psum_tensor("s5b_" + name, shape, dt))
        def o(eng, fn, reads, writes):
            p.op(eng, fn, reads=["s5b_" + r for r in reads], writes=["s5b_" + w for w in writes])
        w1b = [sb(f"w1b{i}", [128, KC, 2048], BF16) for i in range(2)]
        w2b = [sb(f"w2b{i}", [128, KC, D], BF16) for i in range(2)]
        b1t = sb("b1t", [128, NE * 16])
        b2c = [sb(f"b2c{i}", [128, D]) for i in range(2)]
        xg = [sb(f"xg{i}", [128, D], BF16) for i in range(2)]
        xTs = [sb(f"xT{i}", [128, KC, C], BF16) for i in range(2)]
        actT = sb("actT", [128, KC, C], BF16)
        gl = [sb(f"gl{i}", [128, SG]) for i in range(2)]
        sg_ = [sb(f"sg{i}", [128, SG]) for i in range(2)]
        ul = [sb(f"ul{i}", [128, SG]) for i in range(2)]
        ysb = [sb(f"ysb{i}", [128, D], BF16) for i in range(2)]
        pT = [ps(f"pT{i}", [128, D], BF16) for i in range(2)]
        pg = [ps(f"pg{i}", [128, 512]) for i in range(2)]
        pu = [ps(f"pu{i}", [128, 512]) for i in range(2)]
        pyy = [ps(f"py{i}", [128, 512]) for i in range(2)]
        p.dma("sp", lambda e: e.dma_start(out=b1t[:], in_=c.b1_l), writes=["s5b_b1t"])
        xs_keys = [f"XS{i}_{k}" for i in range(NT) for k in range(4)]
        nx = 0; ng = 0; ny = 0
        nxc = [0]
        def load_x(ex_):
            xT = xTs[ex_ % 2]; xk = f"s5b_xT{ex_ % 2}"
            for st in range(NST):
                u = nxc[0] % 2; nxc[0] += 1
                r0 = ex_ * C + st * 128
                p.dma("sp", lambda e, u=u, r0=r0: e.dma_start(out=xg[u][:], in_=c.XS[r0:r0 + 128, :]), reads=[], writes=[f"s5b_xg{u}"])
                for kc in range(KC):
                    o("pe", lambda e, u=u, kc=kc: e.transpose(out=pT[u][:, kc * 128:(kc + 1) * 128], in_=xg[u][:, kc * 128:(kc + 1) * 128], identity=c.ident_b[:]),
                      [f"xg{u}"], [f"pT{u}"])
                p.op("act" if st % 2 == 0 else "dve",
                     (lambda e, u=u, st=st: e.activation(out=xT[:, :, st * 128:(st + 1) * 128], in_=pT[u][:].rearrange("p (k t) -> p k t", k=KC), func=AF.Copy))
                     if st % 2 == 0 else
                     (lambda e, u=u, st=st: e.tensor_copy(out=xT[:, :, st * 128:(st + 1) * 128], in_=pT[u][:].rearrange("p (k t) -> p k t", k=KC))),
                     reads=[f"s5b_pT{u}"], writes=[xk])
        NSTG = 6
        stg = [sb(f"stg{i}", [128, 1024]) for i in range(NSTG)]
        nstg = [0]
        def chunk_list(ex_):
            wb = ex_ % 2
            out = []
            for kc in range(KC):
                for hf in range(2):
                    src = c.exp_w1[ex_][kc * 128:(kc + 1) * 128, hf * 1024:(hf + 1) * 1024]
                    dst = w1b[wb][:, kc, hf * 1024:(hf + 1) * 1024]
                    out.append((src, dst, f"s5b_w1b{wb}_{kc}"))
            for kc in range(KC):
                out.append((c.exp_w2[ex_][kc * 128:(kc + 1) * 128, :], w2b[wb][:, kc, :], f"s5b_w2b{wb}_{kc}"))
            items = []
            for (src, dst, key) in out:
                k = nstg[0] % NSTG; nstg[0] += 1
                def dma_t(src=src, k=k):
                    p.dma("sp", lambda e: e.dma_start(out=stg[k][:], in_=src), writes=[f"s5b_stg{k}"])
                def cast_t(dst=dst, k=k, key=key):
                    p.op("act", lambda e: e.activation(out=dst, in_=stg[k][:], func=AF.Copy), reads=[f"s5b_stg{k}"], writes=[key])
                items.append((dma_t, cast_t))
            return items
        def load_b2(ex_):
            wb = ex_ % 2
            p.dma("sp", lambda e, ex_=ex_, wb=wb: e.dma_start(out=b2c[wb][:], in_=c.exp_b2[ex_].partition_broadcast(128)), writes=[f"s5b_b2c{wb}"])
        for (dt_, ct_) in chunk_list(0):
            dt_(); ct_()
        load_b2(0)
        load_x(0)
        nslots = KC * NSG + NST * 2
        for ex_ in range(NE):
            wb = ex_ % 2
            pf = chunk_list(ex_ + 1) if ex_ + 1 < NE else []
            pfs = {"d": 0, "c": 0}
            per = -(-len(pf) // nslots) if pf else 0
            def pf_dma(n):
                for _ in range(n):
                    if pfs["d"] < len(pf):
                        pf[pfs["d"]][0](); pfs["d"] += 1
            def pf_slot():
                for _ in range(per):
                    if pfs["c"] < len(pf):
                        pf[pfs["c"]][1](); pfs["c"] += 1
                        pf_dma(1)
            if ex_ + 1 < NE:
                load_b2(ex_ + 1)
            pf_dma(NSTG)
            xT = xTs[ex_ % 2]; xk = f"xT{ex_ % 2}"
            w1v = w1b[wb][:].rearrange("p k (f two) -> p k f two", two=2)
            for fc in range(KC):
                for sgi in range(NSG):
                    u = ng % 2; ng += 1
                    ss_ = slice(sgi * SG, (sgi + 1) * SG)
                    for kc in range(KC):
                        o("pe", lambda e, u=u, kc=kc, fc=fc, ss_=ss_, w1v=w1v, xT=xT: e.matmul(pg[u][:, 0:SG], w1v[:, kc, fc * 128:(fc + 1) * 128, 0], xT[:, kc, ss_],
                                                                                    start=(kc == 0), stop=(kc == KC - 1)), [f"w1b{wb}_{kc}", xk], [f"pg{u}"])
                    for kc in range(KC):
                        o("pe", lambda e, u=u, kc=kc, fc=fc, ss_=ss_, w1v=w1v, xT=xT: e.matmul(pu[u][:, 0:SG], w1v[:, kc, fc * 128:(fc + 1) * 128, 1], xT[:, kc, ss_],
                                                                                    start=(kc == 0), stop=(kc == KC - 1)), [f"w1b{wb}_{kc}", xk], [f"pu{u}"])
                    bcol = ex_ * 16 + fc * 2
                    o("act", lambda e, u=u, bcol=bcol: e.activation(out=sg_[u][:], in_=pg[u][:, 0:SG], func=AF.Gelu_apprx_sigmoid, bias=b1t[:, bcol:bcol + 1]),
                      [f"pg{u}", "b1t"], [f"sg{u}"])
                    o("dve", lambda e, u=u, bcol=bcol: e.tensor_scalar(out=ul[u][:], in0=pu[u][:, 0:SG], scalar1=b1t[:, bcol + 1:bcol + 2], scalar2=7.0,
                                                                       op0=ALU.add, op1=ALU.min), [f"pu{u}", "b1t"], [f"ul{u}"])
                    o("dve", lambda e, u=u: e.tensor_scalar(out=ul[u][:], in0=ul[u][:], scalar1=-7.0, scalar2=1.0, op0=ALU.max, op1=ALU.add), [f"ul{u}"], [f"ul{u}"])
                    o("dve", lambda e, u=u, fc=fc, ss_=ss_: e.scalar_tensor_tensor(out=actT[:, fc, ss_], in0=sg_[u][:], scalar=6.999953128303318, in1=ul[u][:],
                                                                                  op0=ALU.min, op1=ALU.mult),
                      [f"ul{u}", f"sg{u}"], ["actT"])
                    pf_slot()
            if ex_ + 1 < NE:
                load_x(ex_ + 1)
            for st in range(NST):
                yb_ = ny % 2
                for n in range(2):
                    u = ny % 2; ny += 1
                    for fc in range(KC):
                        o("pe", lambda e, u=u, fc=fc, st=st, n=n, wb=wb: e.matmul(pyy[u][:], actT[:, fc, st * 128:(st + 1) * 128], w2b[wb][:, fc, n * 512:(n + 1) * 512],
                                                                           start=(fc == 0), stop=(fc == KC - 1)), ["actT", f"w2b{wb}_{fc}"], [f"py{u}"])
                    o("dve", lambda e, u=u, yb_=yb_, n=n, wb=wb: e.tensor_tensor(out=ysb[yb_][:, n * 512:(n + 1) * 512], in0=pyy[u][:], in1=b2c[wb][:, n * 512:(n + 1) * 512], op=ALU.add),
                      [f"py{u}", f"b2c{wb}"], [f"ysb{yb_}"])
                    pf_slot()
                r0 = ex_ * C + st * 128
                p.dma("sp", lambda e, yb_=yb_, r0=r0: e.dma_start(out=c.YS[r0:r0 + 128, :], in_=ysb[yb_][:]), reads=[f"s5b_ysb{yb_}"], writes=[])
            while pfs["c"] < len(pf):
                pf[pfs["c"]][1](); pfs["c"] += 1
                pf_dma(1)


def stage6(p, nc, c):
    T = c.T; NT = T // 128; C = c.CAP; NE = 32
    NST = C // 128
    from contextlib import ExitStack
    with ExitStack() as es:
        def sb(name, shape, dt=F32):
            return es.enter_context(nc.sbuf_tensor("s6_" + name, shape, dt))
        def ps(name, shape, dt=F32):
            return es.enter_context(nc.psum_tensor("s6_" + name, shape, dt))
        def o(eng, fn, reads, writes):
            p.op(eng, fn, reads=[(r[1:] if r.startswith("@") else "s6_" + r) for r in reads], writes=["s6_" + w for w in writes])
        gBp = sb("gBp", [128, D]); gBf = sb("gBf", [128, D])
        wg = sb("wg", [128, KC, D], BF16); wp = sb("wp", [128, 2, D], BF16)
        xt = [sb(f"xt{i}", [128, D]) for i in range(2)]
        yg = [[sb(f"yg{i}_{k}", [128, D], BF16) for k in range(4)] for i in range(2)]
        x3 = sb("x3", [128, D]); hp = sb("hp", [128, D], BF16); hpT = sb("hpT", [128, KC, 128], BF16)
        pt = [sb(f"pt{i}", [128, 256]) for i in range(2)]
        ptb = sb("ptb", [128, 256], BF16); pTs = sb("pTs", [128, 2, 128], BF16)
        sgm = sb("sgm", [128, D]); x4 = sb("x4", [128, D]); ot = [sb(f"ot{i}", [128, D]) for i in range(2)]
        junk = sb("junk", [128, D], BF16); ss = sb("ss", [128, 1]); rstd = sb("rstd", [128, 1])
        pT = ps("pT", [128, D], BF16); pT2 = ps("pT2", [128, 256], BF16)
        PG = [ps(f"PG{i}", [128, 512]) for i in range(2)]
        PQ = [ps(f"PQ{i}", [128, 512]) for i in range(2)]
        p.dma("sp", lambda e: e.dma_start(out=gBp[:], in_=c.ple_norm_g.partition_broadcast(128)), writes=["gB_s6p"])
        p.dma("sp", lambda e: e.dma_start(out=gBf[:], in_=c.final_norm_g.partition_broadcast(128)), writes=["gB_s6f"])
        p.dma("pool", lambda e: e.dma_start(out=wg[:], in_=c.ple_gate_w.rearrange("(kc p) n -> p kc n", p=128)), writes=["s6_wg"])
        p.dma("pool", lambda e: e.dma_start(out=wp[:], in_=c.ple_proj_w.rearrange("(kc p) n -> p kc n", p=128)), writes=["s6_wp"])
        ys_keys = [f"YS{e_}_{st}" for e_ in range(NE) for st in range(NST)]
        def fetch(i):
            b = i % 2
            p.dma("sp", lambda e, i=i, b=b: e.dma_start(out=xt[b][:], in_=c.X2[i * 128:(i + 1) * 128, :]), reads=[], writes=[f"s6_xt{b}"])
            p.dma("sp", lambda e, i=i, b=b: e.dma_start(out=pt[b][:], in_=c.pin[i * 128:(i + 1) * 128, :]), writes=[f"s6_pt{b}"])
            for k in range(4):
                o("dve", lambda e, b=b, k=k: e.memset(yg[b][k][:], 0.0), [], [f"yg{b}_{k}"])
                p.dma("pool", lambda e, i=i, k=k, b=b: e.indirect_dma_start(
                    out=yg[b][k][:], out_offset=None, in_=c.YS[:, :],
                    in_offset=bass.IndirectOffsetOnAxis(ap=c.slot4i[:, i, k:k + 1], axis=0), bounds_check=_bc_reg(c, e, NE * C - 1), oob_is_err=False),
                    reads=["s5_slot4i"], writes=[f"s6_yg{b}_{k}"], grp="i")
        x3s = [x3, sb("x3b", [128, D])]
        hpTs = [hpT, sb("hpTb", [128, KC, 128], BF16)]
        pTss = [pTs, sb("pTsb", [128, 2, 128], BF16)]
        junk2 = sb("junk2", [128, D], BF16); ss2 = sb("ss2", [128, 1]); rstd2 = sb("rstd2", [128, 1])
        def front(i):
            b = i % 2
            x3_ = x3s[b]; hpT_ = hpTs[b]; pTs_ = pTss[b]
            for k in range(4):
                src = xt[b] if k == 0 else x3_
                sk = f"xt{b}" if k == 0 else f"x3_{b}"
                o("dve", lambda e, k=k, src=src: e.scalar_tensor_tensor(out=x3_[:], in0=yg[b][k][:], scalar=c.gate4[:, i, k:k + 1], in1=src[:],
                                                                        op0=ALU.mult, op1=ALU.add), [f"yg{b}_{k}", sk, "@s5_gate4"], [f"x3_{b}"])
            rmsnorm_tile(p, c, x3_[:], f"s6_x3_{b}", gBp, hp[:], "s6_hp", "s6p", junk, ss, rstd)
            for kc in range(KC):
                o("pe", lambda e, kc=kc: e.transpose(out=pT[:, kc * 128:(kc + 1) * 128], in_=hp[:, kc * 128:(kc + 1) * 128], identity=c.ident_b[:]), ["hp"], ["pT"])
            o("act", lambda e: e.activation(out=hpT_[:], in_=pT[:].rearrange("p (k t) -> p k t", k=KC), func=AF.Copy), ["pT"], [f"hpT{b}"])
            o("act", lambda e: e.activation(out=ptb[:], in_=pt[b][:], func=AF.Copy), [f"pt{b}"], ["ptb"])
            for kc in range(2):
                o("pe", lambda e, kc=kc: e.transpose(out=pT2[:, kc * 128:(kc + 1) * 128], in_=ptb[:, kc * 128:(kc + 1) * 128], identity=c.ident_b[:]), ["ptb"], ["pT2"])
            o("dve", lambda e: e.tensor_copy(out=pTs_[:], in_=pT2[:].rearrange("p (k t) -> p k t", k=2)), ["pT2"], [f"pTs{b}"])
        def back(i):
            b = i % 2
            x3_ = x3s[b]; hpT_ = hpTs[b]; pTs_ = pTss[b]
            for n in range(2):
                ns = slice(n * 512, (n + 1) * 512)
                for kc in range(KC):
                    o("pe", lambda e, n=n, kc=kc, ns=ns: e.matmul(PG[n][:], hpT_[:, kc, :], wg[:, kc, ns], start=(kc == 0), stop=(kc == KC - 1)), [f"hpT{b}", "wg"], [f"PG{n}"])
                for kc in range(2):
                    o("pe", lambda e, n=n, kc=kc, ns=ns: e.matmul(PQ[n][:], pTs_[:, kc, :], wp[:, kc, ns], start=(kc == 0), stop=(kc == 1)), [f"pTs{b}", "wp"], [f"PQ{n}"])
                o("act", lambda e, n=n, ns=ns: e.activation(out=sgm[:, ns], in_=PG[n][:], func=AF.Sigmoid), [f"PG{n}"], [f"sgm{n}"])
                o("dve", lambda e, n=n, ns=ns: e.tensor_tensor(out=sgm[:, ns], in0=PQ[n][:], in1=sgm[:, ns], op=ALU.mult), [f"PQ{n}", f"sgm{n}"], [f"sgm{n}"])
                o("dve", lambda e, n=n, ns=ns: e.tensor_tensor(out=x4[:, ns], in0=sgm[:, ns], in1=x3_[:, ns], op=ALU.add), [f"sgm{n}", f"x3_{b}"], [f"x4{n}"])
            p.op("act", lambda e: e.activation(out=junk2[:], in_=x4[:], func=AF.Square, accum_out=ss2[:]), reads=["s6_x40", "s6_x41"], writes=["s6fjunk", "s6fss"])
            p.op("act", lambda e: e.activation(out=rstd2[:], in_=ss2[:], func=AF.Sqrt, scale=1.0 / D, bias=c.eps_t[:]), reads=["s6fss", "eps_t"], writes=["s6frstd"])
            p.op("dve", lambda e: e.reciprocal(out=rstd2[:], in_=rstd2[:]), reads=["s6frstd"], writes=["s6frstd"])
            p.op("dve", lambda e: e.scalar_tensor_tensor(out=ot[b][:], in0=x4[:], scalar=rstd2[:], in1=gBf[:], op0=ALU.mult, op1=ALU.mult),
                 reads=["s6_x40", "s6_x41", "s6frstd", "gB_s6f"], writes=[f"s6_ot{b}"])
            p.dma("sp", lambda e: e.dma_start(out=c.out[i * 128:(i + 1) * 128, :], in_=ot[b][:]), reads=[f"s6_ot{b}"], writes=[f"out{i}"])
        fetch(0)
        if NT > 1:
            fetch(1)
        front(0)
        for i in range(NT):
            def nxt(i=i):
                if i + 2 < NT:
                    fetch(i + 2)
                front(i + 1)
            p.merged(lambda i=i: back(i), nxt if i + 1 < NT else None)


def build(cfg):
    nc = bass.Bass("TRN2", target_bir_lowering=False)
    c = Ctx()
    c.S = cfg["S"]; c.NB = cfg["NB"]; c.T = c.S * c.NB
    c.TG = min(512, c.T)
    c.debug = cfg.get("debug", False)
    c.HI = cfg.get("HI", 4)
    c.R32 = cfg.get("R32", False)
    T = c.T
    c.in_shapes = {}
    def din(name, shape, dt=F32):
        c.in_shapes[name] = (tuple(shape), dt)
        return nc.dram_tensor(name, shape, dt, kind="ExternalInput").ap()
    def dscr(name, shape, dt):
        kind = "ExternalOutput" if (c.debug and name in cfg.get("dbg_out", ())) else "Internal"
        return nc.dram_tensor(name, shape, dt, kind=kind).ap()
    c.x = din("x", [T, D])
    c.mix_norm_g = din("mix_norm_g", [D])
    c.w_in = din("w_in", [D, IN_COLS])
    c.ident_b_d = din("ident_b", [128, 128], BF16)
    c.QT = dscr("QT", [512, T], BF16)
    c.KT = dscr("KT", [512, T], BF16)
    c.V = dscr("V", [T, 512], BF16)
    c.ZR = dscr("ZR", [RW_COLS, T], F32)
    c.GT = dscr("GT", [2048, T], BF16)
    c.YA = dscr("YA", [512, T], BF16)
    c.YB = dscr("YB", [512, T], BF16)
    c.X2 = dscr("X2", [T, D], F32)
    c.CAP = cfg["CAP"]
    c.XS = dscr("XS", [32 * c.CAP, D], BF16)
    c.YS = dscr("YS", [32 * c.CAP, D], BF16)
    c.out = nc.dram_tensor("out", [T, D], F32, kind="ExternalOutput").ap()
    c.pin = din("p", [T, 256])
    c.ffn_norm_g = din("ffn_norm_g", [D]); c.router_w = din("router_w", [D, 32]); c.router_b = din("router_b", [1, 32])
    c.exp_w1 = din("exp_w1", [32, D, 2048]); c.exp_w2 = din("exp_w2", [32, D, D]); c.b1_l = din("b1_l", [128, 512]); c.exp_b2 = din("exp_b2", [32, D])
    c.ple_norm_g = din("ple_norm_g", [D]); c.ple_gate_w = din("ple_gate_w", [D, D]); c.ple_proj_w = din("ple_proj_w", [256, D]); c.final_norm_g = din("final_norm_g", [D])
    c.SUt_d = din("SUt", [128, 128]); c.eC_d = din("eC", [128, 32])
    c.w_out_a = din("w_out_a", [512, D]); c.w_out_b = din("w_out_b", [512, D]); c.w_out = din("w_out", [D, D])
    c.pc_d = din("pc", [64, 80]); c.mul_d = din("mul", [128, 2])
    c.w_up = din("w_up", [64, 512]); c.a_up = din("a_up", [64, 512]); c.g_up = din("g_up", [128, 512])
    c.resetm_d = din("resetm", [64, 512]); c.M1_d = din("M1", [128, 128]); c.MB_d = din("MB", [128, 256]); c.MC_d = din("MC", [128, 256])
    c.identf_d = din("identf", [128, 128]); c.ones64_d = din("ones64", [64, 64]); c.TI0_d = din("TI0", [128, 64])
    c.maskS_d = din("maskS", [128, 128], F32)
    c.negU_d = din("negU", [128, 128], BF16)
    c.negOnes_d = din("negOnes", [128, 128], BF16)
    p = Prog(nc)
    from contextlib import ExitStack
    with ExitStack() as es:
        c.ident_b = es.enter_context(nc.sbuf_tensor("ident_b_sb", [128, 128], BF16))
        c.eps_t = es.enter_context(nc.sbuf_tensor("eps_t", [128, 1], F32))
        p.dma("sp", lambda e: e.dma_start(out=c.ident_b[:], in_=c.ident_b_d), writes=["ident_b"])
        p.op("pool", lambda e: e.memset(c.eps_t[:], RMS_EPS), writes=["eps_t"])
        c.tiny_t = es.enter_context(nc.sbuf_tensor("tiny_t", [128, 1], F32))
        c.gneps_t = es.enter_context(nc.sbuf_tensor("gneps_t", [128, 1], F32))
        p.op("pool", lambda e: e.memset(c.tiny_t[:], 1e-24), writes=["s3_tiny"])
        p.op("pool", lambda e: e.memset(c.gneps_t[:], 64e-5), writes=["s3_gneps"])
        c.maskS = es.enter_context(nc.sbuf_tensor("maskS_sb", [128, 128], F32))
        c.negU = es.enter_context(nc.sbuf_tensor("negU_sb", [128, 128], BF16))
        c.negOnes = es.enter_context(nc.sbuf_tensor("negOnes_sb", [128, 128], BF16))
        p.dma("sp", lambda e: e.dma_start(out=c.maskS[:], in_=c.maskS_d), writes=["maskS"])
        p.dma("sp", lambda e: e.dma_start(out=c.negU[:], in_=c.negU_d), writes=["negU"])
        p.dma("sp", lambda e: e.dma_start(out=c.negOnes[:], in_=c.negOnes_d), writes=["negU"])
        stages = cfg.get("stages", (1, 2))
        zt = es.enter_context(nc.sbuf_tensor("zero_t", [128, 4096], BF16))
        p.op("pool", lambda e: e.memset(zt[:], 0.0), writes=["zero_t"])
        nrow = 32 * c.CAP
        for r0 in range(0, nrow, 512):
            p.dma("sp", lambda e, r0=r0: e.dma_start(out=c.XS[r0:r0 + 512, :].rearrange("(p a) n -> p a n", a=4), in_=zt[:].rearrange("p (a n) -> p a n", a=4)),
                  reads=["zero_t"], writes=[])
        p.barrier()
        if 1 in stages:
            stage1(p, nc, c)
            p.barrier()
        if 2 in stages:
            stage2(p, nc, c)
            p.barrier()
        if 3 in stages:
            stage3(p, nc, c)
            p.barrier()
        if 4 in stages:
            stage4(p, nc, c)
            p.barrier()
        if 5 in stages:
            stage5a(p, nc, c, es)
            p.barrier()
            if c.debug:
                c.dbg_slot = nc.dram_tensor("dbg_slot", [128, (T // 128) * 4], I32, kind="ExternalOutput").ap()
                c.dbg_gate = nc.dram_tensor("dbg_gate", [128, (T // 128) * 4], F32, kind="ExternalOutput").ap()
                stage5_dbg(p, nc, c)
            stage5b(p, nc, c)
            p.barrier()
            stage6(p, nc, c)
        p.finish()
        p.emit()
    build.last_in_shapes = c.in_shapes
    return nc


def consts():
    import ml_dtypes
    i = np.arange(128)
    return {"ident_b": np.eye(128, dtype=np.float32).astype(ml_dtypes.bfloat16),
            "maskS": (i[:, None] < i[None, :]).astype(np.float32),
            "negU": (-(i[:, None] >= i[None, :]).astype(np.float32)).astype(ml_dtypes.bfloat16),
            "negOnes": (-np.ones((128, 128), np.float32)).astype(ml_dtypes.bfloat16),
            **rw_consts()}


def rw_consts():
    i = np.arange(128)
    same = (i[:, None] // 32) == (i[None, :] // 32)
    lt = i[:, None] < i[None, :]
    le = i[:, None] <= i[None, :]
    gt = i[:, None] > i[None, :]
    f = lambda m: m.astype(np.float32)
    TI0 = np.zeros((128, 64), np.float32); TI0[64:, :] = np.eye(64)
    return {"M1": -f(same & gt), "MB": np.concatenate([-f(same & lt), f(same & le)], 1),
            "MC": np.concatenate([f(same & lt), f(same & le)], 1),
            "identf": np.eye(128, dtype=np.float32), "ones64": np.ones((64, 64), np.float32), "TI0": TI0,
            "resetm": np.tile(f(np.arange(512) % 32 != 0)[None, :], (64, 1))}


def moe_consts(cap):
    i = np.arange(128)
    return {"SUt": (i[:, None] < i[None, :]).astype(np.float32),
            "eC": np.tile((np.arange(32) * cap).astype(np.float32)[None, :], (128, 1))}


def b1_layout(b1):
    return np.ascontiguousarray(np.asarray(b1).reshape(32, 8, 128, 2).transpose(2, 0, 1, 3).reshape(128, 512)).astype(np.float32)


def rw_params(mu, w0, a0, k_k, k_a, r_k, lnx_w, lnx_b):
    hd = lambda v: np.ascontiguousarray(np.asarray(v).reshape(8, 64).T)
    pc = np.concatenate([hd(mu[0:512]), hd(mu[512:1024]), hd(mu[1024:1536]), hd(w0), hd(a0), hd(k_k), hd(k_a),
                         hd(r_k.reshape(-1)), hd(lnx_w), hd(lnx_b)], axis=1).astype(np.float32)
    mul = np.ascontiguousarray(np.stack([mu[1536:1664], mu[1664:1792]], 1)).astype(np.float32)
    return {"pc": pc, "mul": mul}


_S = 2048
_NB = 2
_CAP = 640


def kernel(**inputs):
    x = np.asarray(inputs["x"], dtype=np.float32)
    B, S, _ = x.shape
    assert S == _S and B == 8 * _NB
    g0 = lambda k: np.ascontiguousarray(np.asarray(inputs[k], dtype=np.float32)[0])
    pin = np.asarray(inputs["p"], dtype=np.float32)[0]
    common = dict(consts())
    common.update(moe_consts(_CAP))
    common.update(rw_params(g0("rwkv_mu"), g0("rwkv_w0"), g0("rwkv_a0"), g0("rwkv_k_k"), g0("rwkv_k_a"), g0("rwkv_r_k"),
                            g0("rwkv_lnx_w"), g0("rwkv_lnx_b")))
    common.update({
        "mix_norm_g": g0("mix_norm_g"), "w_in": g0("w_in"),
        "w_up": g0("rwkv_w_up"), "a_up": g0("rwkv_a_up"), "g_up": g0("rwkv_g_up"),
        "w_out_a": g0("w_out_a"), "w_out_b": g0("w_out_b"), "w_out": g0("w_out"),
        "ffn_norm_g": g0("ffn_norm_g"), "router_w": g0("router_w"),
        "router_b": np.ascontiguousarray(np.asarray(inputs["router_b"], dtype=np.float32).reshape(1, 32)),
        "exp_w1": g0("exp_w1"), "exp_w2": g0("exp_w2"), "b1_l": b1_layout(g0("exp_b1")), "exp_b2": g0("exp_b2"),
        "ple_norm_g": g0("ple_norm_g"), "ple_gate_w": g0("ple_gate_w"), "ple_proj_w": g0("ple_proj_w"),
        "final_norm_g": np.ascontiguousarray(np.asarray(inputs["final_norm_g"], dtype=np.float32)),
    })
    nc = build(dict(S=_S, NB=_NB, CAP=_CAP, stages=(1, 2, 3, 4, 5)))
    in_maps = []
    for ci in range(8):
        m = dict(common)
        m["x"] = np.ascontiguousarray(x[ci * _NB:(ci + 1) * _NB].reshape(_NB * S, D))
        m["p"] = np.ascontiguousarray(pin[ci * _NB:(ci + 1) * _NB].reshape(_NB * S, 256))
        in_maps.append(m)
    res = run_bass_kernel_spmd(nc, in_maps, core_ids=list(range(8)))
    outs = [np.asarray(r["out"], dtype=np.float32).reshape(_NB, S, D) for r in res.results]
    return np.concatenate(outs, axis=0)
```

```python
import numpy as np
import concourse.bass as bass
import concourse.mybir as mybir
from concourse.bass_utils import run_bass_kernel_spmd

F32 = mybir.dt.float32
BF16 = mybir.dt.bfloat16
I32 = mybir.dt.int32
U32 = mybir.dt.uint32
AF = mybir.ActivationFunctionType
ALU = mybir.AluOpType
AX = mybir.AxisListType

D = 1024
KC = 8
HD = 64
NH = 8
IN_COLS = 5376
RW_COLS = 1792
RMS_EPS = 1e-5


class Prog:
    ENGS = ["pe", "act", "dve", "pool", "sp"]
    EPOCH = 20000
    NDMA = 8

    def __init__(self, nc, same_engine_sync=True):
        self.nc = nc
        self.ops = {e: [] for e in self.ENGS}
        self.ncomp = {e: 0 for e in self.ENGS}
        self.last_write = {}
        self.readers = {}
        self.seen = {e: {} for e in self.ENGS}
        self.sems = {}
        self.dma_cnt = {}
        self.dma_n = {e: 0 for e in self.ENGS}
        self.same_engine_sync = same_engine_sync
        self.sem_ctx = []

    def _sem(self, key):
        if key not in self.sems:
            cm = self.nc.semaphore("s_" + "_".join(str(k) for k in key))
            h = cm.__enter__()
            self.sem_ctx.append(cm)
            self.sems[key] = h
        return self.sems[key]

    def _deps(self, reads, writes):
        deps = []
        for r in reads:
            t = self.last_write.get(r)
            if t is not None:
                deps.append(t)
        for w in writes:
            t = self.last_write.get(w)
            if t is not None:
                deps.append(t)
            deps.extend(self.readers.get(w, ()))
        return deps

    def _commit(self, tok, reads, writes):
        for w in writes:
            self.last_write[w] = tok
            self.readers[w] = []
        for r in reads:
            if r in writes:
                continue
            self.readers.setdefault(r, []).append(tok)

    def _waits(self, eng, deps):
        waits = {}
        for (semkey, val, src_eng) in deps:
            if src_eng == eng and (eng == "pe" or not self.same_engine_sync) and semkey[0] == "c":
                continue
            if self.seen[eng].get(semkey, 0) >= val:
                continue
            if waits.get(semkey, 0) < val:
                waits[semkey] = val
        for k, v in waits.items():
            self.seen[eng][k] = v
        return list(waits.items())

    _cap = None

    def merged(self, fa, fb):
        A = []; B = []
        self._cap = A; fa()
        self._cap = B
        if fb is not None:
            fb()
        self._cap = None
        ia = ib = 0
        while ia < len(A) or ib < len(B):
            if ib >= len(B) or (ia < len(A) and ia * len(B) <= ib * len(A)):
                it = A[ia]; ia += 1
            else:
                it = B[ib]; ib += 1
            (self.op if it[0] == "op" else self.dma)(*it[1], **it[2])

    def op(self, eng, fn, reads=(), writes=()):
        if self._cap is not None:
            self._cap.append(("op", (eng, fn), dict(reads=list(reads), writes=list(writes))))
            return None
        reads = list(reads); writes = list(writes)
        deps = self._deps(reads, writes)
        waits = self._waits(eng, deps)
        k = self.ncomp[eng]
        self.ncomp[eng] += 1
        semkey = ("c", eng, k // self.EPOCH)
        val = k % self.EPOCH + 1
        tok = (semkey, val, eng)
        self.ops[eng].append((waits, fn, semkey, 1))
        self._commit(tok, reads, writes)
        return tok

    def dma(self, eng, fn, reads=(), writes=(), grp=""):
        if self._cap is not None:
            self._cap.append(("dma", (eng, fn), dict(reads=list(reads), writes=list(writes), grp=grp)))
            return None
        reads = list(reads); writes = list(writes)
        deps = self._deps(reads, writes)
        n = self.dma_n.get(eng + grp, 0)
        self.dma_n[eng + grp] = n + 1
        semkey = ("d", eng + grp, n % self.NDMA)
        cnt = self.dma_cnt.get(semkey, 0)
        if cnt > 0:
            deps.append((semkey, cnt * 16, eng + "_dma"))
        waits = self._waits(eng, deps)
        self.dma_cnt[semkey] = cnt + 1
        tok = (semkey, (cnt + 1) * 16, eng + "_dma")
        self.ops[eng].append((waits, fn, semkey, 16))
        self._commit(tok, reads, writes)
        return tok

    def barrier(self):
        deps = [(k, c * 16, "x") for k, c in self.dma_cnt.items()]
        for e in self.ENGS:
            k = self.ncomp[e]
            if k > 0:
                deps.append((("c", e, (k - 1) // self.EPOCH), (k - 1) % self.EPOCH + 1, "x"))
        for e in self.ENGS:
            waits = self._waits(e, deps)
            if waits:
                self.ops[e].append((waits, None, None, 0))

    def finish(self, eng="sp"):
        deps = [(k, c * 16, "x") for k, c in self.dma_cnt.items()]
        waits = self._waits(eng, deps)
        self.ops[eng].append((waits, None, None, 0))

    def emit(self):
        nc = self.nc
        for e in self.ENGS:
            for (waits, fn, semkey, amt) in self.ops[e]:
                for k, v in waits:
                    self._sem(k)
                if semkey is not None:
                    self._sem(semkey)
        handles = {"pe": "tensor", "act": "scalar", "dve": "vector", "pool": "gpsimd", "sp": "sync"}
        with nc.Block() as block:
            for e in self.ENGS:
                def body(engine, e=e):
                    for (waits, fn, semkey, amt) in self.ops[e]:
                        for k, v in waits:
                            engine.wait_ge(self.sems[k], v)
                        if fn is not None:
                            ins = fn(engine)
                            ins.then_inc(self.sems[semkey], amt)
                getattr(block, handles[e])(body)
        for cm in reversed(self.sem_ctx):
            cm.__exit__(None, None, None)


class Ctx:
    pass


def rmsnorm_tile(p, c, xt_ap, xt_key, gB, out_ap, out_key, tag, junk, ss, rstd):
    p.op("act", lambda e: e.activation(out=junk[:], in_=xt_ap, func=AF.Square, accum_out=ss[:]),
         reads=[xt_key], writes=[tag + "junk", tag + "ss"])
    p.op("act", lambda e: e.activation(out=rstd[:], in_=ss[:], func=AF.Sqrt, scale=1.0 / D, bias=c.eps_t[:]),
         reads=[tag + "ss", "eps_t"], writes=[tag + "rstd"])
    p.op("dve", lambda e: e.reciprocal(out=rstd[:], in_=rstd[:]), reads=[tag + "rstd"], writes=[tag + "rstd"])
    p.op("dve", lambda e: e.scalar_tensor_tensor(out=out_ap, in0=xt_ap, scalar=rstd[:], in1=gB[:],
                                                  op0=ALU.mult, op1=ALU.mult),
         reads=[xt_key, tag + "rstd", "gB_" + tag], writes=[out_key])


def stage1(p, nc, c):
    T = c.T
    NT = T // 128
    NG = T // c.TG
    TG = c.TG
    from contextlib import ExitStack
    with ExitStack() as es:
        def sb(name, shape, dt):
            return es.enter_context(nc.sbuf_tensor(name, shape, dt))
        def ps(name, shape, dt):
            return es.enter_context(nc.psum_tensor(name, shape, dt))
        gB = sb("s1_gB", [128, D], F32)
        xt = [sb(f"s1_xt{i}", [128, D], F32) for i in range(2)]
        junk = sb("s1_junk", [128, D], BF16)
        ss = sb("s1_ss", [128, 1], F32)
        rstd = sb("s1_rstd", [128, 1], F32)
        xn = [sb(f"s1_xn{i}", [128, D], BF16) for i in range(2)]
        hT = sb("s1_hT", [128, KC, T], BF16)
        wb = [sb(f"s1_wb{i}", [128, KC, 512], BF16) for i in range(2)]
        evf = [sb(f"s1_evf{i}", [128, TG], F32) for i in range(3)]
        evb = [sb(f"s1_evb{i}", [128, 512], BF16) for i in range(3)]
        psT = [ps(f"s1_psT{i}", [128, D], BF16) for i in range(2)]
        pz = [ps(f"s1_pz{i}", [128, 512], F32) for i in range(4)]

        p.dma("sp", lambda e: e.dma_start(out=gB[:], in_=c.mix_norm_g.partition_broadcast(128)), writes=["gB_s1"])
        for i in range(NT):
            b = i % 2
            p.dma("sp", lambda e, i=i, b=b: e.dma_start(out=xt[b][:], in_=c.x[i * 128:(i + 1) * 128, :]),
                  writes=[f"s1_xt{b}"])
            rmsnorm_tile(p, c, xt[b][:], f"s1_xt{b}", gB, xn[b][:], f"s1_xn{b}", "s1", junk, ss, rstd)
            for kc in range(KC):
                p.op("pe", lambda e, b=b, kc=kc: e.transpose(out=psT[b][:, kc * 128:(kc + 1) * 128],
                                                              in_=xn[b][:, kc * 128:(kc + 1) * 128],
                                                              identity=c.ident_b[:]),
                     reads=[f"s1_xn{b}", "ident_b"], writes=[f"s1_psT{b}"])
            eng = "act" if i % 2 == 0 else "dve"
            if eng == "act":
                p.op("act", lambda e, b=b, i=i: e.activation(out=hT[:, :, i * 128:(i + 1) * 128],
                                                            in_=psT[b][:].rearrange("p (k t) -> p k t", k=KC),
                                                            func=AF.Copy),
                     reads=[f"s1_psT{b}"], writes=[f"s1_hT{i // (TG // 128)}"])
            else:
                p.op("dve", lambda e, b=b, i=i: e.tensor_copy(out=hT[:, :, i * 128:(i + 1) * 128],
                                                             in_=psT[b][:].rearrange("p (k t) -> p k t", k=KC)),
                     reads=[f"s1_psT{b}"], writes=[f"s1_hT{i // (TG // 128)}"])

        w_v = c.w_in.rearrange("(kc p) n -> p kc n", p=128)
        nblk = (IN_COLS + 511) // 512
        cnt = 0
        for blk in range(nblk):
            c0 = blk * 512
            cw = min(512, IN_COLS - c0)
            wbb = wb[blk % 2]
            wkey = f"s1_wb{blk % 2}"
            p.dma("pool", lambda e, wbb=wbb, c0=c0, cw=cw: e.dma_start(out=wbb[:, :, 0:cw], in_=w_v[:, :, c0:c0 + cw]),
                  writes=[wkey])
            if c0 == 1024:
                for i in range(NT):
                    pzz = pz[cnt % 4]; pk = f"s1_pz{cnt % 4}"
                    ev = evb[cnt % 3]; ek = f"s1_evb{cnt % 3}"
                    for kc in range(KC):
                        p.op("pe", lambda e, pzz=pzz, kc=kc, i=i, wbb=wbb: e.matmul(
                            pzz[:], hT[:, kc, i * 128:(i + 1) * 128], wbb[:, kc, :], start=(kc == 0), stop=(kc == KC - 1)),
                            reads=[f"s1_hT{i // (TG // 128)}", wkey], writes=[pk])
                    if cnt % 2 == 0:
                        p.op("act", lambda e, pzz=pzz, ev=ev: e.activation(out=ev[:], in_=pzz[:], func=AF.Copy),
                             reads=[pk], writes=[ek])
                    else:
                        p.op("dve", lambda e, pzz=pzz, ev=ev: e.tensor_copy(out=ev[:], in_=pzz[:]),
                             reads=[pk], writes=[ek])
                    p.dma("sp", lambda e, ev=ev, i=i: e.dma_start(out=c.V[i * 128:(i + 1) * 128, :], in_=ev[:]),
                          reads=[ek], writes=[])
                    cnt += 1
                continue
            for ch in range(cw // 128):
                col = c0 + ch * 128
                for g in range(NG):
                    pzz = pz[cnt % 4]; pk = f"s1_pz{cnt % 4}"
                    for kc in range(KC):
                        p.op("pe", lambda e, pzz=pzz, kc=kc, g=g, ch=ch, wbb=wbb: e.matmul(
                            pzz[:, 0:TG], wbb[:, kc, ch * 128:(ch + 1) * 128], hT[:, kc, g * TG:(g + 1) * TG],
                            start=(kc == 0), stop=(kc == KC - 1)),
                            reads=[f"s1_hT{g}", wkey], writes=[pk])
                    if col < 1024:
                        ev = evb[cnt % 3]; ek = f"s1_evb{cnt % 3}"
                        sc = 0.125 if col < 512 else 1.0
                        dst = (c.QT if col < 512 else c.KT)
                        r0 = col % 512
                        p.op("act", lambda e, pzz=pzz, ev=ev, sc=sc: e.activation(out=ev[:, 0:TG], in_=pzz[:, 0:TG], func=AF.Copy, scale=sc),
                             reads=[pk], writes=[ek])
                        p.dma("sp", lambda e, ev=ev, dst=dst, r0=r0, g=g: e.dma_start(
                            out=dst[r0:r0 + 128, g * TG:(g + 1) * TG], in_=ev[:, 0:TG]), reads=[ek], writes=[])
                    elif col < 1536 + RW_COLS:
                        ev = evf[cnt % 3]; ek = f"s1_evf{cnt % 3}"
                        r0 = col - 1536
                        p.op("dve", lambda e, pzz=pzz, ev=ev: e.tensor_copy(out=ev[:, 0:TG], in_=pzz[:, 0:TG]),
                             reads=[pk], writes=[ek])
                        p.dma("sp", lambda e, ev=ev, r0=r0, g=g: e.dma_start(
                            out=c.ZR[r0:r0 + 128, g * TG:(g + 1) * TG], in_=ev[:, 0:TG]), reads=[ek], writes=[])
                    else:
                        ev = evb[cnt % 3]; ek = f"s1_evb{cnt % 3}"
                        r0 = col - (1536 + RW_COLS)
                        p.op("act", lambda e, pzz=pzz, ev=ev: e.activation(out=ev[:, 0:TG], in_=pzz[:, 0:TG], func=AF.Sigmoid),
                             reads=[pk], writes=[ek])
                        p.dma("sp", lambda e, ev=ev, r0=r0, g=g: e.dma_start(
                            out=c.GT[r0:r0 + 128, g * TG:(g + 1) * TG], in_=ev[:, 0:TG]), reads=[ek], writes=[])
                    cnt += 1


def stage2(p, nc, c):
    S = c.S
    QG = min(512, S)
    NQG = S // QG
    nbg = QG // 128
    NKB = S // 128
    from contextlib import ExitStack
    with ExitStack() as es:
        def sb(name, shape, dt):
            return es.enter_context(nc.sbuf_tensor(name, shape, dt))
        def ps(name, shape, dt):
            return es.enter_context(nc.psum_tensor(name, shape, dt))
        qT = sb("s2_qT", [64, NH, S], BF16)
        kT = sb("s2_kT", [64, NH, S], BF16)
        vv = sb("s2_v", [128, NKB, 512], BF16)
        Lall = [[sb(f"s2_L{pp}_{i}", [128, QG], BF16) for i in range(NKB)] for pp in range(2)]
        Eb = [sb(f"s2_E{i}", [128, QG], F32) for i in range(2)]
        att = [sb(f"s2_att{i}", [128, QG], BF16) for i in range(3)]
        yas = [sb(f"s2_ya{i}", [64, QG], BF16) for i in range(2)]
        pz = [ps(f"s2_pz{i}", [128, QG], F32) for i in range(2)]
        pa = [ps(f"s2_pa{i}", [128, QG], F32) for i in range(3)]
        py = [ps(f"s2_py{i}", [64, QG], F32) for i in range(2)]
        cntA = [0]; cntB = [0]; cntY = [0]
        for b in range(c.NB):
            t0 = b * S
            p.dma("sp", lambda e, t0=t0: e.dma_start(out=qT[:], in_=c.QT.rearrange("(h d) t -> d h t", d=64)[:, :, t0:t0 + S]),
                  reads=[], writes=["s2_q"])
            p.dma("sp", lambda e, t0=t0: e.dma_start(out=kT[:], in_=c.KT.rearrange("(h d) t -> d h t", d=64)[:, :, t0:t0 + S]),
                  reads=[], writes=["s2_k"])
            p.dma("sp", lambda e, t0=t0: e.dma_start(out=vv[:], in_=c.V[t0:t0 + S, :].rearrange("(i p) n -> p i n", p=128)),
                  reads=[], writes=["s2_v"])
            work = [(h, G) for h in range(NH) for G in range(NQG)]
            def phaseA(h, G, par, t0=t0):
                L = Lall[par]
                nkb = (G + 1) * nbg
                q0 = G * QG
                for kb in range(nkb):
                    j = kb - G * nbg
                    c0 = max(0, j) * 128
                    ia = cntA[0]; cntA[0] += 1
                    pzz = pz[ia % 2]; pk = f"s2_pz{ia % 2}"
                    E = Eb[ia % 2]; ek = f"s2_E{ia % 2}"
                    p.op("pe", lambda e, pzz=pzz, kb=kb, c0=c0: e.matmul(
                        pzz[:, c0:QG], kT[:, h, kb * 128:(kb + 1) * 128], qT[:, h, q0 + c0:q0 + QG], start=True, stop=True),
                        reads=["s2_q", "s2_k"], writes=[pk])
                    p.op("act", lambda e, pzz=pzz, E=E, c0=c0: e.activation(out=E[:, c0:QG], in_=pzz[:, c0:QG], func=AF.Exp),
                         reads=[pk], writes=[ek])
                    p.op("act", lambda e, E=E, kb=kb, c0=c0: e.activation(out=L[kb][:, c0:QG], in_=E[:, c0:QG], func=AF.Ln, bias=1.0),
                         reads=[ek], writes=[f"s2_L{par}_{kb}"])
                    if j >= 0:
                        p.op("dve", lambda e, kb=kb, c0=c0: e.tensor_tensor(out=L[kb][:, c0:c0 + 128], in0=L[kb][:, c0:c0 + 128],
                                                                              in1=c.maskS[:], op=ALU.mult),
                             reads=[f"s2_L{par}_{kb}", "maskS"], writes=[f"s2_L{par}_{kb}"])
            def phaseB(h, G, par, t0=t0):
                L = Lall[par]
                nkb = (G + 1) * nbg
                q0 = G * QG
                iy = cntY[0]; cntY[0] += 1
                pyy = py[iy % 2]; pyk = f"s2_py{iy % 2}"
                ya = yas[iy % 2]; yak = f"s2_ya{iy % 2}"
                for kb in range(nkb):
                    j = kb - G * nbg
                    c0 = max(0, j) * 128
                    ib = cntB[0]; cntB[0] += 1
                    paa = pa[ib % 3]; pk = f"s2_pa{ib % 3}"
                    at = att[ib % 3]; ak = f"s2_att{ib % 3}"
                    p.op("pe", lambda e, paa=paa, kb=kb, c0=c0: e.matmul(
                        paa[:, c0:QG], kT[:, h, kb * 128:(kb + 1) * 128], qT[:, h, q0 + c0:q0 + QG], start=True, stop=False),
                        reads=["s2_q", "s2_k"], writes=[pk])
                    for kb2 in range(nkb - 1, kb - 1, -1):
                        j2 = kb2 - G * nbg
                        c2 = max(0, j2) * 128
                        lhs = c.negU if kb2 == kb else c.negOnes
                        p.op("pe", lambda e, paa=paa, lhs=lhs, kb2=kb2, c2=c2, stp=(kb2 == kb): e.matmul(
                            paa[:, c2:QG], lhs[:], L[kb2][:, c2:QG], start=False, stop=stp),
                            reads=[f"s2_L{par}_{kb2}", "negU"], writes=[pk])
                    p.op("act", lambda e, paa=paa, at=at, c0=c0: e.activation(out=at[:, c0:QG], in_=paa[:, c0:QG], func=AF.Exp),
                         reads=[pk], writes=[ak])
                    if j >= 0:
                        p.op("dve", lambda e, at=at, c0=c0: e.tensor_tensor(out=at[:, c0:c0 + 128], in0=at[:, c0:c0 + 128],
                                                                             in1=c.maskS[:], op=ALU.mult),
                             reads=[ak, "maskS"], writes=[ak])
                    p.op("pe", lambda e, pyy=pyy, at=at, kb=kb, c0=c0, nkb=nkb: e.matmul(
                        pyy[:, c0:QG], vv[:, kb, h * 64:(h + 1) * 64], at[:, c0:QG], start=(kb == 0), stop=(kb == nkb - 1)),
                        reads=[ak, "s2_v"], writes=[pyk])
                p.op("dve", lambda e, pyy=pyy, ya=ya: e.tensor_copy(out=ya[:], in_=pyy[:]), reads=[pyk], writes=[yak])
                p.dma("sp", lambda e, ya=ya: e.dma_start(
                    out=c.YA[h * 64:(h + 1) * 64, t0 + q0:t0 + q0 + QG], in_=ya[:]), reads=[yak], writes=[])
            phaseA(work[0][0], work[0][1], 0)
            for wi, (h, G) in enumerate(work):
                par = wi % 2
                if wi + 1 < len(work):
                    hn_, Gn_ = work[wi + 1]
                    p.merged(lambda: phaseB(h, G, par), lambda: phaseA(hn_, Gn_, 1 - par))
                else:
                    phaseB(h, G, par)


def stage3(p, nc, c):
    S = c.S
    TG = min(512, S)
    NG = S // TG
    NTG = TG // 128
    SC = 32
    HI = c.HI
    R32 = c.R32
    from contextlib import ExitStack
    with ExitStack() as es:
        def sb(name, shape, dt=F32):
            return es.enter_context(nc.sbuf_tensor("s3_" + name, shape, dt))
        def ps(name, shape, dt=F32):
            return es.enter_context(nc.psum_tensor("s3_" + name, shape, dt))
        cap = {"buf": None}
        def flat(ks):
            out = []
            for k in ks:
                if isinstance(k, (tuple, list)):
                    out.extend(k)
                else:
                    out.append(k)
            return out
        def o(eng, fn, reads, writes):
            it = ("op", eng, fn, ["s3_" + r for r in flat(reads)], ["s3_" + w for w in flat(writes)])
            if cap["buf"] is not None:
                cap["buf"].append(it)
            else:
                p.op(eng, fn, reads=it[3], writes=it[4])
        def d(eng, fn, reads=(), writes=()):
            it = ("dma", eng, fn, list(reads), list(writes))
            if cap["buf"] is not None:
                cap["buf"].append(it)
            else:
                p.dma(eng, fn, reads=it[3], writes=it[4])
        def run_item(it):
            if it[0] == "op":
                p.op(it[1], it[2], reads=it[3], writes=it[4])
            else:
                p.dma(it[1], it[2], reads=it[3], writes=it[4])
        def merged(fa, fb):
            A = []; B = []
            cap["buf"] = A; fa(); cap["buf"] = B
            if fb is not None:
                fb()
            cap["buf"] = None
            ia = ib = 0
            while ia < len(A) or ib < len(B):
                if ib >= len(B) or (ia < len(A) and ia * len(B) <= ib * len(A)):
                    run_item(A[ia]); ia += 1
                else:
                    run_item(B[ib]); ib += 1
        def r32(ap):
            return ap.bitcast(mybir.dt.float32r) if R32 else ap

        pc = sb("pc", [64, 80]); omka = sb("omka", [64, 8]); mul = sb("mul", [128, 2])
        WL = sb("WL", [128, 512]); GU = sb("GU", [128, 512]); resetm = sb("resetm", [64, TG])
        M1 = sb("M1", [128, 128]); MB = sb("MB", [128, 256]); MC = sb("MC", [128, 256])
        identf = sb("identf", [128, 128]); ones64 = sb("ones64", [64, 64]); TI0 = sb("TI0", [128, 64])
        for nm, t, src in (("pc", pc, c.pc_d), ("mul", mul, c.mul_d), ("GU", GU, c.g_up), ("M1", M1, c.M1_d), ("MB", MB, c.MB_d),
                           ("MC", MC, c.MC_d), ("identf", identf, c.identf_d), ("ones64", ones64, c.ones64_d), ("TI0", TI0, c.TI0_d)):
            p.dma("sp", lambda e, t=t, src=src: e.dma_start(out=t[:], in_=src), writes=["s3_" + nm])
        p.dma("sp", lambda e: e.dma_start(out=WL[0:64, :], in_=c.w_up), writes=["s3_WL"])
        p.dma("sp", lambda e: e.dma_start(out=WL[64:128, :], in_=c.a_up), writes=["s3_WL"])
        p.dma("sp", lambda e: e.dma_start(out=resetm[:], in_=c.resetm_d[:, 0:TG]), writes=["s3_resetm"])
        PMU, PW0, PA0, PKK, PKA, PRK, PLW, PLB = 0, 24, 32, 40, 48, 56, 64, 72
        o("act", lambda e: e.activation(out=WLb[:], in_=WL[:], func=AF.Copy), ["WL"], ["WLb"])
        o("act", lambda e: e.activation(out=GUb[:], in_=GU[:], func=AF.Copy), ["GU"], ["GUb"])
        o("act", lambda e: e.activation(out=ones64b[:], in_=ones64[:], func=AF.Copy), ["ones64"], ["ones64b"])
        o("dve", lambda e: e.tensor_scalar(out=omka[:], in0=pc[:, PKA:PKA + 8], scalar1=-1.0, scalar2=1.0, op0=ALU.mult, op1=ALU.add), ["pc"], ["omka"])

        zl = [sb(f"zl{i}", [128, TG]) for i in range(2)]
        zlb = [sb(f"zlb{i}", [128, TG], BF16) for i in range(2)]
        WLb = sb("WLb", [128, 512], BF16); GUb = sb("GUb", [128, 512], BF16); ones64b = sb("ones64b", [64, 64], BF16)
        tmpAb = sb("tmpAb", [64, TG], BF16); tmpBb = sb("tmpBb", [64, TG], BF16)
        zlp = [sb(f"zlp{i}", [128, TG]) for i in range(2)]
        z3 = sb("z3", [64, 3, TG]); pv3 = sb("pv3", [64, 3, TG])
        logw = sb("logw", [64, TG]); av = sb("av", [64, TG]); kk = sb("kk", [64, TG]); tmpA = sb("tmpA", [64, TG]); rn = sb("rn", [64, TG])
        kkn = sb("kkn", [64, TG]); kp = sb("kp", [64, TG]); bv = sb("bv", [64, TG]); LG = sb("LG", [64, TG]); eNeg = sb("eNeg", [64, TG])
        tmpB = sb("tmpB", [64, TG]); eD = sb("eD", [64, TG]); eLG = sb("eLG", [64, TG])
        KR = [sb(f"KR{i}", [64, 2, TG], BF16) for i in range(2)]; btil = [sb(f"btil{i}", [64, TG], BF16) for i in range(2)]
        ktil = [sb(f"ktil{i}", [64, TG], BF16) for i in range(2)]; BK = [sb(f"BK{i}", [64, 2, TG], BF16) for i in range(2)]
        vv = [sb(f"vv{i}", [64, TG], BF16) for i in range(2)]; GC = [sb(f"GC{i}", [64, TG // SC]) for i in range(2)]
        gv = [sb(f"gv{j}", [64, TG]) for j in range(2 * HI)]; bon = [sb(f"bon{j}", [64, TG]) for j in range(2 * HI)]
        PHI = [[sb(f"PHI{j}_{t}", [128, 4, 64]) for t in range(NTG)] for j in range(HI)]
        RYT = [[sb(f"RYT{j}_{t}", [128, 128]) for t in range(NTG)] for j in range(HI)]
        NBUF = NTG
        def tl(name, shape):
            return [sb(f"{name}{i}", shape, BF16) for i in range(NBUF)]
        idb = c.ident_b[0:64, 0:64]
        def qb_(q):
            return q[:].bitcast(BF16)
        Nm = tl("Nm", [128, 128]); NTm = tl("NTm", [128, 128]); AT = tl("AT", [128, 3, 128])
        KA = tl("KA", [128, 128]); ZV = tl("ZV", [128, 128]); BKtok = tl("BKtok", [128, 128]); X3 = tl("X3", [32, 192])
        Ma = tl("Ma", [128, 128]); Mat = tl("Mat", [128, 128]); Mb = tl("Mb", [128, 128]); Mbt = tl("Mbt", [128, 128])
        Ra = tl("Ra", [128, 128]); Rat = tl("Rat", [128, 128]); Rb = tl("Rb", [128, 128]); Rbt = tl("Rbt", [128, 128])
        nWX = tl("nWX", [128, 128]); nWX3 = tl("nWX3", [32, 128]); ZV3 = tl("ZV3", [32, 128])
        TIall = [[sb(f"TIst{hh}_{i}", [128, 64]) for i in range(2)] for hh in range(NH)]
        ti_cur = [0] * NH

        assert HI <= 4
        py2 = [ps(f"py{i}", [128, 512]) for i in range(2)]
        pL = [ps(f"pL{i}", [128, 512]) for i in range(2)]
        pq = [ps(f"pq{i}", [128, 512]) for i in range(2)]
        pS2 = [ps(f"pS{i}", [128, 512]) for i in range(2)]
        cnt = {"q": 0, "L": 0}
        def nq():
            i = cnt["q"] % 2; cnt["q"] += 1
            return pq[i], f"pq{i}"
        banks6 = [(pq[0], ("pq0",)), (pq[1], ("pq1",)), (py2[0], ("py0", "py1")), (py2[1], ("py2", "py3")),
                  (pS2[0], ("pS0", "pS1")), (pS2[1], ("pS2", "pS3"))]
        cnt["q6"] = 0
        def nq6():
            i = cnt["q6"] % 6; cnt["q6"] += 1
            return banks6[i]
        def nL():
            i = cnt["L"] % 2; cnt["L"] += 1
            return pL[i], f"pL{i}"
        for i in range(NBUF):
            o("pool", lambda e, i=i: e.memset(ZV[i][:, 0:64], 0.0), [], [f"ZV{i}"])
            o("pool", lambda e, i=i: e.memset(ZV3[i][:, 0:64], 0.0), [], [f"X3b{i}"])

        def lora_prep(b, G, t0):
            for li in range(2):
                r0 = 1536 + li * 128
                d("sp", lambda e, li=li, r0=r0: e.dma_start(out=zl[li][:], in_=c.ZR[r0:r0 + 128, t0:t0 + TG]), reads=[], writes=[f"s3_zl{li}"])
                if G == 0:
                    o("pool", lambda e, li=li: e.memset(zlp[li][:, 0:1], 0.0), [], [f"zlp{li}"])
                    d("sp", lambda e, li=li, r0=r0: e.dma_start(out=zlp[li][:, 1:TG], in_=c.ZR[r0:r0 + 128, t0:t0 + TG - 1]), reads=[], writes=[f"s3_zlp{li}"])
                else:
                    d("sp", lambda e, li=li, r0=r0: e.dma_start(out=zlp[li][:], in_=c.ZR[r0:r0 + 128, t0 - 1:t0 + TG - 1]), reads=[], writes=[f"s3_zlp{li}"])
                o("dve", lambda e, li=li: e.tensor_tensor(out=zlp[li][:], in0=zlp[li][:], in1=zl[li][:], op=ALU.subtract), [f"zlp{li}", f"zl{li}"], [f"zlp{li}"])
                o("dve", lambda e, li=li: e.scalar_tensor_tensor(out=zl[li][:], in0=zlp[li][:], scalar=mul[:, li:li + 1], in1=zl[li][:], op0=ALU.mult, op1=ALU.add),
                  [f"zlp{li}", f"zl{li}", "mul"], [f"zl{li}"])
            o("act", lambda e: e.activation(out=zlb[0][0:64, :], in_=zl[0][0:64, :], func=AF.Tanh), ["zl0"], ["zlb0"])
            o("act", lambda e: e.activation(out=zlb[0][64:128, :], in_=zl[0][64:128, :], func=AF.Copy), ["zl0"], ["zlb0"])
            o("act", lambda e: e.activation(out=zlb[1][:], in_=zl[1][:], func=AF.Sigmoid), ["zl1"], ["zlb1"])

        def head_prep(h, j, hp, G, t0, js):
            hc = slice(h * 64, (h + 1) * 64)
            zv = c.ZR[0:1536, :].rearrange("(three hh d) t -> d three hh t", three=3, hh=NH)
            d("sp", lambda e: e.dma_start(out=z3[:], in_=zv[:, :, h, t0:t0 + TG]), reads=[], writes=["s3_z3"])
            if G == 0:
                o("pool", lambda e: e.memset(pv3[:, :, 0:1], 0.0), [], ["pv3"])
                d("sp", lambda e: e.dma_start(out=pv3[:, :, 1:TG], in_=zv[:, :, h, t0:t0 + TG - 1]), reads=[], writes=["s3_pv3"])
            else:
                d("sp", lambda e: e.dma_start(out=pv3[:], in_=zv[:, :, h, t0 - 1:t0 + TG - 1]), reads=[], writes=["s3_pv3"])
            o("dve", lambda e: e.tensor_tensor(out=pv3[:], in0=pv3[:], in1=z3[:], op=ALU.subtract), ["pv3", "z3"], ["pv3"])
            for q in range(3):
                dst = pv3[:, q, :] if q < 2 else vv[hp][:]
                dk = "pv3" if q < 2 else f"vv{hp}"
                o("dve", lambda e, q=q, dst=dst: e.scalar_tensor_tensor(out=dst, in0=pv3[:, q, :], scalar=pc[:, PMU + q * 8 + h:PMU + q * 8 + h + 1],
                                                                        in1=z3[:, q, :], op0=ALU.mult, op1=ALU.add), ["pv3", "z3", "pc"], [dk])
            rr = pv3[:, 0, :]; kr = pv3[:, 1, :]; vr = vv[hp][:]
            pa, pak = nL()
            o("pe", lambda e: e.matmul(pa[0:64, 0:TG], WLb[0:64, hc], zlb[0][0:64, :], start=True, stop=True), ["WLb", "zlb0"], [pak])
            o("act", lambda e: e.activation(out=logw[:], in_=pa[0:64, 0:TG], func=AF.Sigmoid, bias=pc[:, PW0 + h:PW0 + h + 1]), [pak, "pc"], ["logw"])
            pb, pbk = nL()
            o("pe", lambda e: e.matmul(pb[0:64, 0:TG], WLb[64:128, hc], zlb[0][64:128, :], start=True, stop=True), ["WLb", "zlb0"], [pbk])
            o("act", lambda e: e.activation(out=av[:], in_=pb[0:64, 0:TG], func=AF.Sigmoid, bias=pc[:, PA0 + h:PA0 + h + 1]), [pbk, "pc"], ["av"])
            pg_, pgk = nL()
            o("pe", lambda e: e.matmul(pg_[0:64, 0:TG], GUb[:, hc], zlb[1][:], start=True, stop=True), ["GUb", "zlb1"], [pgk])
            o("act", lambda e: e.activation(out=gv[js][:], in_=pg_[0:64, 0:TG], func=AF.Copy), [pgk], [f"gv{js}"])
            o("act", lambda e: e.activation(out=logw[:], in_=logw[:], func=AF.Copy, scale=-0.6065306597126334), ["logw"], ["logw"])
            o("dve", lambda e: e.tensor_scalar(out=kk[:], in0=kr, scalar1=pc[:, PKK + h:PKK + h + 1], scalar2=None, op0=ALU.mult), ["pv3", "pc"], ["kk"])
            o("act", lambda e: e.activation(out=tmpAb[:], in_=kk[:], func=AF.Square), ["kk"], ["tmpAb"])
            pk_, pkk = nL()
            o("pe", lambda e: e.matmul(pk_[0:64, 0:TG], ones64b[:], tmpAb[:], start=True, stop=True), ["ones64b", "tmpAb"], [pkk])
            o("act", lambda e: e.activation(out=rn[:], in_=pk_[0:64, 0:TG], func=AF.Sqrt, bias=c.tiny_t[0:64, :]), [pkk, "tiny"], ["rn"])
            o("dve", lambda e: e.reciprocal(out=rn[:], in_=rn[:]), ["rn"], ["rn"])
            o("dve", lambda e: e.tensor_tensor(out=kkn[:], in0=kk[:], in1=rn[:], op=ALU.mult), ["kk", "rn"], ["kkn"])
            o("act", lambda e: e.activation(out=tmpB[:], in_=av[:], func=AF.Identity, scale=pc[:, PKA + h:PKA + h + 1], bias=omka[:, h:h + 1]),
              ["av", "pc", "omka"], ["tmpB"])
            o("pool", lambda e: e.tensor_tensor(out=kp[:], in0=kr, in1=tmpB[:], op=ALU.mult), ["pv3", "tmpB"], ["kp"])
            o("pool", lambda e: e.tensor_tensor(out=bv[:], in0=av[:], in1=kkn[:], op=ALU.mult), ["av", "kkn"], ["bv"])
            o("dve", lambda e: e.tensor_tensor_scan(out=LG[:], data0=resetm[:], data1=logw[:], initial=0.0, op0=ALU.mult, op1=ALU.add), ["resetm", "logw"], ["LG"])
            o("act", lambda e: e.activation(out=eLG[:], in_=LG[:], func=AF.Exp), ["LG"], ["eLG"])
            o("act", lambda e: e.activation(out=eNeg[:], in_=LG[:], func=AF.Exp, scale=-1.0), ["LG"], ["eNeg"])
            o("pool", lambda e: e.tensor_tensor(out=tmpA[:], in0=LG[:], in1=logw[:], op=ALU.subtract), ["LG", "logw"], ["tmpA"])
            o("act", lambda e: e.activation(out=tmpA[:], in_=tmpA[:], func=AF.Exp), ["tmpA"], ["tmpA"])
            o("dve", lambda e: e.tensor_tensor(out=KR[hp][:, 0, :], in0=kkn[:], in1=tmpA[:], op=ALU.mult), ["kkn", "tmpA"], [f"KR{hp}"])
            o("dve", lambda e: e.tensor_tensor(out=KR[hp][:, 1, :], in0=rr, in1=eLG[:], op=ALU.mult), ["pv3", "eLG"], [f"KR{hp}"])
            o("act", lambda e: e.activation(out=GC[hp][:], in_=eLG[:].rearrange("p (c s) -> p c s", s=SC)[:, :, SC - 1], func=AF.Copy), ["eLG"], [f"GC{hp}"])
            o("pool", lambda e: e.tensor_tensor(out=btil[hp][:], in0=bv[:], in1=eNeg[:], op=ALU.mult), ["bv", "eNeg"], [f"btil{hp}"])
            o("pool", lambda e: e.tensor_tensor(out=ktil[hp][:], in0=kp[:], in1=eNeg[:], op=ALU.mult), ["kp", "eNeg"], [f"ktil{hp}"])
            LG3 = LG[:].rearrange("p (c s) -> p c s", s=SC)
            o("dve", lambda e: e.tensor_tensor(out=eD[:].rearrange("p (c s) -> p c s", s=SC), in0=LG3[:, :, SC - 1:SC].to_broadcast([64, TG // SC, SC]),
                                               in1=LG3, op=ALU.subtract), ["LG"], ["eD"])
            o("act", lambda e: e.activation(out=eD[:], in_=eD[:], func=AF.Exp), ["eD"], ["eD"])
            o("dve", lambda e: e.tensor_tensor(out=BK[hp][:, 0, :], in0=bv[:], in1=eD[:], op=ALU.mult), ["bv", "eD"], [f"BK{hp}"])
            o("pool", lambda e: e.tensor_tensor(out=BK[hp][:, 1, :], in0=kp[:], in1=eD[:], op=ALU.mult), ["kp", "eD"], [f"BK{hp}"])
            o("dve", lambda e: e.scalar_tensor_tensor(out=tmpBb[:], in0=rr, scalar=pc[:, PRK + h:PRK + h + 1], in1=kp[:], op0=ALU.mult, op1=ALU.mult),
              ["pv3", "pc", "kp"], ["tmpBb"])
            pbn, pbnk = nL()
            o("pe", lambda e: e.matmul(pbn[0:64, 0:TG], ones64b[:], tmpBb[:], start=True, stop=True), ["ones64b", "tmpBb"], [pbnk])
            o("dve", lambda e: e.tensor_tensor(out=bon[js][:], in0=pbn[0:64, 0:TG], in1=vr, op=ALU.mult), [pbnk, f"vv{hp}"], [f"bon{js}"])

        def tile_precompute(h, j, hp):
            U = list(range(NTG))
            def K_(n, u):
                return f"{n}{u}"
            def cs(u):
                return slice(u * 128, (u + 1) * 128)
            def step_mm(outs, evac):
                for u in U:
                    pb_, pbk_ = nq6()
                    outs(u, pb_, pbk_)
                    evac(u, pb_, pbk_)
            step_mm(lambda u, q, qk: o("pe", lambda e: e.matmul(q[:, 0:128], r32(KR[hp][:, 0, cs(u)]), r32(btil[hp][:, cs(u)]), start=True, stop=True),
                                       [f"KR{hp}", f"btil{hp}"], [qk]),
                    lambda u, q, qk: o("dve", lambda e: e.tensor_tensor(out=Nm[u][:], in0=q[:, 0:128], in1=M1[:], op=ALU.mult), [qk, "M1"], [K_("Nm", u)]))
            def evB(u, q, qk):
                o("dve", lambda e: e.tensor_tensor(out=NTm[u][:], in0=q[:, 0:128], in1=MB[:, 0:128], op=ALU.mult), [qk, "MB"], [K_("NTm", u)])
                o("pool", lambda e: e.tensor_tensor(out=Rat[u][:], in0=Nm[u][:], in1=identf[:], op=ALU.add), [K_("Nm", u), "identf"], [K_("Rat", u)])
                o("dve", lambda e: e.tensor_tensor(out=AT[u][:, 0, :], in0=q[:, 128:256], in1=MB[:, 128:256], op=ALU.mult), [qk, "MB"], [K_("AT0", u)])
                o("pool", lambda e: e.tensor_tensor(out=Ra[u][:], in0=NTm[u][:], in1=identf[:], op=ALU.add), [K_("NTm", u), "identf"], [K_("Ra", u)])
            step_mm(lambda u, q, qk: o("pe", lambda e: e.matmul(q[:, 0:256], r32(btil[hp][:, cs(u)]), r32(KR[hp][:, :, cs(u)]), start=True, stop=True),
                                       [f"KR{hp}", f"btil{hp}"], [qk]), evB)
            step_mm(lambda u, q, qk: o("pe", lambda e: e.matmul(q[:, 0:256], r32(ktil[hp][:, cs(u)]), r32(KR[hp][:, :, cs(u)]), start=True, stop=True),
                                       [f"KR{hp}", f"ktil{hp}"], [qk]),
                    lambda u, q, qk: o("dve", lambda e: e.tensor_tensor(out=AT[u][:, 1:3, :], in0=q[:, 0:256].rearrange("p (a b) -> p a b", a=2),
                                                                        in1=MC[:].rearrange("p (a b) -> p a b", a=2), op=ALU.mult), [qk, "MC"], [K_("AT12", u)]))
            def trA(u, q, qk):
                o("pe", lambda e: e.transpose(out=qb_(q)[:, 0:64], in_=KR[hp][:, 0, cs(u)], identity=idb), [f"KR{hp}"], [qk])
                o("pe", lambda e: e.transpose(out=qb_(q)[:, 64:128], in_=vv[hp][:, cs(u)], identity=idb), [f"vv{hp}"], [qk])
            def evA(u, q, qk):
                o("act", lambda e: e.activation(out=KA[u][:, 0:64], in_=qb_(q)[:, 0:64], func=AF.Copy), [qk], [K_("KAa", u)])
                o("act", lambda e: e.activation(out=ZV[u][:, 64:128], in_=qb_(q)[:, 64:128], func=AF.Copy), [qk], [K_("ZV", u)])
            step_mm(trA, evA)
            def trB(u, q, qk):
                o("pe", lambda e: e.transpose(out=qb_(q)[:, 0:64], in_=BK[hp][:, 0, cs(u)], identity=idb), [f"BK{hp}"], [qk])
                o("pe", lambda e: e.transpose(out=qb_(q)[:, 64:128], in_=BK[hp][:, 1, cs(u)], identity=idb), [f"BK{hp}"], [qk])
                c3 = u * 128 + 96
                o("pe", lambda e: e.transpose(out=qb_(q)[0:32, 128:192], in_=BK[hp][:, 0, c3:c3 + 32], identity=idb), [f"BK{hp}"], [qk])
                o("pe", lambda e: e.transpose(out=qb_(q)[0:32, 192:256], in_=BK[hp][:, 1, c3:c3 + 32], identity=idb), [f"BK{hp}"], [qk])
            def evB2(u, q, qk):
                o("act", lambda e: e.activation(out=BKtok[u][:], in_=qb_(q)[:, 0:128], func=AF.Copy), [qk], [K_("BKtok", u)])
                o("act", lambda e: e.activation(out=X3[u][:, 0:128], in_=qb_(q)[0:32, 128:256], func=AF.Copy), [qk], [K_("X3a", u)])
            step_mm(trB, evB2)
            def trC(u, q, qk):
                c3 = u * 128 + 96
                o("pe", lambda e: e.transpose(out=qb_(q)[0:32, 0:64], in_=vv[hp][:, c3:c3 + 32], identity=idb), [f"vv{hp}"], [qk])
                o("pe", lambda e: e.matmul(q[:, 64:128], r32(AT[u][:, 1, :]), r32(ZV[u][:, 64:128]), start=True, stop=True), [K_("AT12", u), K_("ZV", u)], [qk])
            def evC(u, q, qk):
                o("act", lambda e: e.activation(out=ZV3[u][:, 64:128], in_=qb_(q)[0:32, 0:64], func=AF.Copy), [qk], [K_("X3b", u)])
                o("act", lambda e: e.activation(out=KA[u][:, 64:128], in_=q[:, 64:128], func=AF.Copy), [qk], [K_("KAb", u)])
            step_mm(trC, evC)
            def nm(dst, lhsT, rhs, add=None, eng="dve"):
                def mmf(u, q, qk):
                    o("pe", lambda e: e.matmul(q[:, 0:128], r32(lhsT[0][u][:]), r32(rhs[0][u][:]), start=True, stop=True), [K_(lhsT[1], u), K_(rhs[1], u)], [qk])
                def evf(u, q, qk):
                    if add is None:
                        if eng == "act":
                            o("act", lambda e: e.activation(out=dst[0][u][:], in_=q[:, 0:128], func=AF.Copy), [qk], [K_(dst[1], u)])
                        else:
                            o("dve", lambda e: e.tensor_copy(out=dst[0][u][:], in_=q[:, 0:128]), [qk], [K_(dst[1], u)])
                    else:
                        o("dve", lambda e: e.tensor_tensor(out=dst[0][u][:], in0=q[:, 0:128], in1=add[0][u][:], op=ALU.add), [qk, K_(add[1], u)], [K_(dst[1], u)])
                step_mm(mmf, evf)
            M_, Mt_ = (NTm, "NTm"), (Nm, "Nm")
            A_, At_ = (Ma, "Ma"), (Mat, "Mat")
            B_, Bt_ = (Mb, "Mb"), (Mbt, "Mbt")
            R_, Rt_ = (Ra, "Ra"), (Rat, "Rat")
            Q_, Qt_ = (Rb, "Rb"), (Rbt, "Rbt")
            nm(A_, Mt_, M_, eng="act"); nm(At_, M_, Mt_, eng="dve")
            nm(Q_, Rt_, A_, add=R_); nm(Qt_, A_, Rt_, add=Rt_)
            nm(B_, At_, A_, eng="act"); nm(Bt_, A_, At_, eng="dve")
            nm(R_, Qt_, B_, add=Q_); nm(Rt_, B_, Qt_, add=Qt_)
            nm(A_, Bt_, B_, eng="act"); nm(At_, B_, Bt_, eng="dve")
            nm(Q_, Rt_, A_, add=R_); nm(Qt_, A_, Rt_, add=Rt_)
            nm(B_, At_, A_, eng="act")
            nm(R_, Qt_, B_, add=Q_)
            invT = Ra
            def mmW(u, q, qk):
                o("pe", lambda e: e.matmul(q[:, 0:128], r32(invT[u][:]), r32(KA[u][:]), start=True, stop=True), [K_("Ra", u), K_("KAa", u), K_("KAb", u)], [qk])
                o("pe", lambda e: e.matmul(q[0:32, 128:256], invT[u][:, 96:128], KA[u][:], start=True, stop=True), [K_("Ra", u), K_("KAa", u), K_("KAb", u)], [qk])
            def evW(u, q, qk):
                o("act", lambda e: e.activation(out=nWX[u][:], in_=q[:, 0:128], func=AF.Copy, scale=-1.0), [qk], [K_("nWX", u)])
                o("act", lambda e: e.activation(out=nWX3[u][:], in_=q[0:32, 128:256], func=AF.Copy, scale=-1.0), [qk], [K_("nWX3", u)])
            step_mm(mmW, evW)
            def mmR(u, q, qk):
                o("pe", lambda e: e.matmul(q[:, 0:128], r32(ZV[u][:]), r32(AT[u][:, 2, :]), start=True, stop=False), [K_("ZV", u), K_("AT12", u)], [qk])
                o("pe", lambda e: e.matmul(q[:, 0:128], r32(nWX[u][:]), r32(AT[u][:, 0, :]), start=False, stop=True), [K_("nWX", u), K_("AT0", u)], [qk])
            def evR(u, q, qk):
                o("dve", lambda e: e.tensor_tensor(out=RYT[j][u][0:64, :], in0=q[0:64, 0:128], in1=KR[hp][:, 1, cs(u)], op=ALU.add), [qk, f"KR{hp}"], [f"RYT{j}_{u}"])
                o("act", lambda e: e.activation(out=RYT[j][u][64:128, :], in_=q[64:128, 0:128], func=AF.Copy), [qk], [f"RYT{j}_{u}"])
            step_mm(mmR, evR)
            for sc in range(4):
                def mmP(u, q, qk, sc=sc):
                    if sc < 3:
                        prt = slice(sc * SC, (sc + 1) * SC)
                        o("pe", lambda e: e.matmul(q[:, 0:64], nWX[u][prt, :], BKtok[u][prt, 0:64], start=True, stop=False), [K_("nWX", u), K_("BKtok", u)], [qk])
                        o("pe", lambda e: e.matmul(q[:, 0:64], ZV[u][prt, :], BKtok[u][prt, 64:128], start=False, stop=True), [K_("ZV", u), K_("BKtok", u)], [qk])
                    else:
                        o("pe", lambda e: e.matmul(q[:, 0:64], nWX3[u][:], X3[u][:, 0:64], start=True, stop=False), [K_("nWX3", u), K_("X3a", u)], [qk])
                        o("pe", lambda e: e.matmul(q[:, 0:64], ZV3[u][:], X3[u][:, 64:128], start=False, stop=True), [K_("X3a", u), K_("X3b", u)], [qk])
                def evP(u, q, qk, sc=sc):
                    gcol = u * 4 + sc
                    o("dve", lambda e: e.scalar_tensor_tensor(out=PHI[j][u][0:64, sc, :], in0=identf[0:64, 0:64], scalar=GC[hp][:, gcol:gcol + 1], in1=q[0:64, 0:64],
                                                              op0=ALU.mult, op1=ALU.add), ["identf", f"GC{hp}", qk], [f"PHI{j}_{u}"])
                    o("act", lambda e: e.activation(out=PHI[j][u][64:128, sc, :], in_=q[64:128, 0:64], func=AF.Copy), [qk], [f"PHI{j}_{u}"])
                step_mm(mmP, evP)

        def chains(heads):
            for tt in range(NTG):
                for sc in range(4):
                    for j, h in enumerate(heads):
                        TI = TIall[h]
                        cur = ti_cur[h]; nxt = 1 - cur
                        gcol = tt * 128 + sc * SC
                        prt = slice(sc * SC, (sc + 1) * SC)
                        r0 = (j % 2) * 64
                        o("pe", lambda e, TI=TI, cur=cur, j=j, tt=tt, sc=sc, r0=r0: e.matmul(pS2[j // 2][r0:r0 + 64, 0:64], PHI[j][tt][:, sc, :], TI[cur][:], start=True, stop=True),
                          [f"PHI{j}_{tt}", f"TI{h}_{cur}_"], [f"pS{j}"])
                        o("pe", lambda e, TI=TI, cur=cur, j=j, tt=tt, prt=prt, gcol=gcol, r0=r0: e.matmul(py2[j // 2][r0:r0 + 64, gcol:gcol + SC], TI[cur][:], RYT[j][tt][:, prt], start=True, stop=True),
                          [f"RYT{j}_{tt}", f"TI{h}_{cur}_"], [f"py{j}"])
                        o("act", lambda e, TI=TI, nxt=nxt, j=j, r0=r0: e.activation(out=TI[nxt][0:64, :], in_=pS2[j // 2][r0:r0 + 64, 0:64], func=AF.Copy), [f"pS{j}"], [f"TI{h}_{nxt}_"])
                        ti_cur[h] = nxt

        ysbs = [sb(f"ysb{j}", [64, TG]) for j in range(HI)]; ysbb = [sb(f"ysbb{j}", [64, TG], BF16) for j in range(HI)]
        ycs = [sb(f"yc{j}", [64, TG]) for j in range(HI)]; tcs = [sb(f"tmpC{j}", [64, TG]) for j in range(HI)]
        tcb = [sb(f"tmpCb{j}", [64, TG], BF16) for j in range(HI)]; yos = [sb(f"yo{j}", [64, TG], BF16) for j in range(HI)]
        def post_block(heads, t0, bp):
            J = list(enumerate(heads))
            pas = {}; pbs = {}
            for j, h in J:
                r0 = (j % 2) * 64
                o("act", lambda e, j=j, r0=r0: e.activation(out=ysbs[j][:], in_=py2[j // 2][r0:r0 + 64, 0:TG], func=AF.Copy), [f"py{j}"], [f"ysb{j}"])
                o("act", lambda e, j=j, r0=r0: e.activation(out=ysbb[j][:], in_=py2[j // 2][r0:r0 + 64, 0:TG], func=AF.Copy), [f"py{j}"], [f"ysbb{j}"])
            for j, h in J:
                pas[j] = nq()
                o("pe", lambda e, j=j: e.matmul(pas[j][0][0:64, 0:TG], ones64b[:], ysbb[j][:], start=True, stop=True), ["ones64b", f"ysbb{j}"], [pas[j][1]])
                o("dve", lambda e, j=j: e.scalar_tensor_tensor(out=ycs[j][:], in0=pas[j][0][0:64, 0:TG], scalar=-1.0 / 64, in1=ysbs[j][:], op0=ALU.mult, op1=ALU.add),
                  [pas[j][1], f"ysb{j}"], [f"yc{j}"])
                o("act", lambda e, j=j: e.activation(out=tcb[j][:], in_=ycs[j][:], func=AF.Square), [f"yc{j}"], [f"tmpCb{j}"])
            for j, h in J:
                pbs[j] = nq()
                o("pe", lambda e, j=j: e.matmul(pbs[j][0][0:64, 0:TG], ones64b[:], tcb[j][:], start=True, stop=True), ["ones64b", f"tmpCb{j}"], [pbs[j][1]])
                o("act", lambda e, j=j: e.activation(out=tcs[j][:], in_=pbs[j][0][0:64, 0:TG], func=AF.Sqrt, scale=1.0 / 64, bias=c.gneps_t[0:64, :]),
                  [pbs[j][1], "gneps"], [f"tmpC{j}"])
            for j, h in J:
                o("dve", lambda e, j=j: e.reciprocal(out=tcs[j][:], in_=tcs[j][:]), [f"tmpC{j}"], [f"tmpC{j}"])
            for j, h in J:
                o("dve", lambda e, j=j: e.tensor_tensor(out=ycs[j][:], in0=ycs[j][:], in1=tcs[j][:], op=ALU.mult), [f"yc{j}", f"tmpC{j}"], [f"yc{j}"])
            for j, h in J:
                o("dve", lambda e, j=j, h=h: e.tensor_scalar(out=ycs[j][:], in0=ycs[j][:], scalar1=pc[:, PLW + h:PLW + h + 1], scalar2=pc[:, PLB + h:PLB + h + 1],
                                                          op0=ALU.mult, op1=ALU.add), [f"yc{j}", "pc"], [f"yc{j}"])
            for j, h in J:
                js = bp * HI + j
                o("pool", lambda e, j=j, js=js: e.tensor_tensor(out=ycs[j][:], in0=ycs[j][:], in1=bon[js][:], op=ALU.add), [f"yc{j}", f"bon{js}"], [f"yc{j}"])
            for j, h in J:
                js = bp * HI + j
                o("pool", lambda e, j=j, js=js: e.tensor_tensor(out=yos[j][:], in0=ycs[j][:], in1=gv[js][:], op=ALU.mult), [f"yc{j}", f"gv{js}"], [f"yo{j}"])
                d("sp", lambda e, j=j, h=h: e.dma_start(out=c.YB[h * 64:(h + 1) * 64, t0:t0 + TG], in_=yos[j][:]), reads=[f"s3_yo{j}"], writes=[])

        nhead = [0]
        nblk = [0]
        def prep_stream(h, j, G, t0, js):
            hp = nhead[0] % 2; nhead[0] += 1
            def f():
                if G == 0:
                    TI = TIall[h]
                    o("dve", lambda e, TI=TI: e.tensor_copy(out=TI[0][:], in_=TI0[:]), ["TI0"], [f"TI{h}_0_"])
                    o("dve", lambda e, TI=TI: e.tensor_copy(out=TI[1][:], in_=TI0[:]), ["TI0"], [f"TI{h}_1_"])
                    ti_cur[h] = 0
                head_prep(h, j, hp, G, t0, js)
            return f, hp
        for b in range(c.NB):
            for G in range(NG):
                t0 = b * S + G * TG
                lora_prep(b, G, t0)
                NBLK = NH // HI
                pend = None
                for hb in range(NBLK):
                    heads = [hb * HI + j for j in range(HI)]
                    bp = nblk[0] % 2; nblk[0] += 1
                    if pend is None:
                        f0, hp0 = prep_stream(heads[0], 0, G, t0, bp * HI)
                        f0()
                    else:
                        hp0 = pend
                    hps = [hp0]
                    for j, h in enumerate(heads):
                        if j + 1 < HI:
                            fb, hpn = prep_stream(heads[j + 1], j + 1, G, t0, bp * HI + j + 1)
                            hps.append(hpn)
                        else:
                            fb = None
                        merged(lambda h=h, j=j: tile_precompute(h, j, hps[j]), fb)
                    def tail(heads=heads, bp=bp):
                        chains(heads)
                        post_block(heads, t0, bp)
                    if hb + 1 < NBLK:
                        fn, pend = prep_stream((hb + 1) * HI, 0, G, t0, (1 - bp) * HI)
                        merged(tail, fn)
                    else:
                        pend = None
                        tail()


def stage4(p, nc, c):
    T = c.T
    TG = min(512, T)
    NG = T // TG
    from contextlib import ExitStack
    with ExitStack() as es:
        def sb(name, shape, dt=F32):
            return es.enter_context(nc.sbuf_tensor("s4_" + name, shape, dt))
        def ps(name, shape, dt=F32):
            return es.enter_context(nc.psum_tensor("s4_" + name, shape, dt))
        def o(eng, fn, reads, writes):
            p.op(eng, fn, reads=["s4_" + r for r in reads], writes=["s4_" + w for w in writes])
        woa = sb("woa", [128, 4, D], BF16); wob = sb("wob", [128, 4, D], BF16); wo = sb("wo", [128, KC, D], BF16)
        ya = sb("ya", [128, 4, TG], BF16); yb = sb("yb", [128, 4, TG], BF16); gt = sb("gt", [128, 16, TG], BF16)
        mg = sb("mg", [128, KC, TG], BF16)
        t1 = [sb(f"t1{i}", [128, TG]) for i in range(2)]
        t2 = [sb(f"t2{i}", [128, TG]) for i in range(2)]
        xt = [sb(f"xt{i}", [128, D]) for i in range(2)]
        x2 = [sb(f"x2{i}", [128, D]) for i in range(2)]
        PA = [ps(f"PA{i}", [128, 512]) for i in range(2)]
        PB = [ps(f"PB{i}", [128, 512]) for i in range(2)]
        PX = [ps(f"PX{i}", [128, 512]) for i in range(2)]
        p.dma("pool", lambda e: e.dma_start(out=woa[:], in_=c.w_out_a.rearrange("(kc p) n -> p kc n", p=128)), writes=["s4_woa"])
        p.dma("pool", lambda e: e.dma_start(out=wob[:], in_=c.w_out_b.rearrange("(kc p) n -> p kc n", p=128)), writes=["s4_wob"])
        p.dma("pool", lambda e: e.dma_start(out=wo[:], in_=c.w_out.rearrange("(kc p) n -> p kc n", p=128)), writes=["s4_wo"])
        n1 = 0; n2 = 0
        for g in range(NG):
            ts = slice(g * TG, (g + 1) * TG)
            p.dma("sp", lambda e, ts=ts: e.dma_start(out=ya[:], in_=c.YA.rearrange("(kc p) t -> p kc t", p=128)[:, :, ts]), reads=[], writes=["s4_ya"])
            p.dma("sp", lambda e, ts=ts: e.dma_start(out=yb[:], in_=c.YB.rearrange("(kc p) t -> p kc t", p=128)[:, :, ts]), reads=[], writes=["s4_yb"])
            p.dma("sp", lambda e, ts=ts: e.dma_start(out=gt[:], in_=c.GT.rearrange("(kc p) t -> p kc t", p=128)[:, :, ts]), reads=[], writes=["s4_gt"])
            for m in range(KC):
                u = n1 % 2; n1 += 1
                ms = slice(m * 128, (m + 1) * 128)
                for kc in range(4):
                    o("pe", lambda e, u=u, kc=kc, ms=ms: e.matmul(PA[u][:, 0:TG], woa[:, kc, ms], ya[:, kc, :], start=(kc == 0), stop=(kc == 3)),
                      ["woa", "ya"], [f"PA{u}"])
                for kc in range(4):
                    o("pe", lambda e, u=u, kc=kc, ms=ms: e.matmul(PB[u][:, 0:TG], wob[:, kc, ms], yb[:, kc, :], start=(kc == 0), stop=(kc == 3)),
                      ["wob", "yb"], [f"PB{u}"])
                o("dve", lambda e, u=u, m=m: e.tensor_tensor(out=t1[u][:], in0=PA[u][:, 0:TG], in1=gt[:, m, :], op=ALU.mult), [f"PA{u}", "gt"], [f"t1{u}"])
                o("dve", lambda e, u=u, m=m: e.tensor_tensor(out=t2[u][:], in0=PB[u][:, 0:TG], in1=gt[:, 8 + m, :], op=ALU.mult), [f"PB{u}", "gt"], [f"t2{u}"])
                o("pool", lambda e, u=u, m=m: e.tensor_tensor(out=mg[:, m, :], in0=t1[u][:], in1=t2[u][:], op=ALU.add), [f"t1{u}", f"t2{u}"], ["mg"])
            for tt in range(TG // 128):
                i = g * (TG // 128) + tt
                b = i % 2
                p.dma("sp", lambda e, i=i, b=b: e.dma_start(out=xt[b][:], in_=c.x[i * 128:(i + 1) * 128, :]), writes=[f"s4_xt{b}"])
                for n in range(2):
                    u = n2 % 2; n2 += 1
                    for m in range(KC):
                        o("pe", lambda e, u=u, m=m, tt=tt, n=n: e.matmul(PX[u][:], mg[:, m, tt * 128:(tt + 1) * 128], wo[:, m, n * 512:(n + 1) * 512],
                                                                          start=(m == 0), stop=(m == KC - 1)), ["mg", "wo"], [f"PX{u}"])
                    o("dve", lambda e, u=u, b=b, n=n: e.tensor_tensor(out=x2[b][:, n * 512:(n + 1) * 512], in0=PX[u][:], in1=xt[b][:, n * 512:(n + 1) * 512], op=ALU.add),
                      [f"PX{u}", f"xt{b}"], [f"x2{b}"])
                p.dma("sp", lambda e, i=i, b=b: e.dma_start(out=c.X2[i * 128:(i + 1) * 128, :], in_=x2[b][:]), reads=[f"s4_x2{b}"], writes=[])


def _bc_reg(c, e, val):
    if getattr(c, "_bc", None) is None:
        c._bc = e.to_reg(val)
    return c._bc


def stage5a(p, nc, c, es):
    T = c.T; NT = T // 128; C = c.CAP; NE = 32
    BIG = 1.0e6
    def sb(name, shape, dt=F32):
        return es.enter_context(nc.sbuf_tensor("s5_" + name, shape, dt))
    def o(eng, fn, reads, writes):
        p.op(eng, fn, reads=["s5_" + r for r in reads], writes=["s5_" + w for w in writes])
    c.slot4i = sb("slot4i", [128, NT, 4], I32)
    c.gate4 = sb("gate4", [128, NT, 4])
    from contextlib import ExitStack
    with ExitStack() as es2:
        def sb2(name, shape, dt=F32):
            return es2.enter_context(nc.sbuf_tensor("s5_" + name, shape, dt))
        def ps(name, shape, dt=F32):
            return es2.enter_context(nc.psum_tensor("s5_" + name, shape, dt))
        gB = sb2("gB", [128, D]); rw = sb2("rw", [128, KC, NE]); rb = sb2("rb", [1, NE]); ones_row = sb2("ones_row", [1, 128])
        SUt = sb2("SUt", [128, 128]); onesf = sb2("onesf", [128, 128]); identf = sb2("identf", [128, 128]); eC = sb2("eC", [128, NE])
        msum = sb2("msum", [128, NE])
        xt = [sb2(f"xt{i}", [128, D]) for i in range(2)]
        hn = sb2("hn", [128, D]); hnb = [sb2(f"hnb{i}", [128, D], BF16) for i in range(2)]
        junk = sb2("junk", [128, D], BF16); ss = sb2("ss", [128, 1]); rstd = sb2("rstd", [128, 1])
        hnT = sb2("hnT", [128, KC, 128])
        lg = sb2("lg", [128, NE]); top8 = sb2("top8", [128, 8]); mask = sb2("mask", [128, NE]); nm1 = sb2("nm1", [128, 1])
        ex = sb2("ex", [128, NE]); den = sb2("den", [128, 1]); gw = sb2("gw", [128, NE]); slotf = sb2("slotf", [128, NE])
        valid = sb2("valid", [128, NE]); big = sb2("big", [128, NE]); oh = sb2("oh", [128, NE]); j32 = sb2("j32", [128, NE])
        slot4f = sb2("slot4f", [128, 4])
        pT = ps("pT", [128, D]); PL = ps("PL", [128, NE]); PP = ps("PP", [128, NE])
        p.dma("sp", lambda e: e.dma_start(out=gB[:], in_=c.ffn_norm_g.partition_broadcast(128)), writes=["gB_s5"])
        p.dma("sp", lambda e: e.dma_start(out=rw[:], in_=c.router_w.rearrange("(kc p) e -> p kc e", p=128)), writes=["s5_rw"])
        p.dma("sp", lambda e: e.dma_start(out=rb[:], in_=c.router_b), writes=["s5_rb"])
        p.dma("sp", lambda e: e.dma_start(out=SUt[:], in_=c.SUt_d), writes=["s5_SUt"])
        p.dma("sp", lambda e: e.dma_start(out=identf[:], in_=c.identf_d), writes=["s5_identf"])
        p.dma("sp", lambda e: e.dma_start(out=eC[:], in_=c.eC_d), writes=["s5_eC"])
        o("pool", lambda e: e.memset(ones_row[:], 1.0), [], ["ones_row"])
        o("pool", lambda e: e.memset(onesf[:], 1.0), [], ["onesf"])
        o("pool", lambda e: e.memset(msum[:], 0.0), [], ["msum"])
        for i in range(NT):
            b = i % 2
            p.dma("sp", lambda e, i=i, b=b: e.dma_start(out=xt[b][:], in_=c.X2[i * 128:(i + 1) * 128, :]), reads=[], writes=[f"s5_xt{b}"])
            rmsnorm_tile(p, c, xt[b][:], f"s5_xt{b}", gB, hn[:], "s5_hn", "s5", junk, ss, rstd)
            o("act", lambda e, b=b: e.activation(out=hnb[b][:], in_=hn[:], func=AF.Copy), ["hn"], [f"hnb{b}"])
            for kc in range(KC):
                o("pe", lambda e, kc=kc: e.transpose(out=pT[:, kc * 128:(kc + 1) * 128], in_=hn[:, kc * 128:(kc + 1) * 128], identity=identf[:]),
                  ["hn", "identf"], ["pT"])
            o("act", lambda e: e.activation(out=hnT[:, 0:4, :], in_=pT[:, 0:512].rearrange("p (k t) -> p k t", k=4), func=AF.Copy), ["pT"], ["hnTa"])
            o("dve", lambda e: e.tensor_copy(out=hnT[:, 4:8, :], in_=pT[:, 512:1024].rearrange("p (k t) -> p k t", k=4)), ["pT"], ["hnTb"])
            for kc in range(KC):
                o("pe", lambda e, kc=kc: e.matmul(PL[:], hnT[:, kc, :], rw[:, kc, :], start=(kc == 0), stop=False), ["hnTa", "hnTb", "rw"], ["PL"])
            o("pe", lambda e: e.matmul(PL[:], ones_row[:], rb[:], start=False, stop=True), ["ones_row", "rb"], ["PL"])
            o("dve", lambda e: e.tensor_copy(out=lg[:], in_=PL[:]), ["PL"], ["lg"])
            o("dve", lambda e: e.max(out=top8[:], in_=lg[:]), ["lg"], ["top8"])
            o("dve", lambda e: e.tensor_scalar(out=mask[:], in0=lg[:], scalar1=top8[:, 3:4], scalar2=None, op0=ALU.is_ge), ["lg", "top8"], ["mask"])
            o("dve", lambda e: e.tensor_scalar(out=nm1[:], in0=top8[:, 0:1], scalar1=-1.0, scalar2=None, op0=ALU.mult), ["top8"], ["nm1"])
            o("act", lambda e: e.activation(out=ex[:], in_=lg[:], func=AF.Exp, bias=nm1[:]), ["lg", "nm1"], ["ex"])
            o("dve", lambda e: e.scalar_tensor_tensor(out=ex[:], in0=ex[:], scalar=1.0, in1=mask[:], op0=ALU.mult, op1=ALU.mult, accum_out=den[:]),
              ["ex", "mask"], ["ex", "den"])
            o("dve", lambda e: e.reciprocal(out=den[:], in_=den[:]), ["den"], ["den"])
            o("dve", lambda e: e.tensor_scalar(out=gw[:], in0=ex[:], scalar1=den[:], scalar2=None, op0=ALU.mult), ["ex", "den"], ["gw"])
            o("pe", lambda e: e.matmul(PP[:], SUt[:], mask[:], start=True, stop=False), ["SUt", "mask"], ["PP"])
            o("pe", lambda e: e.matmul(PP[:], onesf[:], msum[:], start=False, stop=True), ["onesf", "msum"], ["PP"])
            o("dve", lambda e: e.tensor_tensor(out=slotf[:], in0=PP[:], in1=eC[:], op=ALU.add), ["PP", "eC"], ["slotf"])
            o("dve", lambda e: e.tensor_scalar(out=valid[:], in0=PP[:], scalar1=float(C), scalar2=None, op0=ALU.is_lt), ["PP"], ["valid"])
            o("pool", lambda e: e.tensor_tensor(out=msum[:], in0=msum[:], in1=mask[:], op=ALU.add), ["msum", "mask"], ["msum"])
            o("dve", lambda e: e.tensor_tensor(out=valid[:], in0=valid[:], in1=mask[:], op=ALU.mult), ["valid", "mask"], ["valid"])
            o("dve", lambda e: e.tensor_scalar(out=big[:], in0=valid[:], scalar1=-BIG, scalar2=BIG, op0=ALU.mult, op1=ALU.add), ["valid"], ["big"])
            o("dve", lambda e: e.tensor_tensor(out=slotf[:], in0=slotf[:], in1=valid[:], op=ALU.mult), ["slotf", "valid"], ["slotf"])
            o("dve", lambda e: e.tensor_tensor(out=slotf[:], in0=slotf[:], in1=big[:], op=ALU.add), ["slotf", "big"], ["slotf"])
            for k in range(4):
                o("dve", lambda e, k=k: e.tensor_scalar(out=oh[:], in0=lg[:], scalar1=top8[:, k:k + 1], scalar2=None, op0=ALU.is_equal), ["lg", "top8"], ["oh"])
                o("dve", lambda e, k=k: e.scalar_tensor_tensor(out=j32[:], in0=oh[:], scalar=1.0, in1=slotf[:], op0=ALU.mult, op1=ALU.mult,
                                                               accum_out=slot4f[:, k:k + 1]), ["oh", "slotf"], ["j32", "slot4f"])
                o("dve", lambda e, k=k, i=i: e.scalar_tensor_tensor(out=j32[:], in0=oh[:], scalar=1.0, in1=gw[:], op0=ALU.mult, op1=ALU.mult,
                                                                    accum_out=c.gate4[:, i, k:k + 1]), ["oh", "gw"], ["j32", "gate4"])
            o("dve", lambda e, i=i: e.tensor_copy(out=c.slot4i[:, i, :], in_=slot4f[:]), ["slot4f"], ["slot4i"])
            for k in range(4):
                p.dma("pool", lambda e, i=i, k=k, b=b: e.indirect_dma_start(
                    out=c.XS[:, :], out_offset=bass.IndirectOffsetOnAxis(ap=c.slot4i[:, i, k:k + 1], axis=0),
                    in_=hnb[b][:], in_offset=None, bounds_check=_bc_reg(c, e, NE * C - 1), oob_is_err=False),
                    reads=[f"s5_hnb{b}", "s5_slot4i"], writes=[], grp="i")


def stage5_dbg(p, nc, c):
    NT = c.T // 128
    p.dma("sp", lambda e: e.dma_start(out=c.dbg_slot, in_=c.slot4i[:].rearrange("p a b -> p (a b)")), reads=["s5_slot4i"], writes=["dbg_slot"])
    p.dma("sp", lambda e: e.dma_start(out=c.dbg_gate, in_=c.gate4[:].rearrange("p a b -> p (a b)")), reads=["s5_gate4"], writes=["dbg_gate"])


def stage5b(p, nc, c):
    T = c.T; NT = T // 128; C = c.CAP; NE = 32
    NST = C // 128
    SG = C // 2 if C > 512 else C
    NSG = C // SG
    from contextlib import ExitStack
    with ExitStack() as es:
        def sb(name, shape, dt=F32):
            return es.enter_context(nc.sbuf_tensor("s5b_" + name, shape, dt))
        def ps(name, shape, dt=F32):
            return es.enter_context(nc.psum_tensor("s5b_" + name, shape, dt))
        def o(eng, fn, reads, writes):
            p.op(eng, fn, reads=["s5b_" + r for r in reads], writes=["s5b_" + w for w in writes])
        w1b = [sb(f"w1b{i}", [128, KC, 2048], BF16) for i in range(2)]
        w2b = [sb(f"w2b{i}", [128, KC, D], BF16) for i in range(2)]
        b1t = sb("b1t", [128, NE * 16])
        b2c = [sb(f"b2c{i}", [128, D]) for i in range(2)]
        xg = [sb(f"xg{i}", [128, D], BF16) for i in range(2)]
        xTs = [sb(f"xT{i}", [128, KC, C], BF16) for i in range(2)]
        actT = sb("actT", [128, KC, C], BF16)
        gl = [sb(f"gl{i}", [128, SG]) for i in range(2)]
        sg_ = [sb(f"sg{i}", [128, SG]) for i in range(2)]
        ul = [sb(f"ul{i}", [128, SG]) for i in range(2)]
        ysb = [sb(f"ysb{i}", [128, D], BF16) for i in range(2)]
        pT = [ps(f"pT{i}", [128, D], BF16) for i in range(2)]
        pg = [ps(f"pg{i}", [128, 512]) for i in range(2)]
        pu = [ps(f"pu{i}", [128, 512]) for i in range(2)]
        pyy = [ps(f"py{i}", [128, 512]) for i in range(2)]
        p.dma("sp", lambda e: e.dma_start(out=b1t[:], in_=c.b1_l), writes=["s5b_b1t"])
        xs_keys = [f"XS{i}_{k}" for i in range(NT) for k in range(4)]
        nx = 0; ng = 0; ny = 0
        nxc = [0]
        def load_x(ex_):
            xT = xTs[ex_ % 2]; xk = f"s5b_xT{ex_ % 2}"
            for st in range(NST):
                u = nxc[0] % 2; nxc[0] += 1
                r0 = ex_ * C + st * 128
                p.dma("sp", lambda e, u=u, r0=r0: e.dma_start(out=xg[u][:], in_=c.XS[r0:r0 + 128, :]), reads=[], writes=[f"s5b_xg{u}"])
                for kc in range(KC):
                    o("pe", lambda e, u=u, kc=kc: e.transpose(out=pT[u][:, kc * 128:(kc + 1) * 128], in_=xg[u][:, kc * 128:(kc + 1) * 128], identity=c.ident_b[:]),
                      [f"xg{u}"], [f"pT{u}"])
                p.op("act" if st % 2 == 0 else "dve",
                     (lambda e, u=u, st=st: e.activation(out=xT[:, :, st * 128:(st + 1) * 128], in_=pT[u][:].rearrange("p (k t) -> p k t", k=KC), func=AF.Copy))
                     if st % 2 == 0 else
                     (lambda e, u=u, st=st: e.tensor_copy(out=xT[:, :, st * 128:(st + 1) * 128], in_=pT[u][:].rearrange("p (k t) -> p k t", k=KC))),
                     reads=[f"s5b_pT{u}"], writes=[xk])
        NSTG = 6
        stg = [sb(f"stg{i}", [128, 1024]) for i in range(NSTG)]
        nstg = [0]
        def chunk_list(ex_):
            wb = ex_ % 2
            out = []
            for kc in range(KC):
                for hf in range(2):
                    src = c.exp_w1[ex_][kc * 128:(kc + 1) * 128, hf * 1024:(hf + 1) * 1024]
                    dst = w1b[wb][:, kc, hf * 1024:(hf + 1) * 1024]
                    out.append((src, dst, f"s5b_w1b{wb}_{kc}"))
            for kc in range(KC):
                out.append((c.exp_w2[ex_][kc * 128:(kc + 1) * 128, :], w2b[wb][:, kc, :], f"s5b_w2b{wb}_{kc}"))
            items = []
            for (src, dst, key) in out:
                k = nstg[0] % NSTG; nstg[0] += 1
                def dma_t(src=src, k=k):
                    p.dma("sp", lambda e: e.dma_start(out=stg[k][:], in_=src), writes=[f"s5b_stg{k}"])
                def cast_t(dst=dst, k=k, key=key):
                    p.op("act", lambda e: e.activation(out=dst, in_=stg[k][:], func=AF.Copy), reads=[f"s5b_stg{k}"], writes=[key])
                items.append((dma_t, cast_t))
            return items
        def load_b2(ex_):
            wb = ex_ % 2
            p.dma("sp", lambda e, ex_=ex_, wb=wb: e.dma_start(out=b2c[wb][:], in_=c.exp_b2[ex_].partition_broadcast(128)), writes=[f"s5b_b2c{wb}"])
        for (dt_, ct_) in chunk_list(0):
            dt_(); ct_()
        load_b2(0)
        load_x(0)
        nslots = KC * NSG + NST * 2
        for ex_ in range(NE):
            wb = ex_ % 2
            pf = chunk_list(ex_ + 1) if ex_ + 1 < NE else []
            pfs = {"d": 0, "c": 0}
            per = -(-len(pf) // nslots) if pf else 0
            def pf_dma(n):
                for _ in range(n):
                    if pfs["d"] < len(pf):
                        pf[pfs["d"]][0](); pfs["d"] += 1
            def pf_slot():
                for _ in range(per):
                    if pfs["c"] < len(pf):
                        pf[pfs["c"]][1](); pfs["c"] += 1
                        pf_dma(1)
            if ex_ + 1 < NE:
                load_b2(ex_ + 1)
            pf_dma(NSTG)
            xT = xTs[ex_ % 2]; xk = f"xT{ex_ % 2}"
            w1v = w1b[wb][:].rearrange("p k (f two) -> p k f two", two=2)
            for fc in range(KC):
                for sgi in range(NSG):
                    u = ng % 2; ng += 1
                    ss_ = slice(sgi * SG, (sgi + 1) * SG)
                    for kc in range(KC):
                        o("pe", lambda e, u=u, kc=kc, fc=fc, ss_=ss_, w1v=w1v, xT=xT: e.matmul(pg[u][:, 0:SG], w1v[:, kc, fc * 128:(fc + 1) * 128, 0], xT[:, kc, ss_],
                                                                                    start=(kc == 0), stop=(kc == KC - 1)), [f"w1b{wb}_{kc}", xk], [f"pg{u}"])
                    for kc in range(KC):
                        o("pe", lambda e, u=u, kc=kc, fc=fc, ss_=ss_, w1v=w1v, xT=xT: e.matmul(pu[u][:, 0:SG], w1v[:, kc, fc * 128:(fc + 1) * 128, 1], xT[:, kc, ss_],
                                                                                    start=(kc == 0), stop=(kc == KC - 1)), [f"w1b{wb}_{kc}", xk], [f"pu{u}"])
                    bcol = ex_ * 16 + fc * 2
                    o("dve", lambda e, u=u, bcol=bcol: e.tensor_scalar(out=gl[u][:], in0=pg[u][:, 0:SG], scalar1=b1t[:, bcol:bcol + 1], scalar2=7.0,
                                                                       op0=ALU.add, op1=ALU.min), [f"pg{u}", "b1t"], [f"gl{u}"])
                    o("act", lambda e, u=u: e.activation(out=sg_[u][:], in_=gl[u][:], func=AF.Gelu_apprx_sigmoid), [f"gl{u}"], [f"sg{u}"])
                    o("dve", lambda e, u=u, bcol=bcol: e.tensor_scalar(out=ul[u][:], in0=pu[u][:, 0:SG], scalar1=b1t[:, bcol + 1:bcol + 2], scalar2=7.0,
                                                                       op0=ALU.add, op1=ALU.min), [f"pu{u}", "b1t"], [f"ul{u}"])
                    o("dve", lambda e, u=u: e.tensor_scalar(out=ul[u][:], in0=ul[u][:], scalar1=-7.0, scalar2=1.0, op0=ALU.max, op1=ALU.add), [f"ul{u}"], [f"ul{u}"])
                    o("dve", lambda e, u=u, fc=fc, ss_=ss_: e.tensor_tensor(out=actT[:, fc, ss_], in0=ul[u][:], in1=sg_[u][:], op=ALU.mult),
                      [f"ul{u}", f"sg{u}"], ["actT"])
                    pf_slot()
            if ex_ + 1 < NE:
                load_x(ex_ + 1)
            for st in range(NST):
                yb_ = ny % 2
                for n in range(2):
                    u = ny % 2; ny += 1
                    for fc in range(KC):
                        o("pe", lambda e, u=u, fc=fc, st=st, n=n, wb=wb: e.matmul(pyy[u][:], actT[:, fc, st * 128:(st + 1) * 128], w2b[wb][:, fc, n * 512:(n + 1) * 512],
                                                                           start=(fc == 0), stop=(fc == KC - 1)), ["actT", f"w2b{wb}_{fc}"], [f"py{u}"])
                    o("dve", lambda e, u=u, yb_=yb_, n=n, wb=wb: e.tensor_tensor(out=ysb[yb_][:, n * 512:(n + 1) * 512], in0=pyy[u][:], in1=b2c[wb][:, n * 512:(n + 1) * 512], op=ALU.add),
                      [f"py{u}", f"b2c{wb}"], [f"ysb{yb_}"])
                    pf_slot()
                r0 = ex_ * C + st * 128
                p.dma("sp", lambda e, yb_=yb_, r0=r0: e.dma_start(out=c.YS[r0:r0 + 128, :], in_=ysb[yb_][:]), reads=[f"s5b_ysb{yb_}"], writes=[])
            while pfs["c"] < len(pf):
                pf[pfs["c"]][1](); pfs["c"] += 1
                pf_dma(1)


def stage6(p, nc, c):
    T = c.T; NT = T // 128; C = c.CAP; NE = 32
    NST = C // 128
    from contextlib import ExitStack
    with ExitStack() as es:
        def sb(name, shape, dt=F32):
            return es.enter_context(nc.sbuf_tensor("s6_" + name, shape, dt))
        def ps(name, shape, dt=F32):
            return es.enter_context(nc.psum_tensor("s6_" + name, shape, dt))
        def o(eng, fn, reads, writes):
            p.op(eng, fn, reads=[(r[1:] if r.startswith("@") else "s6_" + r) for r in reads], writes=["s6_" + w for w in writes])
        gBp = sb("gBp", [128, D]); gBf = sb("gBf", [128, D])
        wg = sb("wg", [128, KC, D], BF16); wp = sb("wp", [128, 2, D], BF16)
        xt = [sb(f"xt{i}", [128, D]) for i in range(2)]
        yg = [[sb(f"yg{i}_{k}", [128, D], BF16) for k in range(4)] for i in range(2)]
        x3 = sb("x3", [128, D]); hp = sb("hp", [128, D], BF16); hpT = sb("hpT", [128, KC, 128], BF16)
        pt = [sb(f"pt{i}", [128, 256]) for i in range(2)]
        ptb = sb("ptb", [128, 256], BF16); pTs = sb("pTs", [128, 2, 128], BF16)
        sgm = sb("sgm", [128, D]); x4 = sb("x4", [128, D]); ot = [sb(f"ot{i}", [128, D]) for i in range(2)]
        junk = sb("junk", [128, D], BF16); ss = sb("ss", [128, 1]); rstd = sb("rstd", [128, 1])
        pT = ps("pT", [128, D], BF16); pT2 = ps("pT2", [128, 256], BF16)
        PG = [ps(f"PG{i}", [128, 512]) for i in range(2)]
        PQ = [ps(f"PQ{i}", [128, 512]) for i in range(2)]
        p.dma("sp", lambda e: e.dma_start(out=gBp[:], in_=c.ple_norm_g.partition_broadcast(128)), writes=["gB_s6p"])
        p.dma("sp", lambda e: e.dma_start(out=gBf[:], in_=c.final_norm_g.partition_broadcast(128)), writes=["gB_s6f"])
        p.dma("pool", lambda e: e.dma_start(out=wg[:], in_=c.ple_gate_w.rearrange("(kc p) n -> p kc n", p=128)), writes=["s6_wg"])
        p.dma("pool", lambda e: e.dma_start(out=wp[:], in_=c.ple_proj_w.rearrange("(kc p) n -> p kc n", p=128)), writes=["s6_wp"])
        ys_keys = [f"YS{e_}_{st}" for e_ in range(NE) for st in range(NST)]
        def fetch(i):
            b = i % 2
            p.dma("sp", lambda e, i=i, b=b: e.dma_start(out=xt[b][:], in_=c.X2[i * 128:(i + 1) * 128, :]), reads=[], writes=[f"s6_xt{b}"])
            p.dma("sp", lambda e, i=i, b=b: e.dma_start(out=pt[b][:], in_=c.pin[i * 128:(i + 1) * 128, :]), writes=[f"s6_pt{b}"])
            for k in range(4):
                o("dve", lambda e, b=b, k=k: e.memset(yg[b][k][:], 0.0), [], [f"yg{b}_{k}"])
                p.dma("pool", lambda e, i=i, k=k, b=b: e.indirect_dma_start(
                    out=yg[b][k][:], out_offset=None, in_=c.YS[:, :],
                    in_offset=bass.IndirectOffsetOnAxis(ap=c.slot4i[:, i, k:k + 1], axis=0), bounds_check=_bc_reg(c, e, NE * C - 1), oob_is_err=False),
                    reads=["s5_slot4i"], writes=[f"s6_yg{b}_{k}"], grp="i")
        x3s = [x3, sb("x3b", [128, D])]
        hpTs = [hpT, sb("hpTb", [128, KC, 128], BF16)]
        pTss = [pTs, sb("pTsb", [128, 2, 128], BF16)]
        junk2 = sb("junk2", [128, D], BF16); ss2 = sb("ss2", [128, 1]); rstd2 = sb("rstd2", [128, 1])
        def front(i):
            b = i % 2
            x3_ = x3s[b]; hpT_ = hpTs[b]; pTs_ = pTss[b]
            for k in range(4):
                src = xt[b] if k == 0 else x3_
                sk = f"xt{b}" if k == 0 else f"x3_{b}"
                o("dve", lambda e, k=k, src=src: e.scalar_tensor_tensor(out=x3_[:], in0=yg[b][k][:], scalar=c.gate4[:, i, k:k + 1], in1=src[:],
                                                                        op0=ALU.mult, op1=ALU.add), [f"yg{b}_{k}", sk, "@s5_gate4"], [f"x3_{b}"])
            rmsnorm_tile(p, c, x3_[:], f"s6_x3_{b}", gBp, hp[:], "s6_hp", "s6p", junk, ss, rstd)
            for kc in range(KC):
                o("pe", lambda e, kc=kc: e.transpose(out=pT[:, kc * 128:(kc + 1) * 128], in_=hp[:, kc * 128:(kc + 1) * 128], identity=c.ident_b[:]), ["hp"], ["pT"])
            o("act", lambda e: e.activation(out=hpT_[:], in_=pT[:].rearrange("p (k t) -> p k t", k=KC), func=AF.Copy), ["pT"], [f"hpT{b}"])
            o("act", lambda e: e.activation(out=ptb[:], in_=pt[b][:], func=AF.Copy), [f"pt{b}"], ["ptb"])
            for kc in range(2):
                o("pe", lambda e, kc=kc: e.transpose(out=pT2[:, kc * 128:(kc + 1) * 128], in_=ptb[:, kc * 128:(kc + 1) * 128], identity=c.ident_b[:]), ["ptb"], ["pT2"])
            o("dve", lambda e: e.tensor_copy(out=pTs_[:], in_=pT2[:].rearrange("p (k t) -> p k t", k=2)), ["pT2"], [f"pTs{b}"])
        def back(i):
            b = i % 2
            x3_ = x3s[b]; hpT_ = hpTs[b]; pTs_ = pTss[b]
            for n in range(2):
                ns = slice(n * 512, (n + 1) * 512)
                for kc in range(KC):
                    o("pe", lambda e, n=n, kc=kc, ns=ns: e.matmul(PG[n][:], hpT_[:, kc, :], wg[:, kc, ns], start=(kc == 0), stop=(kc == KC - 1)), [f"hpT{b}", "wg"], [f"PG{n}"])
                for kc in range(2):
                    o("pe", lambda e, n=n, kc=kc, ns=ns: e.matmul(PQ[n][:], pTs_[:, kc, :], wp[:, kc, ns], start=(kc == 0), stop=(kc == 1)), [f"pTs{b}", "wp"], [f"PQ{n}"])
                o("act", lambda e, n=n, ns=ns: e.activation(out=sgm[:, ns], in_=PG[n][:], func=AF.Sigmoid), [f"PG{n}"], [f"sgm{n}"])
                o("dve", lambda e, n=n, ns=ns: e.tensor_tensor(out=sgm[:, ns], in0=PQ[n][:], in1=sgm[:, ns], op=ALU.mult), [f"PQ{n}", f"sgm{n}"], [f"sgm{n}"])
                o("dve", lambda e, n=n, ns=ns: e.tensor_tensor(out=x4[:, ns], in0=sgm[:, ns], in1=x3_[:, ns], op=ALU.add), [f"sgm{n}", f"x3_{b}"], [f"x4{n}"])
            p.op("act", lambda e: e.activation(out=junk2[:], in_=x4[:], func=AF.Square, accum_out=ss2[:]), reads=["s6_x40", "s6_x41"], writes=["s6fjunk", "s6fss"])
            p.op("act", lambda e: e.activation(out=rstd2[:], in_=ss2[:], func=AF.Sqrt, scale=1.0 / D, bias=c.eps_t[:]), reads=["s6fss", "eps_t"], writes=["s6frstd"])
            p.op("dve", lambda e: e.reciprocal(out=rstd2[:], in_=rstd2[:]), reads=["s6frstd"], writes=["s6frstd"])
            p.op("dve", lambda e: e.scalar_tensor_tensor(out=ot[b][:], in0=x4[:], scalar=rstd2[:], in1=gBf[:], op0=ALU.mult, op1=ALU.mult),
                 reads=["s6_x40", "s6_x41", "s6frstd", "gB_s6f"], writes=[f"s6_ot{b}"])
            p.dma("sp", lambda e: e.dma_start(out=c.out[i * 128:(i + 1) * 128, :], in_=ot[b][:]), reads=[f"s6_ot{b}"], writes=[f"out{i}"])
        fetch(0)
        if NT > 1:
            fetch(1)
        front(0)
        for i in range(NT):
            def nxt(i=i):
                if i + 2 < NT:
                    fetch(i + 2)
                front(i + 1)
            p.merged(lambda i=i: back(i), nxt if i + 1 < NT else None)


def build(cfg):
    nc = bass.Bass("TRN2", target_bir_lowering=False)
    c = Ctx()
    c.S = cfg["S"]; c.NB = cfg["NB"]; c.T = c.S * c.NB
    c.TG = min(512, c.T)
    c.debug = cfg.get("debug", False)
    c.HI = cfg.get("HI", 4)
    c.R32 = cfg.get("R32", False)
    T = c.T
    c.in_shapes = {}
    def din(name, shape, dt=F32):
        c.in_shapes[name] = (tuple(shape), dt)
        return nc.dram_tensor(name, shape, dt, kind="ExternalInput").ap()
    def dscr(name, shape, dt):
        kind = "ExternalOutput" if (c.debug and name in cfg.get("dbg_out", ())) else "Internal"
        return nc.dram_tensor(name, shape, dt, kind=kind).ap()
    c.x = din("x", [T, D])
    c.mix_norm_g = din("mix_norm_g", [D])
    c.w_in = din("w_in", [D, IN_COLS])
    c.ident_b_d = din("ident_b", [128, 128], BF16)
    c.QT = dscr("QT", [512, T], BF16)
    c.KT = dscr("KT", [512, T], BF16)
    c.V = dscr("V", [T, 512], BF16)
    c.ZR = dscr("ZR", [RW_COLS, T], F32)
    c.GT = dscr("GT", [2048, T], BF16)
    c.YA = dscr("YA", [512, T], BF16)
    c.YB = dscr("YB", [512, T], BF16)
    c.X2 = dscr("X2", [T, D], F32)
    c.CAP = cfg["CAP"]
    c.XS = dscr("XS", [32 * c.CAP, D], BF16)
    c.YS = dscr("YS", [32 * c.CAP, D], BF16)
    c.out = nc.dram_tensor("out", [T, D], F32, kind="ExternalOutput").ap()
    c.pin = din("p", [T, 256])
    c.ffn_norm_g = din("ffn_norm_g", [D]); c.router_w = din("router_w", [D, 32]); c.router_b = din("router_b", [1, 32])
    c.exp_w1 = din("exp_w1", [32, D, 2048]); c.exp_w2 = din("exp_w2", [32, D, D]); c.b1_l = din("b1_l", [128, 512]); c.exp_b2 = din("exp_b2", [32, D])
    c.ple_norm_g = din("ple_norm_g", [D]); c.ple_gate_w = din("ple_gate_w", [D, D]); c.ple_proj_w = din("ple_proj_w", [256, D]); c.final_norm_g = din("final_norm_g", [D])
    c.SUt_d = din("SUt", [128, 128]); c.eC_d = din("eC", [128, 32])
    c.w_out_a = din("w_out_a", [512, D]); c.w_out_b = din("w_out_b", [512, D]); c.w_out = din("w_out", [D, D])
    c.pc_d = din("pc", [64, 80]); c.mul_d = din("mul", [128, 2])
    c.w_up = din("w_up", [64, 512]); c.a_up = din("a_up", [64, 512]); c.g_up = din("g_up", [128, 512])
    c.resetm_d = din("resetm", [64, 512]); c.M1_d = din("M1", [128, 128]); c.MB_d = din("MB", [128, 256]); c.MC_d = din("MC", [128, 256])
    c.identf_d = din("identf", [128, 128]); c.ones64_d = din("ones64", [64, 64]); c.TI0_d = din("TI0", [128, 64])
    c.maskS_d = din("maskS", [128, 128], F32)
    c.negU_d = din("negU", [128, 128], BF16)
    c.negOnes_d = din("negOnes", [128, 128], BF16)
    p = Prog(nc)
    from contextlib import ExitStack
    with ExitStack() as es:
        c.ident_b = es.enter_context(nc.sbuf_tensor("ident_b_sb", [128, 128], BF16))
        c.eps_t = es.enter_context(nc.sbuf_tensor("eps_t", [128, 1], F32))
        p.dma("sp", lambda e: e.dma_start(out=c.ident_b[:], in_=c.ident_b_d), writes=["ident_b"])
        p.op("pool", lambda e: e.memset(c.eps_t[:], RMS_EPS), writes=["eps_t"])
        c.tiny_t = es.enter_context(nc.sbuf_tensor("tiny_t", [128, 1], F32))
        c.gneps_t = es.enter_context(nc.sbuf_tensor("gneps_t", [128, 1], F32))
        p.op("pool", lambda e: e.memset(c.tiny_t[:], 1e-24), writes=["s3_tiny"])
        p.op("pool", lambda e: e.memset(c.gneps_t[:], 64e-5), writes=["s3_gneps"])
        c.maskS = es.enter_context(nc.sbuf_tensor("maskS_sb", [128, 128], F32))
        c.negU = es.enter_context(nc.sbuf_tensor("negU_sb", [128, 128], BF16))
        c.negOnes = es.enter_context(nc.sbuf_tensor("negOnes_sb", [128, 128], BF16))
        p.dma("sp", lambda e: e.dma_start(out=c.maskS[:], in_=c.maskS_d), writes=["maskS"])
        p.dma("sp", lambda e: e.dma_start(out=c.negU[:], in_=c.negU_d), writes=["negU"])
        p.dma("sp", lambda e: e.dma_start(out=c.negOnes[:], in_=c.negOnes_d), writes=["negU"])
        stages = cfg.get("stages", (1, 2))
        zt = es.enter_context(nc.sbuf_tensor("zero_t", [128, 4096], BF16))
        p.op("pool", lambda e: e.memset(zt[:], 0.0), writes=["zero_t"])
        nrow = 32 * c.CAP
        for r0 in range(0, nrow, 512):
            p.dma("sp", lambda e, r0=r0: e.dma_start(out=c.XS[r0:r0 + 512, :].rearrange("(p a) n -> p a n", a=4), in_=zt[:].rearrange("p (a n) -> p a n", a=4)),
                  reads=["zero_t"], writes=[])
        p.barrier()
        if 1 in stages:
            stage1(p, nc, c)
            p.barrier()
        if 2 in stages:
            stage2(p, nc, c)
            p.barrier()
        if 3 in stages:
            stage3(p, nc, c)
            p.barrier()
        if 4 in stages:
            stage4(p, nc, c)
            p.barrier()
        if 5 in stages:
            stage5a(p, nc, c, es)
            p.barrier()
            if c.debug:
                c.dbg_slot = nc.dram_tensor("dbg_slot", [128, (T // 128) * 4], I32, kind="ExternalOutput").ap()
                c.dbg_gate = nc.dram_tensor("dbg_gate", [128, (T // 128) * 4], F32, kind="ExternalOutput").ap()
                stage5_dbg(p, nc, c)
            stage5b(p, nc, c)
            p.barrier()
            stage6(p, nc, c)
        p.finish()
        p.emit()
    build.last_in_shapes = c.in_shapes
    return nc


def consts():
    import ml_dtypes
    i = np.arange(128)
    return {"ident_b": np.eye(128, dtype=np.float32).astype(ml_dtypes.bfloat16),
            "maskS": (i[:, None] < i[None, :]).astype(np.float32),
            "negU": (-(i[:, None] >= i[None, :]).astype(np.float32)).astype(ml_dtypes.bfloat16),
            "negOnes": (-np.ones((128, 128), np.float32)).astype(ml_dtypes.bfloat16),
            **rw_consts()}


def rw_consts():
    i = np.arange(128)
    same = (i[:, None] // 32) == (i[None, :] // 32)
    lt = i[:, None] < i[None, :]
    le = i[:, None] <= i[None, :]
    gt = i[:, None] > i[None, :]
    f = lambda m: m.astype(np.float32)
    TI0 = np.zeros((128, 64), np.float32); TI0[64:, :] = np.eye(64)
    return {"M1": -f(same & gt), "MB": np.concatenate([-f(same & lt), f(same & le)], 1),
            "MC": np.concatenate([f(same & lt), f(same & le)], 1),
            "identf": np.eye(128, dtype=np.float32), "ones64": np.ones((64, 64), np.float32), "TI0": TI0,
            "resetm": np.tile(f(np.arange(512) % 32 != 0)[None, :], (64, 1))}


def moe_consts(cap):
    i = np.arange(128)
    return {"SUt": (i[:, None] < i[None, :]).astype(np.float32),
            "eC": np.tile((np.arange(32) * cap).astype(np.float32)[None, :], (128, 1))}


def b1_layout(b1):
    return np.ascontiguousarray(np.asarray(b1).reshape(32, 8, 128, 2).transpose(2, 0, 1, 3).reshape(128, 512)).astype(np.float32)


def rw_params(mu, w0, a0, k_k, k_a, r_k, lnx_w, lnx_b):
    hd = lambda v: np.ascontiguousarray(np.asarray(v).reshape(8, 64).T)
    pc = np.concatenate([hd(mu[0:512]), hd(mu[512:1024]), hd(mu[1024:1536]), hd(w0), hd(a0), hd(k_k), hd(k_a),
                         hd(r_k.reshape(-1)), hd(lnx_w), hd(lnx_b)], axis=1).astype(np.float32)
    mul = np.ascontiguousarray(np.stack([mu[1536:1664], mu[1664:1792]], 1)).astype(np.float32)
    return {"pc": pc, "mul": mul}


_S = 2048
_NB = 2
_CAP = 640


def kernel(**inputs):
    x = np.asarray(inputs["x"], dtype=np.float32)
    B, S, _ = x.shape
    assert S == _S and B == 8 * _NB
    g0 = lambda k: np.ascontiguousarray(np.asarray(inputs[k], dtype=np.float32)[0])
    pin = np.asarray(inputs["p"], dtype=np.float32)[0]
    common = dict(consts())
    common.update(moe_consts(_CAP))
    common.update(rw_params(g0("rwkv_mu"), g0("rwkv_w0"), g0("rwkv_a0"), g0("rwkv_k_k"), g0("rwkv_k_a"), g0("rwkv_r_k"),
                            g0("rwkv_lnx_w"), g0("rwkv_lnx_b")))
    common.update({
        "mix_norm_g": g0("mix_norm_g"), "w_in": g0("w_in"),
        "w_up": g0("rwkv_w_up"), "a_up": g0("rwkv_a_up"), "g_up": g0("rwkv_g_up"),
        "w_out_a": g0("w_out_a"), "w_out_b": g0("w_out_b"), "w_out": g0("w_out"),
        "ffn_norm_g": g0("ffn_norm_g"), "router_w": g0("router_w"),
        "router_b": np.ascontiguousarray(np.asarray(inputs["router_b"], dtype=np.float32).reshape(1, 32)),
        "exp_w1": g0("exp_w1"), "exp_w2": g0("exp_w2"), "b1_l": b1_layout(g0("exp_b1")), "exp_b2": g0("exp_b2"),
        "ple_norm_g": g0("ple_norm_g"), "ple_gate_w": g0("ple_gate_w"), "ple_proj_w": g0("ple_proj_w"),
        "final_norm_g": np.ascontiguousarray(np.asarray(inputs["final_norm_g"], dtype=np.float32)),
    })
    nc = build(dict(S=_S, NB=_NB, CAP=_CAP, stages=(1, 2, 3, 4, 5)))
    in_maps = []
    for ci in range(8):
        m = dict(common)
        m["x"] = np.ascontiguousarray(x[ci * _NB:(ci + 1) * _NB].reshape(_NB * S, D))
        m["p"] = np.ascontiguousarray(pin[ci * _NB:(ci + 1) * _NB].reshape(_NB * S, 256))
        in_maps.append(m)
    res = run_bass_kernel_spmd(nc, in_maps, core_ids=list(range(8)))
    outs = [np.asarray(r["out"], dtype=np.float32).reshape(_NB, S, D) for r in res.results]
    return np.concatenate(outs, axis=0)
```
